# Optimizing a Trainium2 kernel written in Bass

```python
import math
import jax, jax.numpy as jnp
from jax import lax
import numpy as np


D_MODEL = 1024
BATCH = 4
SEQ = 8192
DEPTH = 1

HEAD_DIM = 64
ROPE_DIM = HEAD_DIM // 4
ROPE_THETA = 500000.0
Q_BLOCK = 128
A_HEADS = 8
A_KV_RANK = 256
A_TOPK_MAX = 256
IDX_HEADS = 8
IDX_DIM = 32
IDX_ROPE = IDX_DIM // 4
B_PATTERNS = ((128, 1), (512, 4), (2048, 16))
B_HEADS_PER_GROUP = 4
B_HEADS = B_HEADS_PER_GROUP * len(B_PATTERNS)
N_EXPERTS = 64
TOP_K = 8
N_GROUPS = 8
TOPK_GROUPS = 4
D_EXPERT = 256
D_SHARED = 256
ROUTED_SCALE = 2.5
MOE_BLOCK = 128
ALPHA = (2.0 * DEPTH) ** 0.25
BETA = (8.0 * DEPTH) ** -0.25
LN_EPS = 1e-5
RMS_EPS = 1e-6
IN_SIZES = (A_HEADS * HEAD_DIM, A_KV_RANK, ROPE_DIM, IDX_HEADS * IDX_DIM, IDX_DIM, IDX_HEADS,
            B_HEADS * HEAD_DIM, B_HEADS * HEAD_DIM, B_HEADS * HEAD_DIM, D_MODEL, D_MODEL)
IN_TOTAL = sum(IN_SIZES)
V_COL_BLOCK = 8

kernel_name = 'hybrid_dsa_dilated_moe_deepnorm'


def layer_norm(x, g, b):
    xf = x.astype(jnp.float32)
    mu = jnp.mean(xf, -1, keepdims=True)
    var = jnp.mean(jnp.square(xf - mu), -1, keepdims=True)
    y = (xf - mu) * lax.rsqrt(var + LN_EPS) * g.astype(jnp.float32) + b.astype(jnp.float32)
    return y.astype(x.dtype)


def rms_norm(x, g):
    xf = x.astype(jnp.float32)
    y = xf * lax.rsqrt(jnp.mean(jnp.square(xf), -1, keepdims=True) + RMS_EPS) * g.astype(jnp.float32)
    return y.astype(x.dtype)


def rope(x, pos):
    dim = x.shape[-1]
    inv = ROPE_THETA ** (-jnp.arange(0, dim, 2, dtype=jnp.float32) / dim)
    ang = pos.astype(jnp.float32)[:, :, None] * inv
    cos = jnp.cos(ang)[:, :, None, :]
    sin = jnp.sin(ang)[:, :, None, :]
    xf = x.astype(jnp.float32)
    x1, x2 = xf[..., :dim // 2], xf[..., dim // 2:]
    return jnp.concatenate([x1 * cos - x2 * sin, x2 * cos + x1 * sin], -1).astype(x.dtype)


def partial_rope(x, pos, rot):
    return jnp.concatenate([rope(x[..., :rot], pos), x[..., rot:]], -1)


def to_blocks(x):
    b, s = x.shape[:2]
    return jnp.moveaxis(x.reshape(b, s // Q_BLOCK, Q_BLOCK, *x.shape[2:]), 1, 0)


def from_blocks(x):
    y = jnp.moveaxis(x, 0, 1)
    return y.reshape(y.shape[0], y.shape[1] * y.shape[2], *y.shape[3:])


def dsa_mixer(q_a, ckv, k_rope, iq, ik, iw, w_uk, w_uv):
    s_len = q_a.shape[1]
    n_sel = min(A_TOPK_MAX, s_len // 4)
    scale = HEAD_DIM ** -0.5
    key_pos = jnp.arange(s_len)

    def block(args):
        qb, iqb, iwb, start = args
        tq = start + jnp.arange(Q_BLOCK)
        causal = key_pos[None, :] <= tq[:, None]
        s = jnp.einsum('bthd,bsd->bths', iqb, ik)
        score = jnp.einsum('bths,bth->bts', jax.nn.relu(s), iwb).astype(jnp.float32)
        score = jnp.where(causal[None], score, -jnp.inf)
        _, idx = lax.top_k(score, n_sel)
        valid = idx <= tq[None, :, None]
        c_sel = jax.vmap(lambda c, i: c[i])(ckv, idx)
        r_sel = jax.vmap(lambda r, i: r[i])(k_rope, idx)
        q_lat = jnp.einsum('bthd,rhd->bthr', qb[..., ROPE_DIM:], w_uk)
        logits = (jnp.einsum('bthr,btkr->bthk', q_lat, c_sel)
                  + jnp.einsum('bthd,btkd->bthk', qb[..., :ROPE_DIM], r_sel)).astype(jnp.float32) * scale
        logits = jnp.where(valid[:, :, None, :], logits, -jnp.inf)
        p = jax.nn.softmax(logits, axis=-1).astype(c_sel.dtype)
        o_lat = jnp.einsum('bthk,btkr->bthr', p, c_sel)
        return jnp.einsum('bthr,rhd->bthd', o_lat, w_uv)

    starts = jnp.arange(s_len // Q_BLOCK, dtype=jnp.int32) * Q_BLOCK
    out = lax.map(block, (to_blocks(q_a), to_blocks(iq), to_blocks(iw), starts))
    return from_blocks(out)


def dilated_mixer(qb, kb, vb):
    s_len = qb.shape[1]
    scale = HEAD_DIM ** -0.5
    groups = []
    for g, (win, dil) in enumerate(B_PATTERNS):
        hs = slice(g * B_HEADS_PER_GROUP, (g + 1) * B_HEADS_PER_GROUP)
        groups.append((hs, win, dil, kb[:, :, hs], vb[:, :, hs]))

    def block(args):
        q, start = args
        tq = start + jnp.arange(Q_BLOCK)
        outs, lses = [], []
        for hs, win, dil, kg, vg in groups:
            n_keys = win // dil + 1
            kpos = tq[:, None] - dil * jnp.arange(n_keys)[None, :]
            valid = kpos >= 0
            kpos = jnp.maximum(kpos, 0)
            ksel = jnp.take(kg, kpos, axis=1)
            vsel = jnp.take(vg, kpos, axis=1)
            l = jnp.einsum('bthd,btjhd->bthj', q[:, :, hs], ksel).astype(jnp.float32) * scale
            l = jnp.where(valid[None, :, None, :], l, -jnp.inf)
            m = jnp.max(l, -1, keepdims=True)
            e = jnp.exp(l - m)
            den = jnp.sum(e, -1)
            o = jnp.einsum('bthj,btjhd->bthd', e, vsel.astype(jnp.float32)) / den[..., None]
            outs.append(o)
            lses.append(m[..., 0] + jnp.log(den))
        wts = jax.nn.softmax(jnp.stack(lses), axis=0)
        return jnp.sum(wts[..., None] * jnp.stack(outs), axis=0).astype(q.dtype)

    starts = jnp.arange(s_len // Q_BLOCK, dtype=jnp.int32) * Q_BLOCK
    return from_blocks(lax.map(block, (to_blocks(qb), starts)))


def routed_experts(hf, top_idx, top_w, w1_e, w3_e, w2_e):
    n_tok, d = hf.shape
    n_asg = n_tok * TOP_K
    flat_e = top_idx.reshape(-1).astype(jnp.int32)
    flat_t = jnp.repeat(jnp.arange(n_tok, dtype=jnp.int32), TOP_K)
    flat_w = top_w.reshape(-1)
    order = jnp.argsort(flat_e)
    se, st, sw = flat_e[order], flat_t[order], flat_w[order]
    counts = jnp.bincount(flat_e, length=N_EXPERTS)
    padded = (counts + MOE_BLOCK - 1) // MOE_BLOCK * MOE_BLOCK
    start = jnp.cumsum(counts) - counts
    pend = jnp.cumsum(padded)
    pstart = pend - padded
    dest = pstart[se] + (jnp.arange(n_asg) - start[se])
    n_rows = n_asg + N_EXPERTS * MOE_BLOCK
    n_blk = n_rows // MOE_BLOCK
    row_t = jnp.full((n_rows,), n_tok, jnp.int32).at[dest].set(st)
    row_w = jnp.zeros((n_rows,), hf.dtype).at[dest].set(sw.astype(hf.dtype))
    blk_e = jnp.minimum(jnp.searchsorted(pend, jnp.arange(n_blk) * MOE_BLOCK, side='right'), N_EXPERTS - 1)
    x_pad = jnp.concatenate([hf, jnp.zeros((1, d), hf.dtype)], 0)

    def step(acc, inp):
        t, w, e = inp
        xb = x_pad[t]
        hb = jax.nn.silu(xb @ w1_e[e]) * (xb @ w3_e[e])
        return acc.at[t].add((hb @ w2_e[e]) * w[:, None]), None

    acc, _ = lax.scan(step, jnp.zeros((n_tok + 1, d), hf.dtype),
                      (row_t.reshape(n_blk, MOE_BLOCK), row_w.reshape(n_blk, MOE_BLOCK), blk_e))
    return acc[:n_tok]


def moe(h, w_router, router_bias, w1_e, w3_e, w2_e, ws1, ws3, ws2):
    b, s, d = h.shape
    hf = h.reshape(-1, d)
    n_tok = hf.shape[0]
    scores = jax.nn.sigmoid((hf @ w_router).astype(jnp.float32))
    biased = scores + router_bias.astype(jnp.float32)
    grp = biased.reshape(n_tok, N_GROUPS, N_EXPERTS // N_GROUPS)
    grp_score = jnp.sum(lax.top_k(grp, 2)[0], -1)
    _, gidx = lax.top_k(grp_score, TOPK_GROUPS)
    gmask = jnp.any(gidx[..., None] == jnp.arange(N_GROUPS), axis=1)
    emask = jnp.repeat(gmask, N_EXPERTS // N_GROUPS, axis=1)
    _, top_idx = lax.top_k(jnp.where(emask, biased, -jnp.inf), TOP_K)
    top_s = jnp.take_along_axis(scores, top_idx, axis=1)
    top_w = top_s / jnp.sum(top_s, -1, keepdims=True) * ROUTED_SCALE
    routed = routed_experts(hf, top_idx, top_w, w1_e, w3_e, w2_e)
    shared = (jax.nn.silu(hf @ ws1) * (hf @ ws3)) @ ws2
    return (routed + shared).reshape(b, s, d)


def hybrid_layer(x, positions, w_in, b_gate, g_kv, w_uk, w_uv, w_branch_a, w_branch_b, w_o,
                 ln1_g, ln1_b, w_router, router_bias, w1_e, w3_e, w2_e, ws1, ws3, ws2, ln2_g, ln2_b):
    b, s, d = x.shape
    proj = x @ w_in
    splits = np.cumsum(IN_SIZES)[:-1].tolist()
    aq, ckv, akr, iq, ik, iw, bq, bk, bv, ga, gb = jnp.split(proj, splits, axis=-1)
    g_a = jax.nn.sigmoid(ga + b_gate[:d])
    g_b = jax.nn.sigmoid(gb + b_gate[d:])
    aq = partial_rope(aq.reshape(b, s, A_HEADS, HEAD_DIM), positions, ROPE_DIM)
    ckv = rms_norm(ckv, g_kv)
    akr = rope(akr[:, :, None, :], positions)[:, :, 0]
    iq = partial_rope(iq.reshape(b, s, IDX_HEADS, IDX_DIM), positions, IDX_ROPE)
    ik = partial_rope(ik[:, :, None, :], positions, IDX_ROPE)[:, :, 0]
    iw = iw * (IDX_HEADS * IDX_DIM) ** -0.5
    a_out = dsa_mixer(aq, ckv, akr, iq, ik, iw, w_uk, w_uv).reshape(b, s, A_HEADS * HEAD_DIM)
    bq = partial_rope(bq.reshape(b, s, B_HEADS, HEAD_DIM), positions, ROPE_DIM)
    bk = partial_rope(bk.reshape(b, s, B_HEADS, HEAD_DIM), positions, ROPE_DIM)
    bv = bv.reshape(b, s, B_HEADS, HEAD_DIM)
    b_out = dilated_mixer(bq, bk, bv).reshape(b, s, B_HEADS_PER_GROUP * HEAD_DIM)
    mix = (g_a * (a_out @ w_branch_a) + g_b * (b_out @ w_branch_b)) @ w_o
    h = layer_norm(ALPHA * x + mix, ln1_g, ln1_b)
    ffn = moe(h, w_router, router_bias, w1_e, w3_e, w2_e, ws1, ws3, ws2)
    return layer_norm(ALPHA * h + ffn, ln2_g, ln2_b)


def setup_inputs(seed: int = 0) -> dict:
    key = jax.random.key(seed)
    ks = jax.random.split(key, 24)
    f32 = jnp.float32
    L, D = DEPTH, D_MODEL

    def nrm(k, shape, scale):
        return jax.random.normal(k, shape, f32) * scale

    x = jax.random.normal(ks[0], (BATCH, SEQ, D), f32)
    offset = jax.random.randint(ks[1], (BATCH, 1), 0, 4096, dtype=jnp.int32)
    positions = offset + jnp.arange(SEQ, dtype=jnp.int32)[None, :]
    col_scale = jnp.concatenate([jnp.full((n,), BETA if i == V_COL_BLOCK else 1.0, f32)
                                 for i, n in enumerate(IN_SIZES)])
    w_in = nrm(ks[2], (L, D, IN_TOTAL), D ** -0.5) * col_scale
    b_gate = nrm(ks[3], (L, 2 * D), 0.02)
    g_kv = 1.0 + nrm(ks[4], (L, A_KV_RANK), 0.02)
    w_uk = nrm(ks[5], (L, A_KV_RANK, A_HEADS, HEAD_DIM - ROPE_DIM), A_KV_RANK ** -0.5)
    w_uv = nrm(ks[6], (L, A_KV_RANK, A_HEADS, HEAD_DIM), BETA * A_KV_RANK ** -0.5)
    w_branch_a = nrm(ks[7], (L, A_HEADS * HEAD_DIM, D), BETA * (A_HEADS * HEAD_DIM) ** -0.5)
    w_branch_b = nrm(ks[8], (L, B_HEADS_PER_GROUP * HEAD_DIM, D), BETA * (B_HEADS_PER_GROUP * HEAD_DIM) ** -0.5)
    w_o = nrm(ks[9], (L, D, D), BETA * D ** -0.5)
    ln1_g = 1.0 + nrm(ks[10], (L, D), 0.02)
    ln1_b = nrm(ks[11], (L, D), 0.02)
    w_router = nrm(ks[12], (L, D, N_EXPERTS), D ** -0.5)
    router_bias = nrm(ks[13], (L, N_EXPERTS), 0.01)
    w1_e = nrm(ks[14], (L, N_EXPERTS, D, D_EXPERT), BETA * D ** -0.5)
    w3_e = nrm(ks[15], (L, N_EXPERTS, D, D_EXPERT), BETA * D ** -0.5)
    w2_e = nrm(ks[16], (L, N_EXPERTS, D_EXPERT, D), BETA * D_EXPERT ** -0.5)
    ws1 = nrm(ks[17], (L, D, D_SHARED), BETA * D ** -0.5)
    ws3 = nrm(ks[18], (L, D, D_SHARED), BETA * D ** -0.5)
    ws2 = nrm(ks[19], (L, D_SHARED, D), BETA * D_SHARED ** -0.5)
    ln2_g = 1.0 + nrm(ks[20], (L, D), 0.02)
    ln2_b = nrm(ks[21], (L, D), 0.02)
    return {'x': x, 'positions': positions, 'w_in': w_in, 'b_gate': b_gate, 'g_kv': g_kv,
            'w_uk': w_uk, 'w_uv': w_uv, 'w_branch_a': w_branch_a, 'w_branch_b': w_branch_b,
            'w_o': w_o, 'ln1_g': ln1_g, 'ln1_b': ln1_b, 'w_router': w_router,
            'router_bias': router_bias, 'w1_e': w1_e, 'w3_e': w3_e, 'w2_e': w2_e,
            'ws1': ws1, 'ws3': ws3, 'ws2': ws2, 'ln2_g': ln2_g, 'ln2_b': ln2_b}


def reference(x, positions, w_in, b_gate, g_kv, w_uk, w_uv, w_branch_a, w_branch_b, w_o,
              ln1_g, ln1_b, w_router, router_bias, w1_e, w3_e, w2_e, ws1, ws3, ws2, ln2_g, ln2_b):
    h = x
    for l in range(DEPTH):
        h = hybrid_layer(h, positions, w_in[l], b_gate[l], g_kv[l], w_uk[l], w_uv[l],
                         w_branch_a[l], w_branch_b[l], w_o[l], ln1_g[l], ln1_b[l],
                         w_router[l], router_bias[l], w1_e[l], w3_e[l], w2_e[l],
                         ws1[l], ws3[l], ws2[l], ln2_g[l], ln2_b[l])
    return h
```

```python
import math
from contextlib import ExitStack

import numpy as np
import concourse.bass as bass
import concourse.mybir as mybir
from concourse.bass_utils import run_bass_kernel_spmd

F32 = mybir.dt.float32
BF16 = mybir.dt.bfloat16
I32 = mybir.dt.int32
U32 = mybir.dt.uint32
AF = mybir.ActivationFunctionType
ALU = mybir.AluOpType
AX = mybir.AxisListType

ENGS = ["tensor", "vector", "scalar", "gpsimd", "sync"]

D = 1024
S = 8192
NBLK = 64
NOWN = 32
ROPE_THETA = 500000.0
NEG = -1.0e30
N_BISECT = 16
TWO_PI = 2.0 * math.pi
CW1 = 6.28125
CW2 = TWO_PI - CW1


class Buf:
    __slots__ = ("name", "w", "readers", "dma_sem", "dma_cnt", "excl")

    def __init__(self, name, excl=False):
        self.name = name
        self.excl = excl
        self.w = None
        self.readers = []
        self.dma_sem = None
        self.dma_cnt = 0


class Prog:
    def __init__(self, nc, stack):
        self.nc = nc
        self.stack = stack
        self.ops = {e: [] for e in ENGS}
        self.cnt = {e: 0 for e in ENGS}
        self.sems = {e: stack.enter_context(nc.semaphore("s_" + e)) for e in ENGS}
        self.known = {e: {} for e in ENGS}
        self.nsem = len(ENGS)
        self.dma_bufs = []

    def new_sem(self, name):
        self.nsem += 1
        return self.stack.enter_context(self.nc.semaphore("%s_%d" % (name, self.nsem)))

    def _add_wait(self, waits, tok, eng):
        if tok is None:
            return
        if tok[0] == "e":
            if tok[1] == eng and eng in ("tensor", "sync"):
                return
            key = ("e", tok[1])
            val = tok[2]
        else:
            key = ("d", id(tok[1]))
            val = tok[2] * 16
            waits.setdefault("_sem", {})[key] = tok[1].dma_sem
        if waits.get(key, 0) < val:
            waits[key] = val

    def op(self, eng, fn, reads=(), writes=(), dma_out=None):
        xr = [b for b in reads if b.excl]
        if xr:
            reads = [b for b in reads if not b.excl]
            writes = list(writes) + [b for b in xr if b not in writes]
        waits = {}
        for b in reads:
            self._add_wait(waits, b.w, eng)
        for b in writes:
            self._add_wait(waits, b.w, eng)
            for r in b.readers:
                self._add_wait(waits, r, eng)
        semmap = waits.pop("_sem", {})
        wl = []
        kn = self.known[eng]
        for key, val in waits.items():
            if kn.get(key, 0) >= val:
                continue
            kn[key] = val
            if key[0] == "e":
                wl.append((self.sems[key[1]], val))
            else:
                wl.append((semmap[key], val))
        if dma_out is not None:
            if dma_out.dma_sem is None:
                dma_out.dma_sem = self.new_sem("d_" + dma_out.name)
                self.dma_bufs.append(dma_out)
            dma_out.dma_cnt += 1
            tok = ("d", dma_out, dma_out.dma_cnt)
            self.ops[eng].append((wl, fn, (dma_out.dma_sem, 16)))
        else:
            self.cnt[eng] += 1
            tok = ("e", eng, self.cnt[eng])
            self.ops[eng].append((wl, fn, (self.sems[eng], 1)))
        for b in reads:
            b.readers.append(tok)
            if len(b.readers) > 64:
                b.readers = b.readers[-48:]
        for b in writes:
            b.w = tok
            b.readers = []
        return tok

    def barrier(self):
        for eng in ENGS:
            wl = []
            for f in ENGS:
                if f != eng and self.cnt[f] > 0:
                    wl.append((self.sems[f], self.cnt[f]))
                    self.known[eng][("e", f)] = self.cnt[f]
            for b in self.dma_bufs:
                wl.append((b.dma_sem, b.dma_cnt * 16))
                self.known[eng][("d", id(b))] = b.dma_cnt * 16
            self.ops[eng].append((wl, None, None))

    def final_wait(self, eng, bufs):
        waits = {}
        for b in bufs:
            self._add_wait(waits, b.w, eng)
        semmap = waits.pop("_sem", {})
        wl = []
        for key, val in waits.items():
            if key[0] == "e":
                wl.append((self.sems[key[1]], val))
            else:
                wl.append((semmap[key], val))
        self.ops[eng].append((wl, None, None))

    def emit(self):
        nc = self.nc
        with nc.Block() as block:
            def mk(ename):
                def body(engine):
                    for wl, fn, inc in self.ops[ename]:
                        for sem, val in wl:
                            engine.wait_ge(sem, val)
                        if fn is not None:
                            ins = fn(engine)
                            ins.then_inc(inc[0], inc[1])
                return body
            block.tensor(mk("tensor"))
            block.vector(mk("vector"))
            block.scalar(mk("scalar"))
            block.gpsimd(mk("gpsimd"))
            block.sync(mk("sync"))


class Builder:
    def __init__(self, nc, st, n_own=NOWN, debug=False):
        self.nc = nc
        self.st = st
        self.P = Prog(nc, st)
        self.n_own = n_own
        self.debug = debug
        self.outs = []
        self.pool = None
        self.bump = 0

    POOL_BYTES = 212800

    def sb(self, name, shape, dt):
        if self.pool is None:
            self.pool = self.st.enter_context(self.nc.sbuf_tensor("sb_pool", [128, self.POOL_BYTES // 2], BF16))
            self.bump = 0
        esz = mybir.dt.size(dt)
        n = 1
        for d_ in shape[1:]:
            n *= d_
        nbytes = (n * esz + 63) // 64 * 64
        off = self.bump
        self.bump += nbytes
        assert self.bump <= self.POOL_BYTES, "SBUF pool overflow at %s: %d" % (name, self.bump)
        v = self.pool[0:shape[0], off // 2: off // 2 + (n * esz) // 2]
        if dt != BF16:
            v = v.bitcast(dt)
        if len(shape) == 3:
            v = v.rearrange("p (a b) -> p a b", a=shape[1])
        elif len(shape) == 4:
            v = v.rearrange("p (a b c) -> p a b c", a=shape[1], b=shape[2])
        return v

    def phase_mark(self):
        return self.bump

    def phase_reset(self, mark):
        self.P.barrier()
        self.bump = mark

    def dram_in(self, name, shape, dt):
        return self.nc.dram_tensor(name, shape, dt, kind="ExternalInput").ap()

    def dram_out(self, name, shape, dt):
        t = self.nc.dram_tensor(name, shape, dt, kind="ExternalOutput").ap()
        return t

    def dram_tmp(self, name, shape, dt):
        return self.nc.dram_tensor(name, shape, dt, kind="Internal").ap()

    def bc_reg(self, e, val):
        if getattr(self, "_bcreg", None) is None:
            self._bcreg = e.to_reg(val)
        return self._bcreg

    def cast_rr(self, out, in_, r, w):
        k = getattr(self, "_rr", 0)
        self._rr = k + 1
        if k % 3 == 0:
            self.A(lambda e: e.activation(out=out, in_=in_, func=AF.Copy), r, w)
        elif k % 3 == 1:
            self.V(lambda e: e.tensor_copy(out=out, in_=in_), r, w)
        else:
            self.G(lambda e: e.tensor_copy(out=out, in_=in_), r, w)

    def V(self, fn, r=(), w=()):
        return self.P.op("vector", fn, r, w)

    def A(self, fn, r=(), w=()):
        return self.P.op("scalar", fn, r, w)

    def G(self, fn, r=(), w=()):
        return self.P.op("gpsimd", fn, r, w)

    def T(self, fn, r=(), w=()):
        return self.P.op("tensor", fn, r, w)

    def dma(self, out, in_, r, w, dma_buf, eng="sync"):
        return self.P.op(eng, lambda e: e.dma_start(out=out, in_=in_), r, w, dma_out=dma_buf)

    def setup_common(self):
        nc = self.nc
        self.bank = []
        self.bbank = []
        for k in range(8):
            t = self.st.enter_context(nc.psum_tensor("bank%d" % k, [128, 512], F32))
            self.bank.append(t)
            self.bbank.append(Buf("bank%d" % k, excl=True))
        self.idsrc = self.sb("idsrc", [128, 128], F32)
        self.identf = self.sb("identf", [128, 128], F32)
        self.identb = self.sb("identb", [128, 128], BF16)
        self.onesf = self.sb("onesf", [128, 128], F32)
        self.onesb = self.sb("onesb", [128, 128], BF16)
        self.b_const = Buf("const")
        bc = self.b_const
        self.G(lambda e: e.iota(self.idsrc[:], pattern=[[1, 128]], base=0, channel_multiplier=-1,
                                allow_small_or_imprecise_dtypes=True), w=[bc])
        self.V(lambda e: e.tensor_scalar(out=self.identf[:], in0=self.idsrc[:], scalar1=0.0, scalar2=None,
                                         op0=ALU.is_equal), r=[bc], w=[bc])
        self.V(lambda e: e.tensor_copy(out=self.identb[:], in_=self.identf[:]), r=[bc], w=[bc])
        self.V(lambda e: e.memset(self.onesf[:], 1.0), w=[bc])
        self.V(lambda e: e.memset(self.onesb[:], 1.0), w=[bc])
        NSLOT = 64 * self.CAP
        zeros_d = self.dram_in("zeros_d", [1024, D], BF16)
        self.xe_d = self.dram_tmp("xe_d", [NSLOT, D], BF16)
        self.b_xed = Buf("xed")
        for r0 in range(0, NSLOT, 1024):
            self.P.op("gpsimd", lambda e, r0=r0: e.dma_start(out=self.xe_d[r0:r0 + 1024, :], in_=zeros_d[:, :]), [], [self.b_xed], dma_out=self.b_xed)

    def bview(self, k, dt=BF16):
        return self.bank[k][:].bitcast(dt)

    def rope_table(self, pos_i32, nblk, tab, scr, b_scr, b_tab, ropec, b_in):
        n = nblk * 24
        ang = scr[:, 0:n]
        kf = scr[:, n:2 * n]
        mm = scr[:, 2 * n:3 * n]
        ki = scr[:, 3 * n:4 * n].bitcast(I32)
        posf = scr[:, 4 * n:4 * n + nblk]
        inv = ropec[:, 0:24]
        off = ropec[:, 24:48]
        ang3 = ang.rearrange("p (b j) -> p b j", j=24)
        V = self.V
        V(lambda e: e.tensor_copy(out=posf, in_=pos_i32), r=[b_in], w=[b_scr])
        V(lambda e: e.tensor_tensor(out=ang3, in0=posf.unsqueeze(2).to_broadcast([128, nblk, 24]),
                                    in1=inv.unsqueeze(1).to_broadcast([128, nblk, 24]), op=ALU.mult), r=[b_in], w=[b_scr])
        V(lambda e: e.tensor_tensor(out=ang3, in0=ang3, in1=off.unsqueeze(1).to_broadcast([128, nblk, 24]),
                                    op=ALU.add), r=[b_in], w=[b_scr])
        V(lambda e: e.tensor_scalar(out=ki, in0=ang, scalar1=1.0 / TWO_PI, scalar2=None, op0=ALU.mult), w=[b_scr])
        V(lambda e: e.tensor_copy(out=kf, in_=ki), w=[b_scr])
        V(lambda e: e.scalar_tensor_tensor(out=ang, in0=kf, scalar=-CW1, in1=ang, op0=ALU.mult, op1=ALU.add), w=[b_scr])
        V(lambda e: e.scalar_tensor_tensor(out=ang, in0=kf, scalar=-CW2, in1=ang, op0=ALU.mult, op1=ALU.add), w=[b_scr])
        V(lambda e: e.tensor_scalar(out=mm, in0=ang, scalar1=math.pi, scalar2=-TWO_PI, op0=ALU.is_gt, op1=ALU.mult), w=[b_scr])
        V(lambda e: e.tensor_tensor(out=ang, in0=ang, in1=mm, op=ALU.add), w=[b_scr])
        V(lambda e: e.tensor_scalar(out=mm, in0=ang, scalar1=-math.pi, scalar2=TWO_PI, op0=ALU.is_lt, op1=ALU.mult), w=[b_scr])
        V(lambda e: e.tensor_tensor(out=ang, in0=ang, in1=mm, op=ALU.add), w=[b_scr])
        V(lambda e: e.tensor_scalar(out=ang, in0=ang, scalar1=3.14159, scalar2=-3.14159, op0=ALU.min, op1=ALU.max), w=[b_scr])
        self.A(lambda e: e.activation(out=tab[:].rearrange("p b j -> p (b j)"), in_=ang, func=AF.Sin), r=[b_scr], w=[b_tab])

    def rope(self, o1, o2, x1, x2, cos, sin, tA, tB, r, w, b_tmp):
        V = self.V
        V(lambda e: e.tensor_tensor(out=tA, in0=x1, in1=cos, op=ALU.mult), r=r, w=[b_tmp])
        V(lambda e: e.tensor_tensor(out=tB, in0=x2, in1=sin, op=ALU.mult), r=r, w=[b_tmp])
        V(lambda e: e.tensor_tensor(out=o1, in0=tA, in1=tB, op=ALU.subtract), r=[b_tmp], w=w)
        V(lambda e: e.tensor_tensor(out=tA, in0=x2, in1=cos, op=ALU.mult), r=r, w=[b_tmp])
        V(lambda e: e.tensor_tensor(out=tB, in0=x1, in1=sin, op=ALU.mult), r=r, w=[b_tmp])
        V(lambda e: e.tensor_tensor(out=o2, in0=tA, in1=tB, op=ALU.add), r=[b_tmp], w=w)

    def load_weight_bf16(self, dst, src_ap, ncols, b_dst, stage, b_stage, chunk=512):
        srcv = src_ap.rearrange("(c p) n -> p c n", p=128)
        k = 0
        for c0 in range(0, ncols, chunk):
            cw = min(chunk, ncols - c0)
            sidx = k % len(stage)
            k += 1
            stg = stage[sidx]
            self.dma(stg[:, :, 0:cw], srcv[:, :, c0:c0 + cw], [], [b_stage[sidx]], b_stage[sidx])
            self.cast_rr(dst[:, :, c0:c0 + cw], stg[:, :, 0:cw], [b_stage[sidx]], [b_dst])

    def set_x_sequence(self, seq):
        self.xseq = list(seq)
        self.xl_k = 0
        self.x_issued = 0

    def _issue_x(self, k):
        s = k % 2
        self.dma(self.xf[s][:], self.xseq[k], [], [self.b_xf[s]], self.b_xf[s])

    def load_xT(self, src_rows=None):
        k = self.xl_k
        self.xl_k += 1
        s = k % 2
        if self.x_issued <= k:
            self._issue_x(k)
            self.x_issued = k + 1
        if k + 1 < len(self.xseq) and self.x_issued <= k + 1:
            self._issue_x(k + 1)
            self.x_issued = k + 2
        xf, bxf = self.xf[s], self.b_xf[s]
        xT, bxT = self.xT[s], self.b_xT[s]
        for half, bk in ((0, 0), (1, 2)):
            for c4 in range(4):
                c = half * 4 + c4
                self.T(lambda e, c=c, c4=c4, bk=bk: e.transpose(out=self.bank[bk][:, c4 * 128:(c4 + 1) * 128], in_=xf[:, c * 128:(c + 1) * 128],
                                                              identity=self.identf[:]), r=[bxf, self.b_const], w=[self.bbank[bk]])
        self.A(lambda e: e.activation(out=xT[:, 0:4, :], in_=self.bank[0][:].rearrange("p (c t) -> p c t", c=4), func=AF.Copy),
               r=[self.bbank[0]], w=[bxT])
        self.V(lambda e: e.tensor_copy(out=xT[:, 4:8, :], in_=self.bank[2][:].rearrange("p (c t) -> p c t", c=4)),
               r=[self.bbank[2]], w=[bxT])
        self.last_s = s
        return xT, bxT

    def phase1a(self):
        nc, P = self.nc, self.P
        V, A, G, T = self.V, self.A, self.G, self.T
        n_own = self.n_own
        x_all = self.dram_in("x_all", [S, D], F32)
        x_own = self.dram_in("x_own", [NOWN * 128, D], F32)
        pos_all = self.dram_in("pos_all_t", [128, NBLK], I32)
        pos_own = self.dram_in("pos_own_t", [128, NOWN], I32)
        par_d = self.dram_in("par", [128, 1], F32)
        ropec_d = self.dram_in("rope_c", [128, 48], F32)
        wk_d = self.dram_in("wk_dsa", [D, 304], F32)
        wq_d = self.dram_in("wq_dsa", [D, 776], F32)
        wuk_d = self.dram_in("wuk_t", [48, 8, 256], F32)
        wuv_d = self.dram_in("wuv", [256, 512], F32)
        gkv_d = self.dram_in("g_kv", [1, 256], F32)
        ckv_d = self.dram_out("ckv_d", [S, 256], BF16) if self.debug else self.dram_tmp("ckv_d", [S, 256], BF16)
        if self.debug:
            self.aout_d = self.dram_out("aout_d", [NOWN * 128, 512], BF16)
        else:
            self.aout_d = self.dram_tmp("aout_d", [NOWN * 128, 512], BF16)
        _ckvd8 = [Buf("ckvd%d" % k) for k in range(8)]
        b_ckvd = [_ckvd8[k % 8] for k in range(NBLK)]
        self.b_aoutd = Buf("aoutd")

        sb = self.sb
        par = sb("par", [128, 1], F32)
        cpar = sb("cpar", [128, 4], F32)
        ropec = sb("ropec", [128, 48], F32)
        posa = sb("posa", [128, NBLK], I32)
        poso = sb("poso", [128, NOWN], I32)
        b_small = Buf("smallin")
        TABA = sb("TABA", [128, NBLK, 24], F32)
        TABO = sb("TABO", [128, NOWN, 24], F32)
        b_taba, b_tabo = Buf("taba"), Buf("tabo")
        bm8 = sb("bm8", [128, 8], BF16)
        tmpv = sb("tmpv", [128, 16], F32)
        self.SLOT8 = sb("SLOT8", [128, NOWN, 8], I32)
        self.W8 = sb("W8", [128, NOWN, 8], F32)
        self.b_route = Buf("route")
        self.par, self.cpar, self.TABA, self.TABO = par, cpar, TABA, TABO
        self.b_small, self.b_taba, self.b_tabo = b_small, b_taba, b_tabo
        self.x_all, self.x_own = x_all, x_own
        mark = self.phase_mark()
        CKVT = sb("CKVT", [128, 2, S], BF16)
        KRT = sb("KRT", [128, S], BF16)
        IKT = sb("IKT", [128, S], BF16)
        b_kside = [Buf("kside%d" % k) for k in range(NBLK)]
        SCORE = sb("SCORE", [128, S], F32)
        b_score = [Buf("score%d" % k) for k in range(16)]
        b_scoreall = b_score
        MT = sb("MT", [128, NBLK, 128], BF16)
        b_mt = Buf("MT")
        junk = sb("junk", [128, 1024], BF16)
        b_junk = Buf("junk")
        self.alloc_xload()
        xa = lambda kb: x_all[kb * 128:(kb + 1) * 128, :]
        xo = lambda i_: x_own[i_ * 128:(i_ + 1) * 128, :]
        nkb_tot = 2 * n_own
        seq = [xa(0), xa(1)]
        for i_ in range(n_own):
            seq.append(xo(i_))
            if 2 * i_ + 2 < nkb_tot:
                seq += [xa(2 * i_ + 2), xa(2 * i_ + 3)]
        self.set_x_sequence(seq)
        wk = sb("wk", [128, 8, 304], BF16)
        wq = sb("wq", [128, 8, 776], BF16)
        wuk = sb("wuk", [48, 8, 256], BF16)
        wuv = sb("wuv", [128, 2, 512], BF16)
        gkv = sb("gkv", [128, 256], F32)
        b_w = Buf("weights")

        self.dma(par[:], par_d[:, :], [], [b_small], b_small)
        self.dma(ropec[:], ropec_d[:, :], [], [b_small], b_small)
        self.dma(posa[:], pos_all[:, :], [], [b_small], b_small)
        self.dma(poso[:], pos_own[:, :], [], [b_small], b_small)
        self.dma(gkv[:], gkv_d.partition_broadcast(128).rearrange("p o r -> p (o r)"), [], [b_small], b_small)
        b_setup = Buf("setup")
        stg = [SCORE[:, k * 4096:(k + 1) * 4096].rearrange("p (c n) -> p c n", c=8) for k in range(2)]
        b_stg = [b_setup, b_setup]
        self.load_weight_bf16(wk, wk_d, 304, b_w, stg, b_stg)
        self.load_weight_bf16(wq, wq_d, 776, b_w, stg, b_stg)
        s0 = SCORE[0:48, 0:2048].rearrange("p (h r) -> p h r", h=8)
        self.dma(s0, wuk_d[:, :, :], [], [b_setup], b_setup)
        G(lambda e: e.tensor_copy(out=wuk[:], in_=s0), r=[b_setup], w=[b_w])
        s1 = SCORE[:, 4096:5120].rearrange("p (c n) -> p c n", c=2)
        self.dma(s1, wuv_d.rearrange("(c p) n -> p c n", p=128), [], [b_setup], b_setup)
        G(lambda e: e.tensor_copy(out=wuv[:], in_=s1), r=[b_setup], w=[b_w])
        V(lambda e: e.tensor_scalar(out=cpar[:, 0:1], in0=par[:], scalar1=-128.0, scalar2=None, op0=ALU.mult), r=[b_small], w=[b_small])
        V(lambda e: e.tensor_scalar(out=cpar[:, 1:2], in0=par[:], scalar1=-128.0, scalar2=128.0, op0=ALU.mult, op1=ALU.add), r=[b_small], w=[b_small])
        V(lambda e: e.tensor_scalar(out=cpar[:, 2:3], in0=par[:], scalar1=128.0, scalar2=None, op0=ALU.mult), r=[b_small], w=[b_small])
        G(lambda e: e.iota(tmpv[:, 0:8], pattern=[[-16, 8]], base=0, channel_multiplier=1, allow_small_or_imprecise_dtypes=True), w=[b_small])
        V(lambda e: e.tensor_scalar(out=tmpv[:, 8:16], in0=tmpv[:, 0:8], scalar1=0.0, scalar2=0.125, op0=ALU.is_ge, op1=ALU.mult), r=[b_small], w=[b_small])
        V(lambda e: e.tensor_scalar(out=tmpv[:, 0:8], in0=tmpv[:, 0:8], scalar1=15.0, scalar2=None, op0=ALU.is_le), r=[b_small], w=[b_small])
        V(lambda e: e.tensor_tensor(out=bm8[:], in0=tmpv[:, 0:8], in1=tmpv[:, 8:16], op=ALU.mult), r=[b_small], w=[b_small])
        scr = SCORE[:, 0:8192]
        self.rope_table(posa[:], NBLK, TABA, scr, b_setup, b_taba, ropec, b_small)
        self.rope_table(poso[:], NOWN, TABO, scr, b_setup, b_tabo, ropec, b_small)
        tok = V(lambda e: e.memset(SCORE[:, 0:2], 0.0), w=[b_setup])
        for k in range(16):
            b_score[k].w = tok

        ss = sb("ss", [128, 4], F32)
        b_ss = Buf("ss")
        ckvn = [sb("ckvn%d" % k, [128, 256], BF16) for k in range(2)]
        b_ckvn = [Buf("ckvn%d" % k) for k in range(2)]
        rtmp = sb("rtmp", [128, 2, 64], F32)
        b_rtmp = Buf("rtmp")
        krr = sb("krr", [128, 16], F32)
        krrep = sb("krrep", [128, 8, 16], BF16)
        ikr = sb("ikr", [128, 32], BF16)
        b_kr = Buf("kr")

        def kside(kb):
            xT, bxT = self.load_xT(x_all[kb * 128:(kb + 1) * 128, :])
            pk = self.bank[1]
            bpk = self.bbank[1]
            yield
            for c in range(8):
                T(lambda e, c=c: e.matmul(pk[:, 0:304], lhsT=xT[:, c, :], rhs=wk[:, c, :], start=(c == 0), stop=(c == 7)),
                  r=[bxT, b_w], w=[bpk])
            s = kb % 2
            A(lambda e: e.activation(out=junkA[:, 0:256], in_=pk[:, 0:256], func=AF.Square, accum_out=ss[:, 0:1]), r=[bpk], w=[b_junkA, b_ss])
            V(lambda e: e.tensor_scalar(out=ss[:, 1:2], in0=ss[:, 0:1], scalar1=1.0 / 256.0, scalar2=1e-6, op0=ALU.mult, op1=ALU.add), r=[b_ss], w=[b_ss])
            A(lambda e: e.sqrt(out=ss[:, 2:3], in_=ss[:, 1:2]), r=[b_ss], w=[b_ss])
            V(lambda e: e.reciprocal(out=ss[:, 3:4], in_=ss[:, 2:3]), r=[b_ss], w=[b_ss])
            V(lambda e: e.scalar_tensor_tensor(out=ckvn[s][:], in0=pk[:, 0:256], scalar=ss[:, 3:4], in1=gkv[:], op0=ALU.mult, op1=ALU.mult),
              r=[bpk, b_ss, b_small], w=[b_ckvn[s]])
            self.dma(ckv_d[kb * 128:(kb + 1) * 128, :], ckvn[s][:], [b_ckvn[s]], [b_ckvd[kb]], b_ckvd[kb], eng="gpsimd")
            cosA = TABA[:, kb, 0:8]
            sinA = TABA[:, kb, 8:16]
            cosI = TABA[:, kb, 16:20]
            sinI = TABA[:, kb, 20:24]
            self.rope(krr[:, 0:8], krr[:, 8:16], pk[:, 256:264], pk[:, 264:272], cosA, sinA, rtmp[:, 0, 0:8], rtmp[:, 1, 0:8],
                      [bpk, b_taba], [b_kr], b_rtmp)
            V(lambda e: e.tensor_copy(out=krrep[:], in_=krr[:].unsqueeze(1).to_broadcast([128, 8, 16])), r=[b_kr], w=[b_kr])
            self.rope(ikr[:, 0:4], ikr[:, 4:8], pk[:, 272:276], pk[:, 276:280], cosI, sinI, rtmp[:, 0, 0:4], rtmp[:, 1, 0:4],
                      [bpk, b_taba], [b_kr], b_rtmp)
            V(lambda e: e.tensor_copy(out=ikr[:, 8:32], in_=pk[:, 280:304]), r=[bpk], w=[b_kr])
            bv = self.bview(2)
            bb2 = self.bbank[2]
            yield
            for c in range(2):
                T(lambda e, c=c: e.transpose(out=bv[:, c * 128:(c + 1) * 128], in_=ckvn[s][:, c * 128:(c + 1) * 128], identity=self.identb[:]),
                  r=[b_ckvn[s], self.b_const], w=[bb2])
            yield
            T(lambda e: e.transpose(out=bv[:, 256:384], in_=krrep[:].rearrange("p h d -> p (h d)"), identity=self.identb[:]),
              r=[b_kr, self.b_const], w=[bb2])
            T(lambda e: e.transpose(out=bv[0:32, 384:512], in_=ikr[:], identity=self.identb[:]), r=[b_kr, self.b_const], w=[bb2])
            ksl = slice(kb * 128, (kb + 1) * 128)
            A(lambda e: e.activation(out=CKVT[:, :, ksl], in_=bv[:, 0:256].rearrange("p (c t) -> p c t", c=2), func=AF.Copy), r=[bb2], w=[b_kside[kb]])
            V(lambda e: e.tensor_copy(out=KRT[:, ksl], in_=bv[:, 256:384]), r=[bb2], w=[b_kside[kb]])
            V(lambda e: e.tensor_copy(out=IKT[0:32, ksl], in_=bv[0:32, 384:512]), r=[bb2], w=[b_kside[kb]])

        aqn = sb("aqn", [128, 8, 48], BF16)
        aqrp = sb("aqrp", [128, 8, 16], BF16)
        b_aq = Buf("aq")
        aqnT = sb("aqnT", [48, 8, 128], BF16)
        b_aqnT = Buf("aqnT")
        Qm2 = [sb("Qm%d" % k, [128, 8, 128], BF16) for k in range(2)]
        b_qm2 = [Buf("Qm%d" % k) for k in range(2)]
        QLT2 = [sb("QLT%d" % k, [128, 2, 8, 128], BF16) for k in range(2)]
        b_qlt2 = [Buf("QLT%d" % k) for k in range(2)]
        iqr = sb("iqr", [128, 8, 32], BF16)
        b_iq = Buf("iq")
        IQT = sb("IQT", [128, 8, 128], BF16)
        b_iqt = Buf("IQT")
        b_zpad = Buf("zpad")
        G(lambda e: e.memset(IQT[:], 0.0), w=[b_iqt, b_zpad])
        for q4 in range(4):
            G(lambda e, q4=q4: e.memset(IKT[:, q4 * 2048:(q4 + 1) * 2048], 0.0), w=[b_zpad] + [b_kside[k] for k in range(q4 * 16, (q4 + 1) * 16)])
        tbf = [sb("tbf%d" % k, [128, 512], BF16) for k in range(3)]
        b_tbf = [Buf("tbf%d" % k) for k in range(3)]
        wq16 = sb("wq16", [128, 8], F32)
        DGW = sb("DGW", [128, 8, 128], BF16)
        b_dgw = Buf("DGW")
        negi = sb("negi", [128, 128], BF16)
        V(lambda e: e.tensor_scalar(out=negi[:], in0=self.identf[:], scalar1=-30000.0, scalar2=None, op0=ALU.mult), r=[self.b_const], w=[self.b_const])
        junkA = sb("junkA", [128, 1024], BF16)
        b_junkA = Buf("junkA")
        bis2 = sb("bis2", [128, 8], F32)
        b_bis2 = Buf("bis2")
        bis = sb("bis", [128, 8], F32)
        b_bis = Buf("bis")
        DG = sb("DG", [128, 128], F32)
        THRB = sb("THRB", [128, 128], F32)
        b_thr = Buf("thr")
        mk1, b_mk1 = DG, b_thr
        PT = [sb("PT%d" % k, [128, 512], BF16) for k in range(3)]
        b_pt = [Buf("PT%d" % k) for k in range(3)]
        CKVs = [sb("CKVs%d" % k, [128, 8, 256], BF16) for k in range(2)]
        b_ckvs = [Buf("CKVs%d" % k) for k in range(2)]
        OT = sb("OT", [128, 2, 512], BF16)
        b_ot = Buf("OT")
        denr = sb("denr", [1, 512], F32)
        b_denr = Buf("denr")
        rden = sb("rden", [128, 4], F32)
        b_rden = Buf("rden")
        AOUT = [sb("AOUT0", [128, 512], BF16)] * 2
        b_aout = [Buf("AOUT0")] * 2
        self.cnt_ckvs = 0
        self.cnt_pt = 0
        self.cnt_tb = 0

        def qside(i):
            xT, bxT = self.load_xT(x_own[i * 128:(i + 1) * 128, :])
            Qm, b_qm, QLT, b_qlt = Qm2[i % 2], b_qm2[i % 2], QLT2[i % 2], b_qlt2[i % 2]
            p1, bp1 = self.bank[1], self.bbank[1]
            p3, bp3 = self.bank[0], self.bbank[0]
            yield
            for c in range(8):
                T(lambda e, c=c: e.matmul(p1[:, 0:512], lhsT=xT[:, c, :], rhs=wq[:, c, 0:512], start=(c == 0), stop=(c == 7)),
                  r=[bxT, b_w], w=[bp1])
            for c in range(8):
                T(lambda e, c=c: e.matmul(p3[:, 0:264], lhsT=xT[:, c, :], rhs=wq[:, c, 512:776], start=(c == 0), stop=(c == 7)),
                  r=[bxT, b_w], w=[bp3])
            qs = ""
            p1v = p1[:, 0:512].rearrange("p (h d) -> p h d", h=8)
            cosA = TABO[:, i, 0:8].unsqueeze(1).to_broadcast([128, 8, 8])
            sinA = TABO[:, i, 8:16].unsqueeze(1).to_broadcast([128, 8, 8])
            cosI = TABO[:, i, 16:20].unsqueeze(1).to_broadcast([128, 8, 4])
            sinI = TABO[:, i, 20:24].unsqueeze(1).to_broadcast([128, 8, 4])
            rt0 = rtmp[:, 0, 0:64].rearrange("p (h d) -> p h d", h=8)
            rt1 = rtmp[:, 1, 0:64].rearrange("p (h d) -> p h d", h=8)
            if qs != "q0b_norope" and qs != "q0b_none":
                self.rope(aqrp[:, :, 0:8], aqrp[:, :, 8:16], p1v[:, :, 0:8], p1v[:, :, 8:16], cosA, sinA, rt0, rt1, [bp1, b_tabo], [b_aq], b_rtmp)
            if qs != "q0b_nocopy" and qs != "q0b_none":
                A(lambda e: e.activation(out=aqn[:], in_=p1v[:, :, 16:64], func=AF.Copy), r=[bp1], w=[b_aq])
            if qs.startswith("q0b_"):
                return
            if qs == "q0b":
                return
            p3v = p3[:, 0:256].rearrange("p (h d) -> p h d", h=8)
            rt0i = rtmp[:, 0, 0:32].rearrange("p (h d) -> p h d", h=8)
            rt1i = rtmp[:, 1, 0:32].rearrange("p (h d) -> p h d", h=8)
            A(lambda e: e.activation(out=iqr[:, :, 8:32], in_=p3v[:, :, 8:32], func=AF.Copy), r=[bp3], w=[b_iq])
            self.rope(iqr[:, :, 0:4], iqr[:, :, 4:8], p3v[:, :, 0:4], p3v[:, :, 4:8], cosI, sinI, rt0i, rt1i, [bp3, b_tabo], [b_iq], b_rtmp)
            if qs == "q0c":
                return
            V(lambda e: e.tensor_scalar(out=wq16[:], in0=p3[:, 256:264], scalar1=1.0 / 16.0, scalar2=None, op0=ALU.mult), r=[bp3], w=[b_dgw])
            V(lambda e: e.tensor_tensor(out=DGW[:], in0=self.identf[:].unsqueeze(1).to_broadcast([128, 8, 128]),
                                        in1=wq16[:].unsqueeze(2).to_broadcast([128, 8, 128]), op=ALU.mult), r=[self.b_const], w=[b_dgw])
            if qs == "q1":
                return
            bv = self.bview(2)
            bb2 = self.bbank[2]
            yield
            for h in range(8):
                T(lambda e, h=h: e.transpose(out=bv[0:48, h * 128:(h + 1) * 128], in_=aqn[:, h, :], identity=self.identb[:]),
                  r=[b_aq, self.b_const], w=[bb2])
            A(lambda e: e.activation(out=aqnT[:], in_=bv[0:48, :].rearrange("p (h t) -> p h t", h=8), func=AF.Copy), r=[bb2], w=[b_aqnT])
            if qs == "q2":
                return
            yield
            T(lambda e: e.transpose(out=bv[:, 0:128], in_=aqrp[:].rearrange("p h d -> p (h d)"), identity=self.identb[:]),
              r=[b_aq, self.b_const, b_aqnT], w=[bb2])
            V(lambda e: e.tensor_tensor(out=Qm[:], in0=bv[:, 0:128].unsqueeze(1).to_broadcast([128, 8, 128]),
                                        in1=bm8[:].unsqueeze(2).to_broadcast([128, 8, 128]), op=ALU.mult),
              r=[bb2, b_small], w=[b_qm])
            if qs == "q3":
                return
            for h in range(8):
                T(lambda e, h=h: e.transpose(out=bv[0:32, h * 128:(h + 1) * 128], in_=iqr[:, h, :], identity=self.identb[:]),
                  r=[b_iq, self.b_const, b_qm], w=[bb2])
            A(lambda e: e.activation(out=IQT[0:32, :, :], in_=bv[0:32, :].rearrange("p (h t) -> p h t", h=8), func=AF.Copy), r=[bb2], w=[b_iqt])
            if qs == "q4":
                return
            yield
            for c in range(2):
                for hg in range(2):
                    bk = hg
                    for hh in range(4):
                        h = hg * 4 + hh
                        T(lambda e, c=c, h=h, hh=hh, bk=bk: e.matmul(self.bank[bk][:, hh * 128:(hh + 1) * 128],
                                                                     lhsT=wuk[:, h, c * 128:(c + 1) * 128], rhs=aqnT[:, h, :],
                                                                     start=True, stop=True),
                          r=[b_w, b_aqnT], w=[self.bbank[bk]])
                    A(lambda e, c=c, hg=hg, bk=bk: e.activation(out=QLT[:, c, hg * 4:(hg + 1) * 4, :],
                                                                in_=self.bank[bk][:].rearrange("p (h t) -> p h t", h=4),
                                                                func=AF.Copy, scale=0.125),
                      r=[self.bbank[bk]], w=[b_qlt])

        def score_and_threshold(i):
            nkb = 2 * i + 2
            nk = nkb * 128
            nch = (nk + 511) // 512
            for j in range(nch):
                wj = min(512, nk - 512 * j)
                sc = SCORE[:, 512 * j:512 * j + wj]
                abk = 2
                pacc, bpacc = self.bank[abk], self.bbank[abk]
                kbufs = [b_kside[k] for k in range(4 * j, 4 * j + wj // 128)]
                tt = {}

                def s_stage(h):
                    bk = 1 if (h % 2 == 0) else 0
                    pb, bpb = self.bank[bk], self.bbank[bk]
                    T(lambda e, pb=pb, h=h, wj=wj, j=j: e.matmul(pb[:, 0:wj], lhsT=IQT[:, h, :], rhs=IKT[:, 512 * j:512 * j + wj], start=True, stop=True),
                      r=[b_iqt] + kbufs, w=[bpb])
                    t = self.cnt_tb % 3
                    self.cnt_tb += 1
                    tt[h] = t
                    if h % 2 == 0:
                        A(lambda e, pb=pb, t=t, wj=wj: e.activation(out=tbf[t][:, 0:wj], in_=pb[:, 0:wj], func=AF.Relu), r=[bpb], w=[b_tbf[t]])
                    else:
                        V(lambda e, pb=pb, t=t, wj=wj: e.tensor_scalar(out=tbf[t][:, 0:wj], in0=pb[:, 0:wj], scalar1=0.0, scalar2=None, op0=ALU.max),
                          r=[bpb], w=[b_tbf[t]])

                def a_stage(h):
                    t = tt[h]
                    T(lambda e, h=h, t=t, wj=wj, pacc=pacc: e.matmul(pacc[:, 0:wj], lhsT=DGW[:, h, :], rhs=tbf[t][:, 0:wj], start=(h == 0), stop=(h == 7)),
                      r=[b_tbf[t], b_dgw], w=[bpacc])

                s_stage(0)
                for h in range(8):
                    if h + 1 < 8:
                        s_stage(h + 1)
                    a_stage(h)
                V(lambda e, sc=sc, pacc=pacc, wj=wj: e.tensor_copy(out=sc, in_=pacc[:, 0:wj]), r=[bpacc], w=[b_score[j]])
                yield
            allsc = [b_score[j] for j in range(nch)]
            V(lambda e: e.tensor_reduce(out=bis[:, 0:1], in_=SCORE[:, 0:nk], axis=AX.X, op=ALU.min), r=allsc, w=[b_bis])
            for t_, kb in enumerate((2 * i, 2 * i + 1)):
                blk = SCORE[:, kb * 128:(kb + 1) * 128]
                j = kb // 4
                V(lambda e, t_=t_: e.tensor_scalar(out=mk1[:], in0=self.idsrc[:], scalar1=cpar[:, t_:t_ + 1], scalar2=0.0, op0=ALU.add, op1=ALU.is_gt),
                  r=[self.b_const, b_small], w=[b_mk1])
                V(lambda e, blk=blk: e.scalar_tensor_tensor(out=blk, in0=mk1[:], scalar=NEG, in1=blk, op0=ALU.mult, op1=ALU.add),
                  r=[b_mk1], w=[b_score[j]])
            V(lambda e: e.tensor_reduce(out=bis[:, 1:2], in_=SCORE[:, 0:nk], axis=AX.X, op=ALU.max), r=allsc, w=[b_bis])
            V(lambda e: e.tensor_tensor(out=bis[:, 2:3], in0=bis[:, 1:2], in1=bis[:, 0:1], op=ALU.subtract), r=[b_bis], w=[b_bis])
            split = nk
            n2 = nk - split
            na = (n2 + 1023) // 1024
            for it in range(1, N_BISECT + 1):
                f = 2.0 ** (-it)
                V(lambda e, f=f: e.tensor_scalar(out=bis[:, 3:4], in0=bis[:, 2:3], scalar1=f, scalar2=bis[:, 0:1], op0=ALU.mult, op1=ALU.add),
                  r=[b_bis], w=[b_bis])
                if n2 > 0:
                    V(lambda e: e.tensor_scalar(out=bis[:, 6:7], in0=bis[:, 3:4], scalar1=-1.0, scalar2=None, op0=ALU.mult), r=[b_bis], w=[b_bis])
                    for ci in range(na):
                        c0 = split + ci * 1024
                        cw = min(1024, nk - c0)
                        A(lambda e, c0=c0, cw=cw, ci=ci: e.activation(out=junkA[:, 0:cw], in_=SCORE[:, c0:c0 + cw], func=AF.Sign, bias=bis[:, 6:7],
                                                                      accum_out=bis2[:, ci:ci + 1]), r=allsc + [b_bis], w=[b_junkA, b_bis2])
                first = True
                for c0 in range(0, split, 1024):
                    cw = min(1024, split - c0)
                    if first:
                        V(lambda e, c0=c0, cw=cw: e.tensor_scalar(out=junk[:, 0:cw], in0=SCORE[:, c0:c0 + cw], scalar1=bis[:, 3:4], scalar2=None,
                                                                  op0=ALU.is_ge, op1=ALU.add, accum_out=bis[:, 4:5]),
                          r=allsc + [b_bis], w=[b_junk, b_bis])
                    else:
                        V(lambda e, c0=c0, cw=cw: e.tensor_scalar(out=junk[:, 0:cw], in0=SCORE[:, c0:c0 + cw], scalar1=bis[:, 3:4], scalar2=bis[:, 4:5],
                                                                  op0=ALU.is_ge, op1=ALU.add, accum_out=bis[:, 4:5]),
                          r=allsc + [b_bis], w=[b_junk, b_bis])
                    first = False
                thr_cnt = 255.5
                if n2 > 0:
                    V(lambda e: e.tensor_reduce(out=bis[:, 7:8], in_=bis2[:, 0:na], axis=AX.X, op=ALU.add), r=[b_bis2], w=[b_bis])
                    V(lambda e: e.scalar_tensor_tensor(out=bis[:, 4:5], in0=bis[:, 7:8], scalar=0.5, in1=bis[:, 4:5], op0=ALU.mult, op1=ALU.add),
                      r=[b_bis], w=[b_bis])
                    thr_cnt = 255.5 - 0.5 * n2
                V(lambda e, f=f, thr_cnt=thr_cnt: e.tensor_scalar(out=bis[:, 5:6], in0=bis[:, 4:5], scalar1=thr_cnt, scalar2=f, op0=ALU.is_ge, op1=ALU.mult),
                  r=[b_bis], w=[b_bis])
                V(lambda e: e.scalar_tensor_tensor(out=bis[:, 0:1], in0=bis[:, 5:6], scalar=bis[:, 2:3], in1=bis[:, 0:1], op0=ALU.mult, op1=ALU.add),
                  r=[b_bis], w=[b_bis])
                yield

        def masks(i):
            nkb = 2 * i + 2
            V(lambda e: e.tensor_scalar(out=DG[:], in0=self.identf[:], scalar1=bis[:, 0:1], scalar2=None, op0=ALU.mult), r=[b_bis, self.b_const], w=[b_thr])
            T(lambda e: e.matmul(self.bank[2][:, 0:128], lhsT=self.onesf[:], rhs=DG[:], start=True, stop=True), r=[b_thr, self.b_const], w=[self.bbank[2]])
            A(lambda e: e.activation(out=THRB[:], in_=self.bank[2][:, 0:128], func=AF.Copy), r=[self.bbank[2]], w=[b_thr])
            g = 0
            for kb0 in range(0, nkb, 4):
                n = min(4, nkb - kb0)
                bk = 1 if (g % 2 == 0) else 0
                g += 1
                for t_ in range(n):
                    kb = kb0 + t_
                    T(lambda e, bk=bk, t_=t_, kb=kb: e.transpose(out=self.bank[bk][:, t_ * 128:(t_ + 1) * 128], in_=SCORE[:, kb * 128:(kb + 1) * 128],
                                                                 identity=self.identf[:]),
                      r=[b_score[kb // 4], self.b_const], w=[self.bbank[bk]])
                V(lambda e, bk=bk, n=n, kb0=kb0: e.tensor_tensor(out=MT[:, kb0:kb0 + n, :],
                                                                 in0=self.bank[bk][:, 0:n * 128].rearrange("p (k t) -> p k t", k=n),
                                                                 in1=THRB[:].unsqueeze(1).to_broadcast([128, n, 128]), op=ALU.is_lt),
                  r=[self.bbank[bk], b_thr], w=[b_mt])

        def attention(i):
            nkb = 2 * i + 2
            ao, bao = AOUT[i % 2], b_aout[i % 2]
            Qm, b_qm, QLT, b_qlt = Qm2[i % 2], b_qm2[i % 2], QLT2[i % 2], b_qlt2[i % 2]
            for hg in range(2):
                p_o = [self.bank[4], self.bank[5]]
                bp_o = [self.bbank[4], self.bbank[5]]
                p_d, bp_d = self.bank[3], self.bbank[3]
                hs = slice(hg * 4, (hg + 1) * 4)
                state = {}

                def S_stage(kb):
                    if kb % 8 == 0:
                        s_ = self.cnt_ckvs % 2
                        self.cnt_ckvs += 1
                        n8 = min(8, nkb - kb)
                        self.dma(CKVs[s_][:, 0:n8, :], ckv_d[kb * 128:(kb + n8) * 128, :].rearrange("(k p) r -> p k r", p=128),
                                 [b_ckvd[k] for k in range(kb, kb + n8)], [b_ckvs[s_]], b_ckvs[s_])
                        state["cur%d" % (kb // 8)] = s_
                    pk = 6 + (kb % 2)
                    pst, bpst = self.bank[pk], self.bbank[pk]
                    ksl = slice(kb * 128, (kb + 1) * 128)
                    T(lambda e, pst=pst, ksl=ksl, hs=hs: e.matmul(pst[:], lhsT=CKVT[:, 0, ksl], rhs=QLT[:, 0, hs, :].rearrange("p h t -> p (h t)"),
                                                           start=True, stop=False), r=[b_kside[kb], b_qlt], w=[bpst])
                    T(lambda e, pst=pst, ksl=ksl, hs=hs: e.matmul(pst[:], lhsT=CKVT[:, 1, ksl], rhs=QLT[:, 1, hs, :].rearrange("p h t -> p (h t)"),
                                                           start=False, stop=False), r=[b_kside[kb], b_qlt], w=[bpst])
                    T(lambda e, pst=pst, ksl=ksl, hs=hs: e.matmul(pst[:], lhsT=KRT[:, ksl], rhs=Qm[:, hs, :].rearrange("p h t -> p (h t)"),
                                                           start=False, stop=False), r=[b_kside[kb], b_qm], w=[bpst])
                    T(lambda e, pst=pst, kb=kb: e.matmul(pst[:].rearrange("p (h t) -> p h t", h=4), lhsT=negi[:],
                                                         rhs=MT[:, kb, :].unsqueeze(1).to_broadcast([128, 4, 128]), start=False, stop=True),
                      r=[b_mt, self.b_const], w=[bpst])
                    t = self.cnt_pt % 3
                    self.cnt_pt += 1
                    state["t%d" % kb] = t
                    A(lambda e, pst=pst, t=t: e.activation(out=PT[t][:], in_=pst[:], func=AF.Exp), r=[bpst], w=[b_pt[t]])

                def O_stage(kb):
                    t = state["t%d" % kb]
                    cur = state["cur%d" % (kb // 8)]
                    kk = kb % 8
                    for c in range(2):
                        T(lambda e, c=c, t=t, kk=kk, cur=cur, kb=kb: e.matmul(p_o[c][:], lhsT=CKVs[cur][:, kk, c * 128:(c + 1) * 128], rhs=PT[t][:],
                                                                              start=(kb == 0), stop=(kb == nkb - 1)),
                          r=[b_ckvs[cur], b_pt[t]], w=[bp_o[c]])
                    T(lambda e, t=t, kb=kb: e.matmul(p_d[0:1, :], lhsT=self.onesb[:, 0:1], rhs=PT[t][:], start=(kb == 0), stop=(kb == nkb - 1)),
                      r=[b_pt[t], self.b_const], w=[bp_d])

                S_stage(0)
                for kb in range(nkb):
                    if kb + 1 < nkb:
                        S_stage(kb + 1)
                    O_stage(kb)
                    yield
                A(lambda e: e.activation(out=denr[:], in_=p_d[0:1, :], func=AF.Copy), r=[bp_d], w=[b_denr])
                A(lambda e: e.activation(out=OT[:, 0, :], in_=p_o[0][:], func=AF.Copy), r=[bp_o[0]], w=[b_ot])
                A(lambda e: e.activation(out=OT[:, 1, :], in_=p_o[1][:], func=AF.Copy), r=[bp_o[1]], w=[b_ot])
                p3, bp3 = self.bank[6], self.bbank[6]
                for hh in range(4):
                    T(lambda e, hh=hh: e.matmul(p3[:, hh:hh + 1], lhsT=denr[0:1, hh * 128:(hh + 1) * 128], rhs=self.onesf[0:1, 0:1], start=True, stop=True),
                      r=[b_denr, self.b_const], w=[bp3])
                A(lambda e: e.activation(out=rden[:], in_=p3[:, 0:4], func=AF.Ln), r=[bp3], w=[b_rden])
                A(lambda e: e.activation(out=rden[:], in_=rden[:], func=AF.Exp, scale=-1.0), r=[b_rden], w=[b_rden])
                p1, bp1 = self.bank[7], self.bbank[7]
                for hh in range(4):
                    h = hg * 4 + hh
                    for c in range(2):
                        T(lambda e, hh=hh, h=h, c=c: e.matmul(p1[:, hh * 64:(hh + 1) * 64], lhsT=OT[:, c, hh * 128:(hh + 1) * 128],
                                                              rhs=wuv[:, c, h * 64:(h + 1) * 64], start=(c == 0), stop=(c == 1)),
                          r=[b_ot, b_w], w=[bp1])
                for hh in range(4):
                    A(lambda e, hg=hg, hh=hh: e.activation(out=ao[:, hg * 256 + hh * 64:hg * 256 + (hh + 1) * 64], in_=p1[:, hh * 64:(hh + 1) * 64],
                                                           func=AF.Copy, scale=rden[:, hh:hh + 1]), r=[bp1, b_rden], w=[bao])
                yield
            self.dma(self.aout_d[i * 128:(i + 1) * 128, :], ao[:], [bao], [self.b_aoutd], self.b_aoutd, eng="gpsimd")

        def stage_a(i):
            yield from qside(i)
            yield
            gen = score_and_threshold(i)
            nch_ = ((2 * i + 2) * 128 + 511) // 512
            for _ in range(nch_):
                next(gen)
                yield
            if 2 * i + 2 < 2 * n_own:
                yield from kside(2 * i + 2)
                yield
                yield from kside(2 * i + 3)
                yield
            else:
                for _ in range(8):
                    yield
            yield from gen

        def n_units_a(i):
            nk = (2 * i + 2) * 128
            return 13 + (nk + 511) // 512 + N_BISECT

        def n_units_b(i):
            return 2 * (2 * i + 2) + 2

        for kb0 in (0, 1):
            for _ in kside(kb0):
                pass
        for _ in stage_a(0):
            pass
        masks(0)
        for i in range(n_own):
            gb = attention(i)
            if i + 1 < n_own:
                ga = stage_a(i + 1)
                nb = n_units_b(i)
                na_front = n_units_a(i + 1) - N_BISECT
                done_a = done_b = False
                ca = cb = 0
                while not (done_a and done_b):
                    if not done_a and ca >= na_front:
                        for _ in ga:
                            pass
                        done_a = True
                    elif not done_a and (done_b or ca * nb * 0.4 <= cb * na_front):
                        try:
                            next(ga)
                        except StopIteration:
                            done_a = True
                        ca += 1
                    else:
                        try:
                            next(gb)
                        except StopIteration:
                            done_b = True
                        cb += 1
                masks(i + 1)
            else:
                for _ in gb:
                    pass
        self.mark = mark

    def alloc_xload(self):
        sb = self.sb
        self.xf = [sb("xf%d" % k, [128, D], F32) for k in range(2)]
        self.xT = [sb("xT%d" % k, [128, 8, 128], BF16) for k in range(2)]
        self.b_xf = [Buf("xf%d" % k) for k in range(2)]
        self.b_xT = [Buf("xT%d" % k) for k in range(2)]

    def phase1b(self):
        V, A, G, T = self.V, self.A, self.G, self.T
        sb = self.sb
        n_own = self.n_own
        x_all, x_own = self.x_all, self.x_own
        TABA, TABO, b_taba, b_tabo = self.TABA, self.TABO, self.b_taba, self.b_tabo
        cpar, b_small = self.cpar, self.b_small
        wbq_d = self.dram_in("w_bq", [D, 768], F32)
        wbk_d = self.dram_in("w_bk", [D, 768], F32)
        wbv_d = self.dram_in("w_bv", [D, 768], F32)
        if self.debug:
            self.bout_d = self.dram_out("bout_d", [NOWN * 128, 256], BF16)
        else:
            self.bout_d = self.dram_tmp("bout_d", [NOWN * 128, 256], BF16)
        self.b_boutd = Buf("boutd")
        RING = 20
        wbq = sb("wbq", [128, 8, 768], BF16)
        wbk = sb("wbk", [128, 8, 768], BF16)
        wbv = sb("wbv", [128, 8, 768], BF16)
        b_w = Buf("wB")
        stg = [sb("stgB%d" % k, [128, 8, 512], F32) for k in range(2)]
        b_stg = [Buf("stgB%d" % k) for k in range(2)]
        self.alloc_xload()
        seq = []
        for i_ in range(n_own):
            seq += [x_all[(2 * i_) * 128:(2 * i_ + 1) * 128, :], x_all[(2 * i_ + 1) * 128:(2 * i_ + 2) * 128, :], x_own[i_ * 128:(i_ + 1) * 128, :]]
        self.set_x_sequence(seq)
        BKT = sb("BKT", [128, 6, RING, 128], BF16)
        BV = sb("BV", [128, RING, 12, 65], BF16)
        b_slot = [Buf("bslot%d" % k) for k in range(RING)]
        bkr = sb("bkr", [128, 12, 64], BF16)
        b_bkr = Buf("bkr")
        bqr = sb("bqr", [128, 12, 64], BF16)
        b_bqr = Buf("bqr")
        BQT2 = [sb("BQT%d" % k, [128, 2, 6, 128], BF16) for k in range(2)]
        b_bqt2 = [Buf("BQT%d" % k) for k in range(2)]
        RMs = sb("RMs", [128, 4], F32)
        b_rm = Buf("RMs")
        G(lambda e: e.iota(RMs[:, 2:3], pattern=[[0, 1]], base=0, channel_multiplier=1, allow_small_or_imprecise_dtypes=True), w=[b_rm])
        V(lambda e: e.tensor_scalar(out=RMs[:, 0:1], in0=RMs[:, 2:3], scalar1=63.5, scalar2=0.125, op0=ALU.is_lt, op1=ALU.mult), r=[b_rm], w=[b_rm])
        V(lambda e: e.tensor_scalar(out=RMs[:, 1:2], in0=RMs[:, 2:3], scalar1=63.5, scalar2=0.125, op0=ALU.is_gt, op1=ALU.mult), r=[b_rm], w=[b_rm])
        rtmp = sb("rtmpB", [128, 2, 64], F32)
        b_rtmp = Buf("rtmpB")
        MD = sb("MD", [128, 3, 128], F32)
        MDb = sb("MDb", [128, 3, 128], BF16)
        b_md = Buf("MD")
        vf = sb("vf", [128, 128], F32)
        vf2 = sb("vf2", [128, 128], F32)
        vi = sb("vi", [128, 128], I32)
        dl = sb("dl", [128, 128], F32)
        m1 = sb("m1", [128, 128], F32)
        b_dl = Buf("dl")
        MK = [sb("MK%d" % k, [128, 128], BF16) for k in range(4)]
        b_mk = [Buf("MK%d" % k) for k in range(4)]
        PT = [sb("PTb%d" % k, [128, 512], BF16) for k in range(2)]
        b_pt = [Buf("PTb%d" % k) for k in range(2)]
        PTm = [sb("PTmb%d" % k, [128, 512], BF16) for k in range(2)]
        b_ptm = [Buf("PTmb%d" % k) for k in range(2)]
        bout = sb("bout", [128, 256], BF16)
        b_bout = Buf("bout")
        rden = sb("rdenB", [128, 4], F32)
        b_rden = Buf("rdenB")

        self.load_weight_bf16(wbq, wbq_d, 768, b_w, stg, b_stg)
        self.load_weight_bf16(wbk, wbk_d, 768, b_w, stg, b_stg)
        self.load_weight_bf16(wbv, wbv_d, 768, b_w, stg, b_stg)
        G(lambda e: e.memset(BV[:, :, :, 64:65], 1.0), w=b_slot)
        V(lambda e: e.memset(MD[:, 0, :], 1.0), w=[b_md])
        for g, dil in ((1, 4), (2, 16)):
            V(lambda e, dil=dil: e.tensor_scalar(out=vf[:], in0=self.idsrc[:], scalar1=128.0, scalar2=1.0 / dil, op0=ALU.add, op1=ALU.mult),
              r=[self.b_const], w=[b_dl])
            V(lambda e: e.tensor_copy(out=vi[:], in_=vf[:]), w=[b_dl])
            V(lambda e: e.tensor_copy(out=vf2[:], in_=vi[:]), w=[b_dl])
            V(lambda e, g=g: e.tensor_tensor(out=MD[:, g, :], in0=vf[:], in1=vf2[:], op=ALU.is_equal), r=[b_dl], w=[b_md])
        V(lambda e: e.tensor_copy(out=MDb[:], in_=MD[:]), r=[b_md], w=[b_md])

        def proj768(xT, bxT, w, banks):
            for (bk, c0, cw) in ((banks[0], 0, 512), (banks[1], 512, 256)):
                for c in range(8):
                    T(lambda e, bk=bk, c=c, c0=c0, cw=cw: e.matmul(self.bank[bk][:, 0:cw], lhsT=xT[:, c, :], rhs=w[:, c, c0:c0 + cw],
                                                                   start=(c == 0), stop=(c == 7)),
                      r=[bxT, b_w], w=[self.bbank[bk]])

        def rope_heads(dst, banks, tab, blk, b_tab, b_dst):
            for (bk, hs, nh) in ((banks[0], 0, 8), (banks[1], 8, 4)):
                pv = self.bank[bk][:, 0:nh * 64].rearrange("p (h d) -> p h d", h=nh)
                cos = tab[:, blk, 0:8].unsqueeze(1).to_broadcast([128, nh, 8])
                sin = tab[:, blk, 8:16].unsqueeze(1).to_broadcast([128, nh, 8])
                tA = rtmp[:, 0, 0:nh * 8].rearrange("p (h d) -> p h d", h=nh)
                tB = rtmp[:, 1, 0:nh * 8].rearrange("p (h d) -> p h d", h=nh)
                self.rope(dst[:, hs:hs + nh, 0:8], dst[:, hs:hs + nh, 8:16], pv[:, :, 0:8], pv[:, :, 8:16], cos, sin, tA, tB,
                          [self.bbank[bk], b_tab], [b_dst], b_rtmp)
                A(lambda e, pv=pv, hs=hs, nh=nh: e.activation(out=dst[:, hs:hs + nh, 16:64], in_=pv[:, :, 16:64], func=AF.Copy),
                  r=[self.bbank[bk]], w=[b_dst])

        def kside(kb):
            slot = kb % RING
            xT, bxT = self.load_xT(x_all[kb * 128:(kb + 1) * 128, :])
            proj768(xT, bxT, wbk, (1, 3))
            rope_heads(bkr, (1, 3), TABA, kb, b_taba, b_bkr)
            bv2 = self.bview(2)
            for c in range(6):
                T(lambda e, c=c: e.transpose(out=bv2[:, c * 128:(c + 1) * 128], in_=bkr[:, 2 * c:2 * c + 2, :].rearrange("p h d -> p (h d)"),
                                             identity=self.identb[:]), r=[b_bkr, self.b_const], w=[self.bbank[2]])
            V(lambda e: e.tensor_copy(out=BKT[:, :, slot, :], in_=bv2[:, 0:768].rearrange("p (c t) -> p c t", c=6)),
              r=[self.bbank[2]], w=[b_slot[slot]])
            proj768(xT, bxT, wbv, (4, 5))
            A(lambda e: e.activation(out=BV[:, slot, 0:8, 0:64], in_=self.bank[4][:, 0:512].rearrange("p (h d) -> p h d", h=8), func=AF.Copy),
              r=[self.bbank[4]], w=[b_slot[slot]])
            V(lambda e: e.tensor_copy(out=BV[:, slot, 8:12, 0:64], in_=self.bank[5][:, 0:256].rearrange("p (h d) -> p h d", h=4)),
              r=[self.bbank[5]], w=[b_slot[slot]])

        def qside(i):
            xT, bxT = self.load_xT(x_own[i * 128:(i + 1) * 128, :])
            proj768(xT, bxT, wbq, (1, 3))
            rope_heads(bqr, (1, 3), TABO, i, b_tabo, b_bqr)
            bv2 = self.bview(2)
            for c in range(6):
                T(lambda e, c=c: e.transpose(out=bv2[:, c * 128:(c + 1) * 128], in_=bqr[:, 2 * c:2 * c + 2, :].rearrange("p h d -> p (h d)"),
                                             identity=self.identb[:]), r=[b_bqr, self.b_const], w=[self.bbank[2]])
            A(lambda e: e.activation(out=BQT2[i % 2][:, 0, :, :], in_=bv2[:, 0:768].rearrange("p (c t) -> p c t", c=6), func=AF.Copy, scale=RMs[:, 0:1]),
              r=[self.bbank[2], b_rm], w=[b_bqt2[i % 2]])
            V(lambda e: e.tensor_scalar(out=BQT2[i % 2][:, 1, :, :], in0=bv2[:, 0:768].rearrange("p (c t) -> p c t", c=6), scalar1=RMs[:, 1:2], scalar2=None,
                                        op0=ALU.mult), r=[self.bbank[2], b_rm], w=[b_bqt2[i % 2]])

        self.cnt_b = 0
        self.cnt_mk = 0
        WIN = (128.0, 512.0, 2048.0)
        WB = (1, 4, 16)

        def attention(i):
            pairs = []
            for g in range(3):
                for kb in range(2 * i - WB[g], 2 * i + 2):
                    if kb >= 0:
                        pairs.append((g, kb))
            p_o, bp_o = self.bank[6], self.bbank[6]
            npairs = len(pairs)
            tsel = {}
            BQT, b_bqt = BQT2[i % 2], b_bqt2[i % 2]

            def qk_stage(n):
                g, kb = pairs[n]
                slot = kb % RING
                t = self.cnt_b % 2
                self.cnt_b += 1
                tsel[n] = t
                pk = 7 if t == 0 else 0
                for hh in range(4):
                    h = 4 * g + hh
                    c, z = h // 2, h % 2
                    T(lambda e, pk=pk, hh=hh, c=c, z=z, slot=slot: e.matmul(self.bank[pk][:, hh * 128:(hh + 1) * 128], lhsT=BKT[:, c, slot, :],
                                                                            rhs=BQT[:, z, c, :], start=True, stop=True),
                      r=[b_slot[slot], b_bqt], w=[self.bbank[pk]])
                A(lambda e, t=t, pk=pk: e.activation(out=PT[t][:], in_=self.bank[pk][:], func=AF.Exp), r=[self.bbank[pk]], w=[b_pt[t]])
                d_rel = kb - 2 * i
                interior = (g == 1 and d_rel in (-2, -1)) or (g == 2 and -14 <= d_rel <= -1)
                if interior:
                    mask = MDb[:, g, :]
                    rmask = [b_md]
                else:
                    m = self.cnt_mk % 4
                    self.cnt_mk += 1
                    V(lambda e, d_rel=d_rel: e.tensor_scalar(out=dl[:], in0=self.idsrc[:], scalar1=cpar[:, 2:3], scalar2=-128.0 * d_rel,
                                                             op0=ALU.add, op1=ALU.add), r=[self.b_const, b_small], w=[b_dl])
                    V(lambda e, g=g: e.scalar_tensor_tensor(out=m1[:], in0=dl[:], scalar=0.0, in1=MD[:, g, :], op0=ALU.is_ge, op1=ALU.mult),
                      r=[b_md], w=[b_dl])
                    V(lambda e, g=g, m=m: e.scalar_tensor_tensor(out=MK[m][:], in0=dl[:], scalar=WIN[g], in1=m1[:], op0=ALU.is_le, op1=ALU.mult),
                      r=[b_dl], w=[b_mk[m]])
                    mask = MK[m][:]
                    rmask = [b_mk[m]]
                eng = V
                eng(lambda e, t=t, mask=mask: e.tensor_tensor(out=PTm[t][:].rearrange("p (h t) -> p h t", h=4),
                                                              in0=PT[t][:].rearrange("p (h t) -> p h t", h=4),
                                                              in1=mask.unsqueeze(1).to_broadcast([128, 4, 128]), op=ALU.mult),
                    r=[b_pt[t]] + rmask, w=[b_ptm[t]])

            def pv_stage(n):
                g, kb = pairs[n]
                slot = kb % RING
                t = tsel[n]
                for hh in range(4):
                    ci = hh
                    T(lambda e, t=t, hh=hh, slot=slot, g=g, n=n, ci=ci: e.matmul(p_o[:, hh * 65:(hh + 1) * 65], lhsT=PTm[t][:, ci * 128:(ci + 1) * 128],
                                                                          rhs=BV[:, slot, 4 * g + hh, :], start=(n == 0 and hh == 0),
                                                                          stop=(n == npairs - 1 and hh == 3), skip_group_check=True),
                      r=[b_ptm[t], b_slot[slot]], w=[bp_o])

            qk_stage(0)
            for n in range(npairs):
                if n + 1 < npairs:
                    qk_stage(n + 1)
                pv_stage(n)
                yield
            if st1b in ("attn_qk", "attn_mask", "attn_pv"):
                return
            pov = p_o[:, 0:260].rearrange("p (h d) -> p h d", h=4)
            V(lambda e: e.reciprocal(out=rden[:].unsqueeze(2), in_=pov[:, :, 64:65]), r=[bp_o], w=[b_rden])
            V(lambda e: e.tensor_tensor(out=bout[:].rearrange("p (h d) -> p h d", h=4), in0=pov[:, :, 0:64],
                                        in1=rden[:].unsqueeze(2).to_broadcast([128, 4, 64]), op=ALU.mult), r=[bp_o, b_rden], w=[b_bout])
            self.dma(self.bout_d[i * 128:(i + 1) * 128, :], bout[:], [b_bout], [self.b_boutd], self.b_boutd, eng="gpsimd")

        st1b = ""

        def stage_a(i):
            kside(2 * i)
            yield
            kside(2 * i + 1)
            yield
            qside(i)
            yield

        for _ in stage_a(0):
            pass
        for i in range(n_own):
            gb = attention(i)
            if i + 1 < n_own:
                ga = stage_a(i + 1)
                nb = sum(1 for g in range(3) for kb in range(2 * i - WB[g], 2 * i + 2) if kb >= 0) + 1
                na = 3
                done_a = done_b = False
                ca = cb = 0
                while not (done_a and done_b):
                    if not done_a and (done_b or ca * nb <= cb * na):
                        try:
                            next(ga)
                        except StopIteration:
                            done_a = True
                        ca += 1
                    else:
                        try:
                            next(gb)
                        except StopIteration:
                            done_b = True
                        cb += 1
            else:
                for _ in gb:
                    pass

    def load_bcast(self, dst, src_row_ap, b_dst):
        self.dma(dst, src_row_ap.partition_broadcast(128).rearrange("p o n -> p (o n)"), [], [b_dst], b_dst)

    def phase2(self):
        V, A, G, T = self.V, self.A, self.G, self.T
        sb = self.sb
        n_own = self.n_own
        C = self.CAP
        NSLOT = 64 * C
        ALPHA = 2.0 ** 0.25
        di = self.dram_in
        wga_d, wgb_d = di("w_ga", [D, D], F32), di("w_gb", [D, D], F32)
        bgate_d = di("b_gate", [1, 2 * D], F32)
        wba_d, wbb_d, wo_d = di("w_branch_a", [512, D], F32), di("w_branch_b", [256, D], F32), di("w_o", [D, D], F32)
        ln1g_d, ln1b_d = di("ln1_g", [1, D], F32), di("ln1_b", [1, D], F32)
        wr_d, rb_d = di("w_router", [D, 64], F32), di("router_bias", [1, 64], F32)
        ws1_d, ws3_d, ws2_d = di("ws1", [D, 256], F32), di("ws3", [D, 256], F32), di("ws2", [256, D], F32)
        self.base_d = self.dram_tmp("base_d", [NOWN * 128, D], F32)
        self.b_based = Buf("based")
        if self.debug:
            self.h_d = self.dram_out("h_d", [NOWN * 128, D], F32)
            self.b_hd = Buf("hd")

        wga, wgb = sb("wga", [128, 8, D], BF16), sb("wgb", [128, 8, D], BF16)
        wba, wbb, wo = sb("wba", [128, 4, D], BF16), sb("wbb", [128, 2, D], BF16), sb("wo", [128, 8, D], BF16)
        wr = sb("wr", [128, 8, 64], BF16)
        ws1, ws3, ws2 = sb("ws1", [128, 8, 256], BF16), sb("ws3", [128, 8, 256], BF16), sb("ws2", [128, 2, D], BF16)
        b_w = Buf("w2")
        stg = [sb("stg2%d" % k, [128, 8, 512], F32) for k in range(2)]
        b_stg = [Buf("stg2%d" % k) for k in range(2)]
        bgate = sb("bgate", [128, 2 * D], F32)
        ln1g, ln1b = sb("ln1g", [128, D], F32), sb("ln1b", [128, D], F32)
        rbias = sb("rbias", [128, 64], F32)
        b_vec = Buf("vec2")
        self.alloc_xload()
        self.set_x_sequence([self.x_own[i_ * 128:(i_ + 1) * 128, :] for i_ in range(n_own)])
        gate = [sb("gate%d" % k, [128, D], F32) for k in range(2)]
        b_gate = [Buf("gate%d" % k) for k in range(2)]
        gtmp = [sb("gtmp%d" % k, [128, 512], F32) for k in range(2)]
        b_gtmp = [Buf("gtmp%d" % k) for k in range(2)]
        ab = sb("ab", [128, 768], BF16)
        b_ab = Buf("ab")
        abT = sb("abT", [128, 6, 128], BF16)
        b_abT = Buf("abT")
        mm = sb("mm", [128, D], BF16)
        b_mm = Buf("mm")
        mT = sb("mT", [128, 8, 128], BF16)
        b_mT = Buf("mT")
        u = sb("u", [128, D], F32)
        b_u = Buf("u")
        junkf = sb("junk2", [128, D], F32)
        b_junkf = Buf("junk2")
        st = sb("st2", [128, 8], F32)
        b_st = Buf("st2")
        h2 = [sb("h%d" % k, [128, D], F32) for k in range(2)]
        b_h2 = [Buf("h%d" % k) for k in range(2)]
        hb = [sb("hb%d" % k, [128, D], BF16) for k in range(2)]
        b_hb = [Buf("hb%d" % k) for k in range(2)]
        hT2 = [sb("hT%d" % k, [128, 8, 128], BF16) for k in range(2)]
        b_hT2 = [Buf("hT%d" % k) for k in range(2)]
        EM = sb("EM", [128, NOWN, 64], BF16)
        b_em = Buf("EM")
        UT = sb("UT", [128, 128], BF16)
        eoff = sb("eoff", [128, 64], F32)
        b_c2 = Buf("c2")
        rt = sb("rt", [128, 12, 64], F32)
        b_rt = Buf("rt")
        rs = sb("rs", [128, 64], F32)
        b_rs = Buf("rs")
        s8f = sb("s8f", [128, 8], F32)
        sil = sb("sil", [128, 256], F32)
        b_sil = Buf("sil")
        GT = sb("GT", [128, 256], BF16)
        b_gt = Buf("GT")
        base = sb("base", [128, D], F32)
        b_base = Buf("base")

        def loadw(dst, src, rows, cols):
            nchunk = rows // 128
            srcv = src.rearrange("(c p) n -> p c n", p=128)
            k = 0
            for c0 in range(0, cols, 512):
                cw = min(512, cols - c0)
                sidx = k % 2
                k += 1
                self.dma(stg[sidx][:, 0:nchunk, 0:cw], srcv[:, :, c0:c0 + cw], [], [b_stg[sidx]], b_stg[sidx])
                self.cast_rr(dst[:, :, c0:c0 + cw], stg[sidx][:, 0:nchunk, 0:cw], [b_stg[sidx]], [b_w])
        loadw(wga, wga_d, D, D)
        loadw(wgb, wgb_d, D, D)
        loadw(wba, wba_d, 512, D)
        loadw(wbb, wbb_d, 256, D)
        loadw(wo, wo_d, D, D)
        loadw(wr, wr_d, D, 64)
        loadw(ws1, ws1_d, D, 256)
        loadw(ws3, ws3_d, D, 256)
        loadw(ws2, ws2_d, 256, D)
        self.load_bcast(bgate, bgate_d, b_vec)
        self.load_bcast(ln1g, ln1g_d, b_vec)
        self.load_bcast(ln1b, ln1b_d, b_vec)
        self.load_bcast(rbias, rb_d, b_vec)
        V(lambda e: e.tensor_scalar(out=UT[:], in0=self.idsrc[:], scalar1=0.0, scalar2=None, op0=ALU.is_gt), r=[self.b_const], w=[b_c2])
        G(lambda e: e.iota(eoff[:], pattern=[[C, 64]], base=0, channel_multiplier=0, allow_small_or_imprecise_dtypes=True), w=[b_c2])

        halves = ((0, 512), (512, 512))

        def layer_norm(src, b_src, dst, b_dst, gam, bet, eps):
            A(lambda e: e.activation(out=junkf[:], in_=src[:], func=AF.Copy, accum_out=st[:, 0:1]), r=[b_src], w=[b_junkf, b_st])
            A(lambda e: e.activation(out=junkf[:], in_=src[:], func=AF.Square, accum_out=st[:, 1:2]), r=[b_src], w=[b_junkf, b_st])
            V(lambda e: e.tensor_scalar(out=st[:, 2:3], in0=st[:, 0:1], scalar1=1.0 / D, scalar2=None, op0=ALU.mult), r=[b_st], w=[b_st])
            V(lambda e: e.tensor_tensor(out=st[:, 3:4], in0=st[:, 2:3], in1=st[:, 2:3], op=ALU.mult), r=[b_st], w=[b_st])
            V(lambda e: e.scalar_tensor_tensor(out=st[:, 4:5], in0=st[:, 1:2], scalar=1.0 / D, in1=st[:, 3:4], op0=ALU.mult, op1=ALU.subtract),
              r=[b_st], w=[b_st])
            V(lambda e: e.tensor_scalar(out=st[:, 4:5], in0=st[:, 4:5], scalar1=eps, scalar2=None, op0=ALU.add), r=[b_st], w=[b_st])
            A(lambda e: e.sqrt(out=st[:, 5:6], in_=st[:, 4:5]), r=[b_st], w=[b_st])
            V(lambda e: e.reciprocal(out=st[:, 6:7], in_=st[:, 5:6]), r=[b_st], w=[b_st])
            V(lambda e: e.tensor_scalar(out=dst[:], in0=src[:], scalar1=st[:, 2:3], scalar2=st[:, 6:7], op0=ALU.subtract, op1=ALU.mult),
              r=[b_src, b_st], w=[b_dst])
            V(lambda e: e.tensor_tensor(out=dst[:], in0=dst[:], in1=gam[:], op=ALU.mult), r=[b_vec], w=[b_dst])
            V(lambda e: e.tensor_tensor(out=dst[:], in0=dst[:], in1=bet[:], op=ALU.add), r=[b_vec], w=[b_dst])
        self.layer_norm = layer_norm

        def front(i):
            h, b_h, hT, b_hT = h2[i % 2], b_h2[i % 2], hT2[i % 2], b_hT2[i % 2]
            xT, bxT = self.load_xT(self.x_own[i * 128:(i + 1) * 128, :])
            xs = self.last_s
            xf, b_xf = self.xf[xs], self.b_xf[xs]
            for gi, (w, banks) in enumerate(((wga, (1, 3)), (wgb, (4, 5)))):
                for hi, (c0, cw) in enumerate(halves):
                    bk = banks[hi]
                    for c in range(8):
                        T(lambda e, bk=bk, c=c, c0=c0, w=w: e.matmul(self.bank[bk][:], lhsT=xT[:, c, :], rhs=w[:, c, c0:c0 + 512],
                                                                   start=(c == 0), stop=(c == 7)), r=[bxT, b_w], w=[self.bbank[bk]])
                    V(lambda e, bk=bk, gi=gi, c0=c0, hi=hi: e.tensor_tensor(out=gtmp[hi][:], in0=self.bank[bk][:],
                                                                          in1=bgate[:, gi * D + c0:gi * D + c0 + 512], op=ALU.add),
                      r=[self.bbank[bk], b_vec], w=[b_gtmp[hi]])
                    A(lambda e, gi=gi, c0=c0, hi=hi: e.activation(out=gate[gi][:, c0:c0 + 512], in_=gtmp[hi][:], func=AF.Sigmoid),
                      r=[b_gtmp[hi]], w=[b_gate[gi]])
                    yield
            self.dma(ab[:, 0:512], self.aout_d[i * 128:(i + 1) * 128, :], [self.b_aoutd], [b_ab], b_ab)
            self.dma(ab[:, 512:768], self.bout_d[i * 128:(i + 1) * 128, :], [self.b_boutd], [b_ab], b_ab)
            bv2 = self.bview(2)
            for c in range(6):
                T(lambda e, c=c: e.transpose(out=bv2[:, c * 128:(c + 1) * 128], in_=ab[:, c * 128:(c + 1) * 128], identity=self.identb[:]),
                  r=[b_ab, self.b_const], w=[self.bbank[2]])
            A(lambda e: e.activation(out=abT[:], in_=bv2[:, 0:768].rearrange("p (c t) -> p c t", c=6), func=AF.Copy), r=[self.bbank[2]], w=[b_abT])
            yield
            for hi, (c0, cw) in enumerate(halves):
                ba, bb_ = (1, 3)[hi], (4, 5)[hi]
                for c in range(4):
                    T(lambda e, ba=ba, c=c, c0=c0: e.matmul(self.bank[ba][:], lhsT=abT[:, c, :], rhs=wba[:, c, c0:c0 + 512], start=(c == 0), stop=(c == 3)),
                      r=[b_abT, b_w], w=[self.bbank[ba]])
                for c in range(2):
                    T(lambda e, bb_=bb_, c=c, c0=c0: e.matmul(self.bank[bb_][:], lhsT=abT[:, 4 + c, :], rhs=wbb[:, c, c0:c0 + 512], start=(c == 0), stop=(c == 1)),
                      r=[b_abT, b_w], w=[self.bbank[bb_]])
                V(lambda e, ba=ba, c0=c0: e.tensor_tensor(out=gtmp[0][:], in0=self.bank[ba][:], in1=gate[0][:, c0:c0 + 512], op=ALU.mult),
                  r=[self.bbank[ba], b_gate[0]], w=[b_gtmp[0]])
                V(lambda e, bb_=bb_, c0=c0: e.tensor_tensor(out=gtmp[1][:], in0=self.bank[bb_][:], in1=gate[1][:, c0:c0 + 512], op=ALU.mult),
                  r=[self.bbank[bb_], b_gate[1]], w=[b_gtmp[1]])
                V(lambda e, c0=c0: e.tensor_tensor(out=mm[:, c0:c0 + 512], in0=gtmp[0][:], in1=gtmp[1][:], op=ALU.add),
                  r=[b_gtmp[0], b_gtmp[1]], w=[b_mm])
                yield
            for c in range(8):
                T(lambda e, c=c: e.transpose(out=bv2[:, c * 128:(c + 1) * 128], in_=mm[:, c * 128:(c + 1) * 128], identity=self.identb[:]),
                  r=[b_mm, self.b_const], w=[self.bbank[2]])
            A(lambda e: e.activation(out=mT[:], in_=bv2[:].rearrange("p (c t) -> p c t", c=8), func=AF.Copy), r=[self.bbank[2]], w=[b_mT])
            yield
            for hi, (c0, cw) in enumerate(halves):
                bk = (1, 3)[hi]
                for c in range(8):
                    T(lambda e, bk=bk, c=c, c0=c0: e.matmul(self.bank[bk][:], lhsT=mT[:, c, :], rhs=wo[:, c, c0:c0 + 512], start=(c == 0), stop=(c == 7)),
                      r=[b_mT, b_w], w=[self.bbank[bk]])
                V(lambda e, bk=bk, c0=c0: e.scalar_tensor_tensor(out=u[:, c0:c0 + 512], in0=xf[:, c0:c0 + 512], scalar=ALPHA, in1=self.bank[bk][:],
                                                                 op0=ALU.mult, op1=ALU.add), r=[b_xf, self.bbank[bk]], w=[b_u])
                yield
            layer_norm(u, b_u, h, b_h, ln1g, ln1b, 1e-5)
            yield
            if self.debug:
                self.dma(self.h_d[i * 128:(i + 1) * 128, :], h[:], [b_h], [self.b_hd], self.b_hd)
            hbb, b_hbb = hb[i % 2], b_hb[i % 2]
            A(lambda e: e.activation(out=hbb[:], in_=h[:], func=AF.Copy), r=[b_h], w=[b_hbb])
            for c in range(8):
                T(lambda e, c=c: e.transpose(out=bv2[:, c * 128:(c + 1) * 128], in_=hbb[:, c * 128:(c + 1) * 128], identity=self.identb[:]),
                  r=[b_hbb, self.b_const], w=[self.bbank[2]])
            V(lambda e: e.tensor_copy(out=hT[:], in_=bv2[:].rearrange("p (c t) -> p c t", c=8)), r=[self.bbank[2]], w=[b_hT])

        def back(i):
            h, b_h, hT, b_hT = h2[i % 2], b_h2[i % 2], hT2[i % 2], b_hT2[i % 2]
            hbb, b_hbb = hb[i % 2], b_hb[i % 2]
            p6, bp6 = self.bank[6], self.bbank[6]
            for c in range(8):
                T(lambda e, c=c: e.matmul(p6[:, 0:64], lhsT=hT[:, c, :], rhs=wr[:, c, :], start=(c == 0), stop=(c == 7)), r=[b_hT, b_w], w=[bp6])
            sc, bia, grp2, tt, mb, emk, ts, wfull, slotf, jk = (rt[:, k, :] for k in range(10))
            gm1, gm2, gs, s8, gmask, pen, v8 = rs[:, 0:8], rs[:, 8:16], rs[:, 16:24], rs[:, 24:32], rs[:, 32:40], rs[:, 40:48], rs[:, 48:56]
            den = rs[:, 56:57]
            g3 = lambda ap: ap.rearrange("p (g e) -> p g e", g=8)
            A(lambda e: e.activation(out=sc, in_=p6[:, 0:64], func=AF.Sigmoid), r=[bp6], w=[b_rt])
            yield
            V(lambda e: e.tensor_tensor(out=bia, in0=sc, in1=rbias[:], op=ALU.add), r=[b_vec], w=[b_rt])
            V(lambda e: e.tensor_reduce(out=gm1, in_=g3(bia), axis=AX.X, op=ALU.max), r=[b_rt], w=[b_rs])
            V(lambda e: e.tensor_tensor(out=g3(tt), in0=g3(bia), in1=gm1.unsqueeze(2).to_broadcast([128, 8, 8]), op=ALU.is_ge), r=[b_rs], w=[b_rt])
            V(lambda e: e.scalar_tensor_tensor(out=grp2, in0=tt, scalar=-1.0e9, in1=bia, op0=ALU.mult, op1=ALU.add), w=[b_rt])
            V(lambda e: e.tensor_reduce(out=gm2, in_=g3(grp2), axis=AX.X, op=ALU.max), r=[b_rt], w=[b_rs])
            V(lambda e: e.tensor_tensor(out=gs, in0=gm1, in1=gm2, op=ALU.add), w=[b_rs])
            V(lambda e: e.max(out=s8, in_=gs), w=[b_rs])
            V(lambda e: e.tensor_scalar(out=gmask, in0=gs, scalar1=s8[:, 3:4], scalar2=None, op0=ALU.is_ge), w=[b_rs])
            yield
            V(lambda e: e.tensor_scalar(out=pen, in0=gmask, scalar1=-1.0, scalar2=1.0e9, op0=ALU.add, op1=ALU.mult), w=[b_rs])
            V(lambda e: e.tensor_tensor(out=g3(tt), in0=g3(bia), in1=gmask.unsqueeze(2).to_broadcast([128, 8, 8]), op=ALU.mult), r=[b_rs], w=[b_rt])
            V(lambda e: e.tensor_tensor(out=g3(mb), in0=g3(tt), in1=pen.unsqueeze(2).to_broadcast([128, 8, 8]), op=ALU.add), r=[b_rs], w=[b_rt])
            V(lambda e: e.max(out=v8, in_=mb), r=[b_rt], w=[b_rs])
            V(lambda e: e.tensor_scalar(out=emk, in0=mb, scalar1=v8[:, 7:8], scalar2=None, op0=ALU.is_ge), r=[b_rs], w=[b_rt])
            V(lambda e: e.tensor_copy(out=EM[:, i, :], in_=emk), r=[b_rt], w=[b_em])
            V(lambda e: e.tensor_tensor(out=ts, in0=sc, in1=emk, op=ALU.mult), w=[b_rt])
            V(lambda e: e.tensor_reduce(out=den, in_=ts, axis=AX.X, op=ALU.add), r=[b_rt], w=[b_rs])
            V(lambda e: e.reciprocal(out=den, in_=den), w=[b_rs])
            V(lambda e: e.tensor_scalar(out=wfull, in0=ts, scalar1=den, scalar2=2.5, op0=ALU.mult, op1=ALU.mult), r=[b_rs], w=[b_rt])
            yield
            p7, bp7 = self.bank[7], self.bbank[7]
            for j in range(i):
                T(lambda e, j=j: e.matmul(p7[:, 0:64], lhsT=self.onesb[:], rhs=EM[:, j, :], start=(j == 0), stop=False), r=[b_em, self.b_const], w=[bp7])
            T(lambda e: e.matmul(p7[:, 0:64], lhsT=UT[:], rhs=EM[:, i, :], start=(i == 0), stop=True), r=[b_em, b_c2], w=[bp7])
            V(lambda e: e.tensor_scalar(out=jk, in0=p7[:, 0:64], scalar1=C - 0.5, scalar2=1.0e6, op0=ALU.is_ge, op1=ALU.mult), r=[bp7], w=[b_rt])
            V(lambda e: e.tensor_tensor(out=slotf, in0=p7[:, 0:64], in1=eoff[:], op=ALU.add), r=[bp7, b_c2], w=[b_rt])
            V(lambda e: e.tensor_tensor(out=slotf, in0=slotf, in1=jk, op=ALU.add), w=[b_rt])
            yield
            for k in range(8):
                V(lambda e, k=k: e.scalar_tensor_tensor(out=jk, in0=mb, scalar=v8[:, k:k + 1], in1=slotf, op0=ALU.is_equal, op1=ALU.mult,
                                                        accum_out=s8f[:, k:k + 1]), r=[b_rs], w=[b_rt])
                V(lambda e, k=k: e.scalar_tensor_tensor(out=jk, in0=mb, scalar=v8[:, k:k + 1], in1=wfull, op0=ALU.is_equal, op1=ALU.mult,
                                                        accum_out=self.W8[:, i, k:k + 1]), r=[b_rs], w=[b_rt, self.b_route])
            V(lambda e: e.tensor_copy(out=self.SLOT8[:, i, :], in_=s8f[:]), r=[b_rt], w=[self.b_route])
            yield
            for k in range(8):
                self.P.op("gpsimd", lambda e, k=k: e.indirect_dma_start(
                    out=self.xe_d[:, :], out_offset=bass.IndirectOffsetOnAxis(ap=self.SLOT8[:, i, k:k + 1], axis=0),
                    in_=hbb[:], in_offset=None, bounds_check=self.bc_reg(e, NSLOT - 1), oob_is_err=False),
                    [b_hbb, self.b_route], [self.b_xed], dma_out=self.b_xed)
            yield
            for fi, w in enumerate((ws1, ws3)):
                for fc in range(2):
                    r0 = (fi * 2 + fc) * 128
                    for c in range(8):
                        T(lambda e, w=w, fc=fc, c=c, r0=r0: e.matmul(p6[:, r0:r0 + 128], lhsT=w[:, c, fc * 128:(fc + 1) * 128], rhs=hT[:, c, :],
                                                                     start=(c == 0), stop=(c == 7)), r=[b_w, b_hT], w=[bp6])
            A(lambda e: e.activation(out=sil[:], in_=p6[:, 0:256], func=AF.Silu), r=[bp6], w=[b_sil])
            V(lambda e: e.tensor_tensor(out=GT[:], in0=p6[:, 256:512], in1=sil[:], op=ALU.mult), r=[bp6, b_sil], w=[b_gt])
            yield
            for hi, (c0, cw) in enumerate(halves):
                bk = (1, 3)[hi]
                for fc in range(2):
                    T(lambda e, bk=bk, fc=fc, c0=c0: e.matmul(self.bank[bk][:], lhsT=GT[:, fc * 128:(fc + 1) * 128], rhs=ws2[:, fc, c0:c0 + 512],
                                                              start=(fc == 0), stop=(fc == 1)), r=[b_gt, b_w], w=[self.bbank[bk]])
                V(lambda e, bk=bk, c0=c0: e.scalar_tensor_tensor(out=base[:, c0:c0 + 512], in0=h[:, c0:c0 + 512], scalar=ALPHA, in1=self.bank[bk][:],
                                                                 op0=ALU.mult, op1=ALU.add), r=[b_h, self.bbank[bk]], w=[b_base])
                yield
            self.dma(self.base_d[i * 128:(i + 1) * 128, :], base[:], [b_base], [self.b_based], self.b_based, eng="gpsimd")


        for _ in front(0):
            pass
        for i in range(n_own):
            gb = back(i)
            if i + 1 < n_own:
                ga = front(i + 1)
                na, nb = 12, 9
                done_a = done_b = False
                ca = cb = 0
                while not (done_a and done_b):
                    if not done_a and (done_b or ca * nb <= cb * na):
                        try:
                            next(ga)
                        except StopIteration:
                            done_a = True
                        ca += 1
                    else:
                        try:
                            next(gb)
                        except StopIteration:
                            done_b = True
                        cb += 1
            else:
                for _ in gb:
                    pass


    def phase3(self):
        V, A, G, T = self.V, self.A, self.G, self.T
        sb = self.sb
        C = self.CAP
        NSLOT = 64 * C
        NCH = C // 256
        w1_d = self.dram_in("w1_e", [64, D, 256], F32)
        w3_d = self.dram_in("w3_e", [64, D, 256], F32)
        w2_d = self.dram_in("w2_e", [64, 256, D], F32)
        self.ye_d = self.dram_tmp("ye_d", [NSLOT, D], BF16)
        self.b_yed = Buf("yed")
        n_exp = self.n_exp
        w1s = [sb("w1s%d" % k, [128, 8, 256], F32) for k in range(2)]
        w3s = [sb("w3s%d" % k, [128, 8, 256], F32) for k in range(2)]
        w2s = [sb("w2s%d" % k, [128, 2, D], F32) for k in range(2)]
        b_ws = [[Buf("w%ds%d" % (j, k)) for j in range(3)] for k in range(2)]
        w1b = [sb("w1b%d" % k, [128, 8, 256], BF16) for k in range(2)]
        w3b = [sb("w3b%d" % k, [128, 8, 256], BF16) for k in range(2)]
        w2b = [sb("w2b%d" % k, [128, 2, D], BF16) for k in range(2)]
        b_wb = [Buf("wb%d" % k) for k in range(2)]
        xe = [sb("xe%d" % k, [128, 2, D], BF16) for k in range(2)]
        b_xe = [Buf("xe%d" % k) for k in range(2)]
        XeT = [sb("XeT%d" % k, [128, 8, 256], BF16) for k in range(2)]
        b_xet = [Buf("XeT%d" % k) for k in range(2)]
        sil = [sb("sil3%d" % k, [128, 512], F32) for k in range(2)]
        b_sil = [Buf("sil3%d" % k) for k in range(2)]
        GT = [sb("GT3%d" % k, [128, 512], BF16) for k in range(2)]
        b_gt = [Buf("GT3%d" % k) for k in range(2)]
        Y = [sb("Y%d" % k, [128, D], BF16) for k in range(4)]
        b_y = [Buf("Y%d" % k) for k in range(4)]
        steps = [(ex, ch) for ex in range(n_exp) for ch in range(NCH)]

        def load_w(ex):
            ws = ex % 2
            self.dma(w1s[ws][:], w1_d[ex].rearrange("(c p) n -> p c n", p=128), [], [b_ws[ws][0]], b_ws[ws][0])
            self.dma(w3s[ws][:], w3_d[ex].rearrange("(c p) n -> p c n", p=128), [], [b_ws[ws][1]], b_ws[ws][1])
            self.dma(w2s[ws][:], w2_d[ex].rearrange("(c p) n -> p c n", p=128), [], [b_ws[ws][2]], b_ws[ws][2])
            A(lambda e, ws=ws: e.activation(out=w1b[ws][:], in_=w1s[ws][:], func=AF.Copy), r=[b_ws[ws][0]], w=[b_wb[ws]])
            V(lambda e, ws=ws: e.tensor_copy(out=w3b[ws][:], in_=w3s[ws][:]), r=[b_ws[ws][1]], w=[b_wb[ws]])
            G(lambda e, ws=ws: e.tensor_copy(out=w2b[ws][:], in_=w2s[ws][:]), r=[b_ws[ws][2]], w=[b_wb[ws]])

        def stage1(n):
            ex, ch = steps[n]
            ws = ex % 2
            if ch == 0 and ex == 0:
                load_w(0)
            if ch == 1 and ex + 1 < n_exp:
                load_w(ex + 1)
            t = n % 2
            r0 = ex * C + ch * 256
            self.dma(xe[t][:], self.xe_d[r0:r0 + 256, :].rearrange("(k p) d -> p k d", p=128), [self.b_xed], [b_xe[t]], b_xe[t])
            for kk in range(2):
                bvt = self.bview(kk)
                for c in range(8):
                    T(lambda e, c=c, t=t, kk=kk, bvt=bvt: e.transpose(out=bvt[:, c * 128:(c + 1) * 128], in_=xe[t][:, kk, c * 128:(c + 1) * 128],
                                                                  identity=self.identb[:]), r=[b_xe[t], self.b_const], w=[self.bbank[kk]])
                if kk == 0:
                    A(lambda e, t=t, bvt=bvt: e.activation(out=XeT[t][:, :, 0:128], in_=bvt[:].rearrange("p (c t) -> p c t", c=8), func=AF.Copy),
                      r=[self.bbank[kk]], w=[b_xet[t]])
                else:
                    V(lambda e, t=t, bvt=bvt: e.tensor_copy(out=XeT[t][:, :, 128:256], in_=bvt[:].rearrange("p (c t) -> p c t", c=8)),
                      r=[self.bbank[kk]], w=[b_xet[t]])
            for fi, w in enumerate((w1b[ws], w3b[ws])):
                bk = 2 + fi
                for fc in range(2):
                    for c in range(8):
                        T(lambda e, w=w, fc=fc, c=c, t=t, bk=bk: e.matmul(self.bank[bk][:, fc * 256:(fc + 1) * 256], lhsT=w[:, c, fc * 128:(fc + 1) * 128],
                                                                      rhs=XeT[t][:, c, :], start=(c == 0), stop=(c == 7)),
                          r=[b_wb[ws], b_xet[t]], w=[self.bbank[bk]])
            A(lambda e, t=t: e.activation(out=sil[t][:], in_=self.bank[2][:], func=AF.Silu), r=[self.bbank[2]], w=[b_sil[t]])
            V(lambda e, t=t: e.tensor_tensor(out=GT[t][:], in0=self.bank[3][:], in1=sil[t][:], op=ALU.mult), r=[self.bbank[3], b_sil[t]], w=[b_gt[t]])

        def stage2(n):
            ex, ch = steps[n]
            ws = ex % 2
            t = n % 2
            for kk in range(2):
                r0 = ex * C + ch * 256 + kk * 128
                ybanks = (4, 5) if kk == 0 else (6, 7)
                yi = (2 * n + kk) % 4
                for hi in range(2):
                    bk = ybanks[hi]
                    for fc in range(2):
                        T(lambda e, bk=bk, fc=fc, hi=hi, t=t, ws=ws, kk=kk: e.matmul(self.bank[bk][:], lhsT=GT[t][:, fc * 256 + kk * 128:fc * 256 + (kk + 1) * 128],
                                                                                 rhs=w2b[ws][:, fc, hi * 512:(hi + 1) * 512], start=(fc == 0), stop=(fc == 1)),
                          r=[b_gt[t], b_wb[ws]], w=[self.bbank[bk]])
                A(lambda e, yi=yi, bk=ybanks[0]: e.activation(out=Y[yi][:, 0:512], in_=self.bank[bk][:], func=AF.Copy), r=[self.bbank[ybanks[0]]], w=[b_y[yi]])
                V(lambda e, yi=yi, bk=ybanks[1]: e.tensor_copy(out=Y[yi][:, 512:1024], in_=self.bank[bk][:]), r=[self.bbank[ybanks[1]]], w=[b_y[yi]])
                self.dma(self.ye_d[r0:r0 + 128, :], Y[yi][:], [b_y[yi]], [self.b_yed], self.b_yed, eng="gpsimd")

        ns = len(steps)
        stage1(0)
        for n in range(ns):
            if n + 1 < ns:
                stage1(n + 1)
            stage2(n)

    def phase4(self):
        V, A, G, T = self.V, self.A, self.G, self.T
        sb = self.sb
        C = self.CAP
        NSLOT = 64 * C
        ln2g_d, ln2b_d = self.dram_in("ln2_g", [1, D], F32), self.dram_in("ln2_b", [1, D], F32)
        self.out_d = self.dram_out("out_d", [NOWN * 128, D], F32)
        self.b_outd = Buf("outd")
        ln2g, ln2b = sb("ln2g", [128, D], F32), sb("ln2b", [128, D], F32)
        b_vec = Buf("vec4")
        self.load_bcast(ln2g, ln2g_d, b_vec)
        self.load_bcast(ln2b, ln2b_d, b_vec)
        acc = [sb("acc%d" % k, [128, D], F32) for k in range(2)]
        b_acc = [Buf("acc%d" % k) for k in range(2)]
        yk = [sb("yk%d" % k, [128, D], BF16) for k in range(8)]
        b_yk = [Buf("yk%d" % k) for k in range(8)]
        o = [sb("o%d" % k, [128, D], F32) for k in range(2)]
        b_o = [Buf("o%d" % k) for k in range(2)]
        junkf = sb("junk4", [128, D], F32)
        st = sb("st4", [128, 8], F32)
        b_junkf, b_st = Buf("junk4"), Buf("st4")

        def layer_norm(src, b_src, dst, b_dst, gam, bet, eps):
            A(lambda e: e.activation(out=junkf[:], in_=src[:], func=AF.Copy, accum_out=st[:, 0:1]), r=[b_src], w=[b_junkf, b_st])
            A(lambda e: e.activation(out=junkf[:], in_=src[:], func=AF.Square, accum_out=st[:, 1:2]), r=[b_src], w=[b_junkf, b_st])
            V(lambda e: e.tensor_scalar(out=st[:, 2:3], in0=st[:, 0:1], scalar1=1.0 / D, scalar2=None, op0=ALU.mult), r=[b_st], w=[b_st])
            V(lambda e: e.tensor_tensor(out=st[:, 3:4], in0=st[:, 2:3], in1=st[:, 2:3], op=ALU.mult), r=[b_st], w=[b_st])
            V(lambda e: e.scalar_tensor_tensor(out=st[:, 4:5], in0=st[:, 1:2], scalar=1.0 / D, in1=st[:, 3:4], op0=ALU.mult, op1=ALU.subtract),
              r=[b_st], w=[b_st])
            V(lambda e: e.tensor_scalar(out=st[:, 4:5], in0=st[:, 4:5], scalar1=eps, scalar2=None, op0=ALU.add), r=[b_st], w=[b_st])
            A(lambda e: e.sqrt(out=st[:, 5:6], in_=st[:, 4:5]), r=[b_st], w=[b_st])
            V(lambda e: e.reciprocal(out=st[:, 6:7], in_=st[:, 5:6]), r=[b_st], w=[b_st])
            V(lambda e: e.tensor_scalar(out=dst[:], in0=src[:], scalar1=st[:, 2:3], scalar2=st[:, 6:7], op0=ALU.subtract, op1=ALU.mult),
              r=[b_src, b_st], w=[b_dst])
            V(lambda e: e.tensor_tensor(out=dst[:], in0=dst[:], in1=gam[:], op=ALU.mult), r=[b_vec], w=[b_dst])
            V(lambda e: e.tensor_tensor(out=dst[:], in0=dst[:], in1=bet[:], op=ALU.add), r=[b_vec], w=[b_dst])

        n = 0
        self.dma(acc[0][:], self.base_d[0:128, :], [self.b_based], [b_acc[0]], b_acc[0])
        for i in range(self.n_own):
            a, b_a = acc[i % 2], b_acc[i % 2]
            if i + 1 < self.n_own:
                self.dma(acc[(i + 1) % 2][:], self.base_d[(i + 1) * 128:(i + 2) * 128, :], [self.b_based], [b_acc[(i + 1) % 2]], b_acc[(i + 1) % 2])
            for k in range(8):
                t = n % 8
                n += 1
                self.P.op("gpsimd", lambda e, t=t, i=i, k=k: e.indirect_dma_start(
                    out=yk[t][:], out_offset=None, in_=self.ye_d[:, :],
                    in_offset=bass.IndirectOffsetOnAxis(ap=self.SLOT8[:, i, k:k + 1], axis=0), bounds_check=self.bc_reg(e, NSLOT - 1), oob_is_err=False),
                    [self.b_yed, self.b_route], [b_yk[t]], dma_out=b_yk[t])
                V(lambda e, t=t, i=i, k=k, a=a: e.scalar_tensor_tensor(out=a[:], in0=yk[t][:], scalar=self.W8[:, i, k:k + 1], in1=a[:],
                                                                       op0=ALU.mult, op1=ALU.add), r=[b_yk[t], self.b_route], w=[b_a])
            oo, b_oo = o[i % 2], b_o[i % 2]
            layer_norm(a, b_a, oo, b_oo, ln2g, ln2b, 1e-5)
            self.dma(self.out_d[i * 128:(i + 1) * 128, :], oo[:], [b_oo], [self.b_outd], self.b_outd)


def build_program(n_own=NOWN, debug=False, phases="ab234", n_exp=64):
    nc = bass.Bass("TRN2", target_bir_lowering=False)
    st = ExitStack()
    with st:
        B = Builder(nc, st, n_own=n_own, debug=debug)
        B.CAP = 768
        B.n_exp = n_exp
        B.setup_common()
        B.phase1a()
        fin = [B.b_aoutd]
        if "b" in phases:
            B.phase_reset(B.mark)
            B.phase1b()
            fin.append(B.b_boutd)
        if "2" in phases:
            B.phase_reset(B.mark)
            B.phase2()
            fin += [B.b_based, B.b_xed]
            if debug:
                fin.append(B.b_hd)
        if "3" in phases:
            B.phase_reset(B.mark)
            B.phase3()
            fin.append(B.b_yed)
        if "4" in phases:
            B.phase_reset(B.mark)
            B.phase4()
            fin.append(B.b_outd)
        B.P.final_wait("sync", fin)
        B.P.emit()
        print("[kernel] semaphores used:", B.P.nsem, "ops:", {e: len(v) for e, v in B.P.ops.items()})
    return nc


def rope_consts():
    inv16 = ROPE_THETA ** (-np.arange(0, 16, 2, dtype=np.float32) / 16)
    inv8 = ROPE_THETA ** (-np.arange(0, 8, 2, dtype=np.float32) / 8)
    inv = np.concatenate([inv16, inv16, inv8, inv8]).astype(np.float32)
    off = np.concatenate([np.full(8, math.pi / 2), np.zeros(8), np.full(4, math.pi / 2), np.zeros(4)]).astype(np.float32)
    return np.tile(np.concatenate([inv, off])[None, :], (128, 1)).astype(np.float32)


def make_in_maps(inputs, cores=range(8)):
    x = inputs["x"]
    positions = inputs["positions"]
    w_in = inputs["w_in"][0]
    offs = np.cumsum([0, 512, 256, 16, 256, 32, 8, 768, 768, 768, 1024, 1024])
    col = lambda k: slice(offs[k], offs[k + 1])
    wk_dsa = np.ascontiguousarray(np.concatenate([w_in[:, col(1)], w_in[:, col(2)], w_in[:, col(4)]], axis=1))
    wq_dsa = np.ascontiguousarray(np.concatenate([w_in[:, col(0)], w_in[:, col(3)], w_in[:, col(5)]], axis=1))
    w_bq = np.ascontiguousarray(w_in[:, col(6)])
    w_bk = np.ascontiguousarray(w_in[:, col(7)])
    w_bv = np.ascontiguousarray(w_in[:, col(8)])
    w_ga = np.ascontiguousarray(w_in[:, col(9)])
    w_gb = np.ascontiguousarray(w_in[:, col(10)])
    f32c = lambda a: np.ascontiguousarray(a, dtype=np.float32)
    import ml_dtypes
    zeros_bf = np.zeros((1024, D), dtype=ml_dtypes.bfloat16)
    w1_e, w3_e, w2_e = f32c(inputs["w1_e"][0]), f32c(inputs["w3_e"][0]), f32c(inputs["w2_e"][0])
    wuk_t = np.ascontiguousarray(inputs["w_uk"][0].transpose(2, 1, 0))
    wuv = np.ascontiguousarray(inputs["w_uv"][0].reshape(256, 512))
    rc = rope_consts()
    maps = []
    for c in cores:
        b, par = c // 2, c % 2
        xb = x[b]
        x_own = np.ascontiguousarray(xb.reshape(NBLK, 128, D)[par::2].reshape(NOWN * 128, D))
        pos_t = np.ascontiguousarray(positions[b].reshape(NBLK, 128).T.astype(np.int32))
        pos_own_t = np.ascontiguousarray(positions[b].reshape(NBLK, 128)[par::2].T.astype(np.int32))
        maps.append({
            "x_all": np.ascontiguousarray(xb), "x_own": x_own, "pos_all_t": pos_t, "pos_own_t": pos_own_t,
            "par": np.full((128, 1), float(par), np.float32), "rope_c": rc,
            "wk_dsa": wk_dsa, "wq_dsa": wq_dsa, "wuk_t": wuk_t, "wuv": wuv,
            "w_bq": w_bq, "w_bk": w_bk, "w_bv": w_bv, "w_ga": w_ga, "w_gb": w_gb,
            "b_gate": f32c(inputs["b_gate"].reshape(1, 2048)), "w_branch_a": f32c(inputs["w_branch_a"][0]),
            "w_branch_b": f32c(inputs["w_branch_b"][0]), "w_o": f32c(inputs["w_o"][0]),
            "ln1_g": f32c(inputs["ln1_g"].reshape(1, D)), "ln1_b": f32c(inputs["ln1_b"].reshape(1, D)),
            "w_router": f32c(inputs["w_router"][0]), "router_bias": f32c(inputs["router_bias"].reshape(1, 64)),
            "ws1": f32c(inputs["ws1"][0]), "ws3": f32c(inputs["ws3"][0]), "ws2": f32c(inputs["ws2"][0]),
            "w1_e": w1_e, "w3_e": w3_e, "w2_e": w2_e, "zeros_d": zeros_bf,
            "ln2_g": f32c(inputs["ln2_g"].reshape(1, D)), "ln2_b": f32c(inputs["ln2_b"].reshape(1, D)),
            "g_kv": np.ascontiguousarray(inputs["g_kv"].reshape(1, 256)),
        })
    return maps


_NC_CACHE = {}


def kernel(**inputs):
    inputs = {k: np.asarray(v) for k, v in inputs.items()}
    if "nc" not in _NC_CACHE:
        _NC_CACHE["nc"] = build_program()
    nc = _NC_CACHE["nc"]
    maps = make_in_maps(inputs, cores=range(8))
    res = run_bass_kernel_spmd(nc, maps, core_ids=list(range(8)))
    out = np.empty((4, S, D), np.float32)
    for c in range(8):
        b, par = c // 2, c % 2
        o = np.asarray(res.results[c]["out_d"], dtype=np.float32).reshape(NOWN, 128, D)
        out[b].reshape(NBLK, 128, D)[par::2] = o
    return out
```

```python
import math
from contextlib import ExitStack

import numpy as np
import concourse.bass as bass
import concourse.mybir as mybir
from concourse.bass_utils import run_bass_kernel_spmd

F32 = mybir.dt.float32
BF16 = mybir.dt.bfloat16
I32 = mybir.dt.int32
U32 = mybir.dt.uint32
AF = mybir.ActivationFunctionType
ALU = mybir.AluOpType
AX = mybir.AxisListType

ENGS = ["tensor", "vector", "scalar", "gpsimd", "sync"]

D = 1024
S = 8192
NBLK = 64
NOWN = 32
ROPE_THETA = 500000.0
NEG = -1.0e30
N_BISECT = 16
TWO_PI = 2.0 * math.pi
CW1 = 6.28125
CW2 = TWO_PI - CW1


class Buf:
    __slots__ = ("name", "w", "readers", "dma_sem", "dma_cnt", "excl")

    def __init__(self, name, excl=False):
        self.name = name
        self.excl = excl
        self.w = None
        self.readers = []
        self.dma_sem = None
        self.dma_cnt = 0


class Prog:
    def __init__(self, nc, stack):
        self.nc = nc
        self.stack = stack
        self.ops = {e: [] for e in ENGS}
        self.cnt = {e: 0 for e in ENGS}
        self.sems = {e: stack.enter_context(nc.semaphore("s_" + e)) for e in ENGS}
        self.known = {e: {} for e in ENGS}
        self.nsem = len(ENGS)
        self.dma_bufs = []

    def new_sem(self, name):
        self.nsem += 1
        return self.stack.enter_context(self.nc.semaphore("%s_%d" % (name, self.nsem)))

    def _add_wait(self, waits, tok, eng):
        if tok is None:
            return
        if tok[0] == "e":
            if tok[1] == eng and eng in ("tensor", "sync"):
                return
            key = ("e", tok[1])
            val = tok[2]
        else:
            key = ("d", id(tok[1]))
            val = tok[2] * 16
            waits.setdefault("_sem", {})[key] = tok[1].dma_sem
        if waits.get(key, 0) < val:
            waits[key] = val

    def op(self, eng, fn, reads=(), writes=(), dma_out=None):
        xr = [b for b in reads if b.excl]
        if xr:
            reads = [b for b in reads if not b.excl]
            writes = list(writes) + [b for b in xr if b not in writes]
        waits = {}
        for b in reads:
            self._add_wait(waits, b.w, eng)
        for b in writes:
            self._add_wait(waits, b.w, eng)
            for r in b.readers:
                self._add_wait(waits, r, eng)
        semmap = waits.pop("_sem", {})
        wl = []
        kn = self.known[eng]
        for key, val in waits.items():
            if kn.get(key, 0) >= val:
                continue
            kn[key] = val
            if key[0] == "e":
                wl.append((self.sems[key[1]], val))
            else:
                wl.append((semmap[key], val))
        if dma_out is not None:
            if dma_out.dma_sem is None:
                dma_out.dma_sem = self.new_sem("d_" + dma_out.name)
                self.dma_bufs.append(dma_out)
            dma_out.dma_cnt += 1
            tok = ("d", dma_out, dma_out.dma_cnt)
            self.ops[eng].append((wl, fn, (dma_out.dma_sem, 16)))
        else:
            self.cnt[eng] += 1
            tok = ("e", eng, self.cnt[eng])
            self.ops[eng].append((wl, fn, (self.sems[eng], 1)))
        for b in reads:
            b.readers.append(tok)
            if len(b.readers) > 64:
                b.readers = b.readers[-48:]
        for b in writes:
            b.w = tok
            b.readers = []
        return tok

    def barrier(self):
        for eng in ENGS:
            wl = []
            for f in ENGS:
                if f != eng and self.cnt[f] > 0:
                    wl.append((self.sems[f], self.cnt[f]))
                    self.known[eng][("e", f)] = self.cnt[f]
            for b in self.dma_bufs:
                wl.append((b.dma_sem, b.dma_cnt * 16))
                self.known[eng][("d", id(b))] = b.dma_cnt * 16
            self.ops[eng].append((wl, None, None))

    def final_wait(self, eng, bufs):
        waits = {}
        for b in bufs:
            self._add_wait(waits, b.w, eng)
        semmap = waits.pop("_sem", {})
        wl = []
        for key, val in waits.items():
            if key[0] == "e":
                wl.append((self.sems[key[1]], val))
            else:
                wl.append((semmap[key], val))
        self.ops[eng].append((wl, None, None))

    def emit(self):
        nc = self.nc
        with nc.Block() as block:
            def mk(ename):
                def body(engine):
                    for wl, fn, inc in self.ops[ename]:
                        for sem, val in wl:
                            engine.wait_ge(sem, val)
                        if fn is not None:
                            ins = fn(engine)
                            ins.then_inc(inc[0], inc[1])
                return body
            block.tensor(mk("tensor"))
            block.vector(mk("vector"))
            block.scalar(mk("scalar"))
            block.gpsimd(mk("gpsimd"))
            block.sync(mk("sync"))


class Builder:
    def __init__(self, nc, st, n_own=NOWN, debug=False):
        self.nc = nc
        self.st = st
        self.P = Prog(nc, st)
        self.n_own = n_own
        self.debug = debug
        self.outs = []
        self.pool = None
        self.bump = 0

    POOL_BYTES = 212800

    def sb(self, name, shape, dt):
        if self.pool is None:
            self.pool = self.st.enter_context(self.nc.sbuf_tensor("sb_pool", [128, self.POOL_BYTES // 2], BF16))
            self.bump = 0
        esz = mybir.dt.size(dt)
        n = 1
        for d_ in shape[1:]:
            n *= d_
        nbytes = (n * esz + 63) // 64 * 64
        off = self.bump
        self.bump += nbytes
        assert self.bump <= self.POOL_BYTES, "SBUF pool overflow at %s: %d" % (name, self.bump)
        v = self.pool[0:shape[0], off // 2: off // 2 + (n * esz) // 2]
        if dt != BF16:
            v = v.bitcast(dt)
        if len(shape) == 3:
            v = v.rearrange("p (a b) -> p a b", a=shape[1])
        elif len(shape) == 4:
            v = v.rearrange("p (a b c) -> p a b c", a=shape[1], b=shape[2])
        return v

    def phase_mark(self):
        return self.bump

    def phase_reset(self, mark):
        self.P.barrier()
        self.bump = mark

    def dram_in(self, name, shape, dt):
        return self.nc.dram_tensor(name, shape, dt, kind="ExternalInput").ap()

    def dram_out(self, name, shape, dt):
        t = self.nc.dram_tensor(name, shape, dt, kind="ExternalOutput").ap()
        return t

    def dram_tmp(self, name, shape, dt):
        return self.nc.dram_tensor(name, shape, dt, kind="Internal").ap()

    def bc_reg(self, e, val):
        if getattr(self, "_bcreg", None) is None:
            self._bcreg = e.to_reg(val)
        return self._bcreg

    def cast_rr(self, out, in_, r, w):
        k = getattr(self, "_rr", 0)
        self._rr = k + 1
        if k % 3 == 0:
            self.A(lambda e: e.activation(out=out, in_=in_, func=AF.Copy), r, w)
        elif k % 3 == 1:
            self.V(lambda e: e.tensor_copy(out=out, in_=in_), r, w)
        else:
            self.G(lambda e: e.tensor_copy(out=out, in_=in_), r, w)

    def V(self, fn, r=(), w=()):
        return self.P.op("vector", fn, r, w)

    def A(self, fn, r=(), w=()):
        return self.P.op("scalar", fn, r, w)

    def G(self, fn, r=(), w=()):
        return self.P.op("gpsimd", fn, r, w)

    def T(self, fn, r=(), w=()):
        return self.P.op("tensor", fn, r, w)

    def dma(self, out, in_, r, w, dma_buf, eng="sync"):
        return self.P.op(eng, lambda e: e.dma_start(out=out, in_=in_), r, w, dma_out=dma_buf)

    def setup_common(self):
        nc = self.nc
        self.bank = []
        self.bbank = []
        for k in range(8):
            t = self.st.enter_context(nc.psum_tensor("bank%d" % k, [128, 512], F32))
            self.bank.append(t)
            self.bbank.append(Buf("bank%d" % k, excl=True))
        self.idsrc = self.sb("idsrc", [128, 128], F32)
        self.identf = self.sb("identf", [128, 128], F32)
        self.identb = self.sb("identb", [128, 128], BF16)
        self.onesf = self.sb("onesf", [128, 128], F32)
        self.onesb = self.sb("onesb", [128, 128], BF16)
        self.b_const = Buf("const")
        bc = self.b_const
        self.G(lambda e: e.iota(self.idsrc[:], pattern=[[1, 128]], base=0, channel_multiplier=-1,
                                allow_small_or_imprecise_dtypes=True), w=[bc])
        self.V(lambda e: e.tensor_scalar(out=self.identf[:], in0=self.idsrc[:], scalar1=0.0, scalar2=None,
                                         op0=ALU.is_equal), r=[bc], w=[bc])
        self.V(lambda e: e.tensor_copy(out=self.identb[:], in_=self.identf[:]), r=[bc], w=[bc])
        self.V(lambda e: e.memset(self.onesf[:], 1.0), w=[bc])
        self.V(lambda e: e.memset(self.onesb[:], 1.0), w=[bc])
        NSLOT = 64 * self.CAP
        zeros_d = self.dram_in("zeros_d", [1024, D], BF16)
        self.xe_d = self.dram_tmp("xe_d", [NSLOT, D], BF16)
        self.b_xed = Buf("xed")
        for r0 in range(0, NSLOT, 1024):
            self.P.op("gpsimd", lambda e, r0=r0: e.dma_start(out=self.xe_d[r0:r0 + 1024, :], in_=zeros_d[:, :]), [], [self.b_xed], dma_out=self.b_xed)

    def bview(self, k, dt=BF16):
        return self.bank[k][:].bitcast(dt)

    def rope_table(self, pos_i32, nblk, tab, scr, b_scr, b_tab, ropec, b_in):
        n = nblk * 24
        ang = scr[:, 0:n]
        kf = scr[:, n:2 * n]
        mm = scr[:, 2 * n:3 * n]
        ki = scr[:, 3 * n:4 * n].bitcast(I32)
        posf = scr[:, 4 * n:4 * n + nblk]
        inv = ropec[:, 0:24]
        off = ropec[:, 24:48]
        ang3 = ang.rearrange("p (b j) -> p b j", j=24)
        V = self.V
        V(lambda e: e.tensor_copy(out=posf, in_=pos_i32), r=[b_in], w=[b_scr])
        V(lambda e: e.tensor_tensor(out=ang3, in0=posf.unsqueeze(2).to_broadcast([128, nblk, 24]),
                                    in1=inv.unsqueeze(1).to_broadcast([128, nblk, 24]), op=ALU.mult), r=[b_in], w=[b_scr])
        V(lambda e: e.tensor_tensor(out=ang3, in0=ang3, in1=off.unsqueeze(1).to_broadcast([128, nblk, 24]),
                                    op=ALU.add), r=[b_in], w=[b_scr])
        V(lambda e: e.tensor_scalar(out=ki, in0=ang, scalar1=1.0 / TWO_PI, scalar2=None, op0=ALU.mult), w=[b_scr])
        V(lambda e: e.tensor_copy(out=kf, in_=ki), w=[b_scr])
        V(lambda e: e.scalar_tensor_tensor(out=ang, in0=kf, scalar=-CW1, in1=ang, op0=ALU.mult, op1=ALU.add), w=[b_scr])
        V(lambda e: e.scalar_tensor_tensor(out=ang, in0=kf, scalar=-CW2, in1=ang, op0=ALU.mult, op1=ALU.add), w=[b_scr])
        V(lambda e: e.tensor_scalar(out=mm, in0=ang, scalar1=math.pi, scalar2=-TWO_PI, op0=ALU.is_gt, op1=ALU.mult), w=[b_scr])
        V(lambda e: e.tensor_tensor(out=ang, in0=ang, in1=mm, op=ALU.add), w=[b_scr])
        V(lambda e: e.tensor_scalar(out=mm, in0=ang, scalar1=-math.pi, scalar2=TWO_PI, op0=ALU.is_lt, op1=ALU.mult), w=[b_scr])
        V(lambda e: e.tensor_tensor(out=ang, in0=ang, in1=mm, op=ALU.add), w=[b_scr])
        V(lambda e: e.tensor_scalar(out=ang, in0=ang, scalar1=3.14159, scalar2=-3.14159, op0=ALU.min, op1=ALU.max), w=[b_scr])
        self.A(lambda e: e.activation(out=tab[:].rearrange("p b j -> p (b j)"), in_=ang, func=AF.Sin), r=[b_scr], w=[b_tab])

    def rope(self, o1, o2, x1, x2, cos, sin, tA, tB, r, w, b_tmp):
        V = self.V
        V(lambda e: e.tensor_tensor(out=tA, in0=x1, in1=cos, op=ALU.mult), r=r, w=[b_tmp])
        V(lambda e: e.tensor_tensor(out=tB, in0=x2, in1=sin, op=ALU.mult), r=r, w=[b_tmp])
        V(lambda e: e.tensor_tensor(out=o1, in0=tA, in1=tB, op=ALU.subtract), r=[b_tmp], w=w)
        V(lambda e: e.tensor_tensor(out=tA, in0=x2, in1=cos, op=ALU.mult), r=r, w=[b_tmp])
        V(lambda e: e.tensor_tensor(out=tB, in0=x1, in1=sin, op=ALU.mult), r=r, w=[b_tmp])
        V(lambda e: e.tensor_tensor(out=o2, in0=tA, in1=tB, op=ALU.add), r=[b_tmp], w=w)

    def load_weight_bf16(self, dst, src_ap, ncols, b_dst, stage, b_stage, chunk=512):
        srcv = src_ap.rearrange("(c p) n -> p c n", p=128)
        k = 0
        for c0 in range(0, ncols, chunk):
            cw = min(chunk, ncols - c0)
            sidx = k % len(stage)
            k += 1
            stg = stage[sidx]
            self.dma(stg[:, :, 0:cw], srcv[:, :, c0:c0 + cw], [], [b_stage[sidx]], b_stage[sidx])
            self.cast_rr(dst[:, :, c0:c0 + cw], stg[:, :, 0:cw], [b_stage[sidx]], [b_dst])

    def set_x_sequence(self, seq):
        self.xseq = list(seq)
        self.xl_k = 0
        self.x_issued = 0

    def _issue_x(self, k):
        s = k % 2
        self.dma(self.xf[s][:], self.xseq[k], [], [self.b_xf[s]], self.b_xf[s])

    def load_xT(self, src_rows=None):
        k = self.xl_k
        self.xl_k += 1
        s = k % 2
        if self.x_issued <= k:
            self._issue_x(k)
            self.x_issued = k + 1
        if k + 1 < len(self.xseq) and self.x_issued <= k + 1:
            self._issue_x(k + 1)
            self.x_issued = k + 2
        xf, bxf = self.xf[s], self.b_xf[s]
        xT, bxT = self.xT[s], self.b_xT[s]
        for half, bk in ((0, 0), (1, 2)):
            for c4 in range(4):
                c = half * 4 + c4
                self.T(lambda e, c=c, c4=c4, bk=bk: e.transpose(out=self.bank[bk][:, c4 * 128:(c4 + 1) * 128], in_=xf[:, c * 128:(c + 1) * 128],
                                                              identity=self.identf[:]), r=[bxf, self.b_const], w=[self.bbank[bk]])
        self.A(lambda e: e.activation(out=xT[:, 0:4, :], in_=self.bank[0][:].rearrange("p (c t) -> p c t", c=4), func=AF.Copy),
               r=[self.bbank[0]], w=[bxT])
        self.V(lambda e: e.tensor_copy(out=xT[:, 4:8, :], in_=self.bank[2][:].rearrange("p (c t) -> p c t", c=4)),
               r=[self.bbank[2]], w=[bxT])
        self.last_s = s
        return xT, bxT

    def phase1a(self):
        nc, P = self.nc, self.P
        V, A, G, T = self.V, self.A, self.G, self.T
        n_own = self.n_own
        x_all = self.dram_in("x_all", [S, D], F32)
        x_own = self.dram_in("x_own", [NOWN * 128, D], F32)
        pos_all = self.dram_in("pos_all_t", [128, NBLK], I32)
        pos_own = self.dram_in("pos_own_t", [128, NOWN], I32)
        par_d = self.dram_in("par", [128, 1], F32)
        ropec_d = self.dram_in("rope_c", [128, 48], F32)
        wk_d = self.dram_in("wk_dsa", [D, 304], F32)
        wq_d = self.dram_in("wq_dsa", [D, 776], F32)
        wuk_d = self.dram_in("wuk_t", [48, 8, 256], F32)
        wuv_d = self.dram_in("wuv", [256, 512], F32)
        gkv_d = self.dram_in("g_kv", [1, 256], F32)
        ckv_d = self.dram_out("ckv_d", [S, 256], BF16) if self.debug else self.dram_tmp("ckv_d", [S, 256], BF16)
        if self.debug:
            self.aout_d = self.dram_out("aout_d", [NOWN * 128, 512], BF16)
        else:
            self.aout_d = self.dram_tmp("aout_d", [NOWN * 128, 512], BF16)
        _ckvd8 = [Buf("ckvd%d" % k) for k in range(8)]
        b_ckvd = [_ckvd8[k % 8] for k in range(NBLK)]
        self.b_aoutd = Buf("aoutd")

        sb = self.sb
        par = sb("par", [128, 1], F32)
        cpar = sb("cpar", [128, 4], F32)
        ropec = sb("ropec", [128, 48], F32)
        posa = sb("posa", [128, NBLK], I32)
        poso = sb("poso", [128, NOWN], I32)
        b_small = Buf("smallin")
        TABA = sb("TABA", [128, NBLK, 24], F32)
        TABO = sb("TABO", [128, NOWN, 24], F32)
        b_taba, b_tabo = Buf("taba"), Buf("tabo")
        bm8 = sb("bm8", [128, 8], BF16)
        tmpv = sb("tmpv", [128, 16], F32)
        self.SLOT8 = sb("SLOT8", [128, NOWN, 8], I32)
        self.W8 = sb("W8", [128, NOWN, 8], F32)
        self.b_route = Buf("route")
        self.par, self.cpar, self.TABA, self.TABO = par, cpar, TABA, TABO
        self.b_small, self.b_taba, self.b_tabo = b_small, b_taba, b_tabo
        self.x_all, self.x_own = x_all, x_own
        mark = self.phase_mark()
        CKVT = sb("CKVT", [128, 2, S], BF16)
        KRT = sb("KRT", [128, S], BF16)
        IKT = sb("IKT", [128, S], BF16)
        b_kside = [Buf("kside%d" % k) for k in range(NBLK)]
        SCORE = sb("SCORE", [128, S], F32)
        b_score = [Buf("score%d" % k) for k in range(16)]
        b_scoreall = b_score
        MT = sb("MT", [128, NBLK, 128], BF16)
        b_mt = Buf("MT")
        junk = sb("junk", [128, 1024], BF16)
        b_junk = Buf("junk")
        self.alloc_xload()
        xa = lambda kb: x_all[kb * 128:(kb + 1) * 128, :]
        xo = lambda i_: x_own[i_ * 128:(i_ + 1) * 128, :]
        nkb_tot = 2 * n_own
        seq = [xa(0), xa(1)]
        for i_ in range(n_own):
            seq.append(xo(i_))
            if 2 * i_ + 2 < nkb_tot:
                seq += [xa(2 * i_ + 2), xa(2 * i_ + 3)]
        self.set_x_sequence(seq)
        wk = sb("wk", [128, 8, 304], BF16)
        wq = sb("wq", [128, 8, 776], BF16)
        wuk = sb("wuk", [48, 8, 256], BF16)
        wuv = sb("wuv", [128, 2, 512], BF16)
        gkv = sb("gkv", [128, 256], F32)
        b_w = Buf("weights")

        self.dma(par[:], par_d[:, :], [], [b_small], b_small)
        self.dma(ropec[:], ropec_d[:, :], [], [b_small], b_small)
        self.dma(posa[:], pos_all[:, :], [], [b_small], b_small)
        self.dma(poso[:], pos_own[:, :], [], [b_small], b_small)
        self.dma(gkv[:], gkv_d.partition_broadcast(128).rearrange("p o r -> p (o r)"), [], [b_small], b_small)
        b_setup = Buf("setup")
        stg = [SCORE[:, k * 4096:(k + 1) * 4096].rearrange("p (c n) -> p c n", c=8) for k in range(2)]
        b_stg = [b_setup, b_setup]
        self.load_weight_bf16(wk, wk_d, 304, b_w, stg, b_stg)
        self.load_weight_bf16(wq, wq_d, 776, b_w, stg, b_stg)
        s0 = SCORE[0:48, 0:2048].rearrange("p (h r) -> p h r", h=8)
        self.dma(s0, wuk_d[:, :, :], [], [b_setup], b_setup)
        G(lambda e: e.tensor_copy(out=wuk[:], in_=s0), r=[b_setup], w=[b_w])
        s1 = SCORE[:, 4096:5120].rearrange("p (c n) -> p c n", c=2)
        self.dma(s1, wuv_d.rearrange("(c p) n -> p c n", p=128), [], [b_setup], b_setup)
        G(lambda e: e.tensor_copy(out=wuv[:], in_=s1), r=[b_setup], w=[b_w])
        V(lambda e: e.tensor_scalar(out=cpar[:, 0:1], in0=par[:], scalar1=-128.0, scalar2=None, op0=ALU.mult), r=[b_small], w=[b_small])
        V(lambda e: e.tensor_scalar(out=cpar[:, 1:2], in0=par[:], scalar1=-128.0, scalar2=128.0, op0=ALU.mult, op1=ALU.add), r=[b_small], w=[b_small])
        V(lambda e: e.tensor_scalar(out=cpar[:, 2:3], in0=par[:], scalar1=128.0, scalar2=None, op0=ALU.mult), r=[b_small], w=[b_small])
        G(lambda e: e.iota(tmpv[:, 0:8], pattern=[[-16, 8]], base=0, channel_multiplier=1, allow_small_or_imprecise_dtypes=True), w=[b_small])
        V(lambda e: e.tensor_scalar(out=tmpv[:, 8:16], in0=tmpv[:, 0:8], scalar1=0.0, scalar2=0.125, op0=ALU.is_ge, op1=ALU.mult), r=[b_small], w=[b_small])
        V(lambda e: e.tensor_scalar(out=tmpv[:, 0:8], in0=tmpv[:, 0:8], scalar1=15.0, scalar2=None, op0=ALU.is_le), r=[b_small], w=[b_small])
        V(lambda e: e.tensor_tensor(out=bm8[:], in0=tmpv[:, 0:8], in1=tmpv[:, 8:16], op=ALU.mult), r=[b_small], w=[b_small])
        scr = SCORE[:, 0:8192]
        self.rope_table(posa[:], NBLK, TABA, scr, b_setup, b_taba, ropec, b_small)
        self.rope_table(poso[:], NOWN, TABO, scr, b_setup, b_tabo, ropec, b_small)
        tok = V(lambda e: e.memset(SCORE[:, 0:2], 0.0), w=[b_setup])
        for k in range(16):
            b_score[k].w = tok

        ss = sb("ss", [128, 4], F32)
        b_ss = Buf("ss")
        ckvn = [sb("ckvn%d" % k, [128, 256], BF16) for k in range(2)]
        b_ckvn = [Buf("ckvn%d" % k) for k in range(2)]
        rtmp = sb("rtmp", [128, 2, 64], F32)
        b_rtmp = Buf("rtmp")
        krr = sb("krr", [128, 16], F32)
        krrep = sb("krrep", [128, 8, 16], BF16)
        ikr = sb("ikr", [128, 32], BF16)
        b_kr = Buf("kr")

        def kside(kb):
            xT, bxT = self.load_xT(x_all[kb * 128:(kb + 1) * 128, :])
            pk = self.bank[1]
            bpk = self.bbank[1]
            yield
            for c in range(8):
                T(lambda e, c=c: e.matmul(pk[:, 0:304], lhsT=xT[:, c, :], rhs=wk[:, c, :], start=(c == 0), stop=(c == 7)),
                  r=[bxT, b_w], w=[bpk])
            s = kb % 2
            A(lambda e: e.activation(out=junkA[:, 0:256], in_=pk[:, 0:256], func=AF.Square, accum_out=ss[:, 0:1]), r=[bpk], w=[b_junkA, b_ss])
            V(lambda e: e.tensor_scalar(out=ss[:, 1:2], in0=ss[:, 0:1], scalar1=1.0 / 256.0, scalar2=1e-6, op0=ALU.mult, op1=ALU.add), r=[b_ss], w=[b_ss])
            A(lambda e: e.sqrt(out=ss[:, 2:3], in_=ss[:, 1:2]), r=[b_ss], w=[b_ss])
            V(lambda e: e.reciprocal(out=ss[:, 3:4], in_=ss[:, 2:3]), r=[b_ss], w=[b_ss])
            V(lambda e: e.scalar_tensor_tensor(out=ckvn[s][:], in0=pk[:, 0:256], scalar=ss[:, 3:4], in1=gkv[:], op0=ALU.mult, op1=ALU.mult),
              r=[bpk, b_ss, b_small], w=[b_ckvn[s]])
            self.dma(ckv_d[kb * 128:(kb + 1) * 128, :], ckvn[s][:], [b_ckvn[s]], [b_ckvd[kb]], b_ckvd[kb], eng="gpsimd")
            cosA = TABA[:, kb, 0:8]
            sinA = TABA[:, kb, 8:16]
            cosI = TABA[:, kb, 16:20]
            sinI = TABA[:, kb, 20:24]
            self.rope(krr[:, 0:8], krr[:, 8:16], pk[:, 256:264], pk[:, 264:272], cosA, sinA, rtmp[:, 0, 0:8], rtmp[:, 1, 0:8],
                      [bpk, b_taba], [b_kr], b_rtmp)
            V(lambda e: e.tensor_copy(out=krrep[:], in_=krr[:].unsqueeze(1).to_broadcast([128, 8, 16])), r=[b_kr], w=[b_kr])
            self.rope(ikr[:, 0:4], ikr[:, 4:8], pk[:, 272:276], pk[:, 276:280], cosI, sinI, rtmp[:, 0, 0:4], rtmp[:, 1, 0:4],
                      [bpk, b_taba], [b_kr], b_rtmp)
            V(lambda e: e.tensor_copy(out=ikr[:, 8:32], in_=pk[:, 280:304]), r=[bpk], w=[b_kr])
            bv = self.bview(2)
            bb2 = self.bbank[2]
            yield
            for c in range(2):
                T(lambda e, c=c: e.transpose(out=bv[:, c * 128:(c + 1) * 128], in_=ckvn[s][:, c * 128:(c + 1) * 128], identity=self.identb[:]),
                  r=[b_ckvn[s], self.b_const], w=[bb2])
            yield
            T(lambda e: e.transpose(out=bv[:, 256:384], in_=krrep[:].rearrange("p h d -> p (h d)"), identity=self.identb[:]),
              r=[b_kr, self.b_const], w=[bb2])
            T(lambda e: e.transpose(out=bv[0:32, 384:512], in_=ikr[:], identity=self.identb[:]), r=[b_kr, self.b_const], w=[bb2])
            ksl = slice(kb * 128, (kb + 1) * 128)
            A(lambda e: e.activation(out=CKVT[:, :, ksl], in_=bv[:, 0:256].rearrange("p (c t) -> p c t", c=2), func=AF.Copy), r=[bb2], w=[b_kside[kb]])
            V(lambda e: e.tensor_copy(out=KRT[:, ksl], in_=bv[:, 256:384]), r=[bb2], w=[b_kside[kb]])
            V(lambda e: e.tensor_copy(out=IKT[0:32, ksl], in_=bv[0:32, 384:512]), r=[bb2], w=[b_kside[kb]])

        aqn = sb("aqn", [128, 8, 48], BF16)
        aqrp = sb("aqrp", [128, 8, 16], BF16)
        b_aq = Buf("aq")
        aqnT = sb("aqnT", [48, 8, 128], BF16)
        b_aqnT = Buf("aqnT")
        Qm2 = [sb("Qm%d" % k, [128, 8, 128], BF16) for k in range(2)]
        b_qm2 = [Buf("Qm%d" % k) for k in range(2)]
        QLT2 = [sb("QLT%d" % k, [128, 2, 8, 128], BF16) for k in range(2)]
        b_qlt2 = [Buf("QLT%d" % k) for k in range(2)]
        iqr = sb("iqr", [128, 8, 32], BF16)
        b_iq = Buf("iq")
        IQT = sb("IQT", [128, 8, 128], BF16)
        b_iqt = Buf("IQT")
        b_zpad = Buf("zpad")
        G(lambda e: e.memset(IQT[:], 0.0), w=[b_iqt, b_zpad])
        for q4 in range(4):
            G(lambda e, q4=q4: e.memset(IKT[:, q4 * 2048:(q4 + 1) * 2048], 0.0), w=[b_zpad] + [b_kside[k] for k in range(q4 * 16, (q4 + 1) * 16)])
        tbf = [sb("tbf%d" % k, [128, 512], BF16) for k in range(3)]
        b_tbf = [Buf("tbf%d" % k) for k in range(3)]
        wq16 = sb("wq16", [128, 8], F32)
        DGW = sb("DGW", [128, 8, 128], BF16)
        b_dgw = Buf("DGW")
        negi = sb("negi", [128, 128], BF16)
        V(lambda e: e.tensor_scalar(out=negi[:], in0=self.identf[:], scalar1=-30000.0, scalar2=None, op0=ALU.mult), r=[self.b_const], w=[self.b_const])
        junkA = sb("junkA", [128, 1024], BF16)
        b_junkA = Buf("junkA")
        bis2 = sb("bis2", [128, 8], F32)
        b_bis2 = Buf("bis2")
        bis = sb("bis", [128, 8], F32)
        b_bis = Buf("bis")
        DG = sb("DG", [128, 128], F32)
        THRB = sb("THRB", [128, 128], F32)
        b_thr = Buf("thr")
        mk1, b_mk1 = DG, b_thr
        PT = [sb("PT%d" % k, [128, 512], BF16) for k in range(3)]
        b_pt = [Buf("PT%d" % k) for k in range(3)]
        CKVs = [sb("CKVs%d" % k, [128, 8, 256], BF16) for k in range(2)]
        b_ckvs = [Buf("CKVs%d" % k) for k in range(2)]
        OT = sb("OT", [128, 2, 512], BF16)
        b_ot = Buf("OT")
        denr = sb("denr", [1, 512], F32)
        b_denr = Buf("denr")
        rden = sb("rden", [128, 4], F32)
        b_rden = Buf("rden")
        AOUT = [sb("AOUT0", [128, 512], BF16)] * 2
        b_aout = [Buf("AOUT0")] * 2
        self.cnt_ckvs = 0
        self.cnt_pt = 0
        self.cnt_tb = 0

        def qside(i):
            xT, bxT = self.load_xT(x_own[i * 128:(i + 1) * 128, :])
            Qm, b_qm, QLT, b_qlt = Qm2[i % 2], b_qm2[i % 2], QLT2[i % 2], b_qlt2[i % 2]
            p1, bp1 = self.bank[1], self.bbank[1]
            p3, bp3 = self.bank[0], self.bbank[0]
            yield
            for c in range(8):
                T(lambda e, c=c: e.matmul(p1[:, 0:512], lhsT=xT[:, c, :], rhs=wq[:, c, 0:512], start=(c == 0), stop=(c == 7)),
                  r=[bxT, b_w], w=[bp1])
            for c in range(8):
                T(lambda e, c=c: e.matmul(p3[:, 0:264], lhsT=xT[:, c, :], rhs=wq[:, c, 512:776], start=(c == 0), stop=(c == 7)),
                  r=[bxT, b_w], w=[bp3])
            qs = ""
            p1v = p1[:, 0:512].rearrange("p (h d) -> p h d", h=8)
            cosA = TABO[:, i, 0:8].unsqueeze(1).to_broadcast([128, 8, 8])
            sinA = TABO[:, i, 8:16].unsqueeze(1).to_broadcast([128, 8, 8])
            cosI = TABO[:, i, 16:20].unsqueeze(1).to_broadcast([128, 8, 4])
            sinI = TABO[:, i, 20:24].unsqueeze(1).to_broadcast([128, 8, 4])
            rt0 = rtmp[:, 0, 0:64].rearrange("p (h d) -> p h d", h=8)
            rt1 = rtmp[:, 1, 0:64].rearrange("p (h d) -> p h d", h=8)
            if qs != "q0b_norope" and qs != "q0b_none":
                self.rope(aqrp[:, :, 0:8], aqrp[:, :, 8:16], p1v[:, :, 0:8], p1v[:, :, 8:16], cosA, sinA, rt0, rt1, [bp1, b_tabo], [b_aq], b_rtmp)
            if qs != "q0b_nocopy" and qs != "q0b_none":
                A(lambda e: e.activation(out=aqn[:], in_=p1v[:, :, 16:64], func=AF.Copy), r=[bp1], w=[b_aq])
            if qs.startswith("q0b_"):
                return
            if qs == "q0b":
                return
            p3v = p3[:, 0:256].rearrange("p (h d) -> p h d", h=8)
            rt0i = rtmp[:, 0, 0:32].rearrange("p (h d) -> p h d", h=8)
            rt1i = rtmp[:, 1, 0:32].rearrange("p (h d) -> p h d", h=8)
            A(lambda e: e.activation(out=iqr[:, :, 8:32], in_=p3v[:, :, 8:32], func=AF.Copy), r=[bp3], w=[b_iq])
            self.rope(iqr[:, :, 0:4], iqr[:, :, 4:8], p3v[:, :, 0:4], p3v[:, :, 4:8], cosI, sinI, rt0i, rt1i, [bp3, b_tabo], [b_iq], b_rtmp)
            if qs == "q0c":
                return
            V(lambda e: e.tensor_scalar(out=wq16[:], in0=p3[:, 256:264], scalar1=1.0 / 16.0, scalar2=None, op0=ALU.mult), r=[bp3], w=[b_dgw])
            V(lambda e: e.tensor_tensor(out=DGW[:], in0=self.identf[:].unsqueeze(1).to_broadcast([128, 8, 128]),
                                        in1=wq16[:].unsqueeze(2).to_broadcast([128, 8, 128]), op=ALU.mult), r=[self.b_const], w=[b_dgw])
            if qs == "q1":
                return
            bv = self.bview(2)
            bb2 = self.bbank[2]
            yield
            for h in range(8):
                T(lambda e, h=h: e.transpose(out=bv[0:48, h * 128:(h + 1) * 128], in_=aqn[:, h, :], identity=self.identb[:]),
                  r=[b_aq, self.b_const], w=[bb2])
            A(lambda e: e.activation(out=aqnT[:], in_=bv[0:48, :].rearrange("p (h t) -> p h t", h=8), func=AF.Copy), r=[bb2], w=[b_aqnT])
            if qs == "q2":
                return
            yield
            T(lambda e: e.transpose(out=bv[:, 0:128], in_=aqrp[:].rearrange("p h d -> p (h d)"), identity=self.identb[:]),
              r=[b_aq, self.b_const, b_aqnT], w=[bb2])
            V(lambda e: e.tensor_tensor(out=Qm[:], in0=bv[:, 0:128].unsqueeze(1).to_broadcast([128, 8, 128]),
                                        in1=bm8[:].unsqueeze(2).to_broadcast([128, 8, 128]), op=ALU.mult),
              r=[bb2, b_small], w=[b_qm])
            if qs == "q3":
                return
            for h in range(8):
                T(lambda e, h=h: e.transpose(out=bv[0:32, h * 128:(h + 1) * 128], in_=iqr[:, h, :], identity=self.identb[:]),
                  r=[b_iq, self.b_const, b_qm], w=[bb2])
            A(lambda e: e.activation(out=IQT[0:32, :, :], in_=bv[0:32, :].rearrange("p (h t) -> p h t", h=8), func=AF.Copy), r=[bb2], w=[b_iqt])
            if qs == "q4":
                return
            yield
            for c in range(2):
                for hg in range(2):
                    bk = hg
                    for hh in range(4):
                        h = hg * 4 + hh
                        T(lambda e, c=c, h=h, hh=hh, bk=bk: e.matmul(self.bank[bk][:, hh * 128:(hh + 1) * 128],
                                                                     lhsT=wuk[:, h, c * 128:(c + 1) * 128], rhs=aqnT[:, h, :],
                                                                     start=True, stop=True),
                          r=[b_w, b_aqnT], w=[self.bbank[bk]])
                    A(lambda e, c=c, hg=hg, bk=bk: e.activation(out=QLT[:, c, hg * 4:(hg + 1) * 4, :],
                                                                in_=self.bank[bk][:].rearrange("p (h t) -> p h t", h=4),
                                                                func=AF.Copy, scale=0.125),
                      r=[self.bbank[bk]], w=[b_qlt])

        def score_and_threshold(i):
            nkb = 2 * i + 2
            nk = nkb * 128
            nch = (nk + 511) // 512
            for j in range(nch):
                wj = min(512, nk - 512 * j)
                sc = SCORE[:, 512 * j:512 * j + wj]
                abk = 2
                pacc, bpacc = self.bank[abk], self.bbank[abk]
                kbufs = [b_kside[k] for k in range(4 * j, 4 * j + wj // 128)]
                tt = {}

                def s_stage(h):
                    bk = 1 if (h % 2 == 0) else 0
                    pb, bpb = self.bank[bk], self.bbank[bk]
                    T(lambda e, pb=pb, h=h, wj=wj, j=j: e.matmul(pb[:, 0:wj], lhsT=IQT[:, h, :], rhs=IKT[:, 512 * j:512 * j + wj], start=True, stop=True),
                      r=[b_iqt] + kbufs, w=[bpb])
                    t = self.cnt_tb % 3
                    self.cnt_tb += 1
                    tt[h] = t
                    if h % 2 == 0:
                        A(lambda e, pb=pb, t=t, wj=wj: e.activation(out=tbf[t][:, 0:wj], in_=pb[:, 0:wj], func=AF.Relu), r=[bpb], w=[b_tbf[t]])
                    else:
                        V(lambda e, pb=pb, t=t, wj=wj: e.tensor_scalar(out=tbf[t][:, 0:wj], in0=pb[:, 0:wj], scalar1=0.0, scalar2=None, op0=ALU.max),
                          r=[bpb], w=[b_tbf[t]])

                def a_stage(h):
                    t = tt[h]
                    T(lambda e, h=h, t=t, wj=wj, pacc=pacc: e.matmul(pacc[:, 0:wj], lhsT=DGW[:, h, :], rhs=tbf[t][:, 0:wj], start=(h == 0), stop=(h == 7)),
                      r=[b_tbf[t], b_dgw], w=[bpacc])

                s_stage(0)
                for h in range(8):
                    if h + 1 < 8:
                        s_stage(h + 1)
                    a_stage(h)
                V(lambda e, sc=sc, pacc=pacc, wj=wj: e.tensor_copy(out=sc, in_=pacc[:, 0:wj]), r=[bpacc], w=[b_score[j]])
                yield
            allsc = [b_score[j] for j in range(nch)]
            V(lambda e: e.tensor_reduce(out=bis[:, 0:1], in_=SCORE[:, 0:nk], axis=AX.X, op=ALU.min), r=allsc, w=[b_bis])
            for t_, kb in enumerate((2 * i, 2 * i + 1)):
                blk = SCORE[:, kb * 128:(kb + 1) * 128]
                j = kb // 4
                V(lambda e, t_=t_: e.tensor_scalar(out=mk1[:], in0=self.idsrc[:], scalar1=cpar[:, t_:t_ + 1], scalar2=0.0, op0=ALU.add, op1=ALU.is_gt),
                  r=[self.b_const, b_small], w=[b_mk1])
                V(lambda e, blk=blk: e.scalar_tensor_tensor(out=blk, in0=mk1[:], scalar=NEG, in1=blk, op0=ALU.mult, op1=ALU.add),
                  r=[b_mk1], w=[b_score[j]])
            V(lambda e: e.tensor_reduce(out=bis[:, 1:2], in_=SCORE[:, 0:nk], axis=AX.X, op=ALU.max), r=allsc, w=[b_bis])
            V(lambda e: e.tensor_tensor(out=bis[:, 2:3], in0=bis[:, 1:2], in1=bis[:, 0:1], op=ALU.subtract), r=[b_bis], w=[b_bis])
            split = nk
            n2 = nk - split
            na = (n2 + 1023) // 1024
            for it in range(1, N_BISECT + 1):
                f = 2.0 ** (-it)
                V(lambda e, f=f: e.tensor_scalar(out=bis[:, 3:4], in0=bis[:, 2:3], scalar1=f, scalar2=bis[:, 0:1], op0=ALU.mult, op1=ALU.add),
                  r=[b_bis], w=[b_bis])
                if n2 > 0:
                    V(lambda e: e.tensor_scalar(out=bis[:, 6:7], in0=bis[:, 3:4], scalar1=-1.0, scalar2=None, op0=ALU.mult), r=[b_bis], w=[b_bis])
                    for ci in range(na):
                        c0 = split + ci * 1024
                        cw = min(1024, nk - c0)
                        A(lambda e, c0=c0, cw=cw, ci=ci: e.activation(out=junkA[:, 0:cw], in_=SCORE[:, c0:c0 + cw], func=AF.Sign, bias=bis[:, 6:7],
                                                                      accum_out=bis2[:, ci:ci + 1]), r=allsc + [b_bis], w=[b_junkA, b_bis2])
                first = True
                for c0 in range(0, split, 1024):
                    cw = min(1024, split - c0)
                    if first:
                        V(lambda e, c0=c0, cw=cw: e.tensor_scalar(out=junk[:, 0:cw], in0=SCORE[:, c0:c0 + cw], scalar1=bis[:, 3:4], scalar2=None,
                                                                  op0=ALU.is_ge, op1=ALU.add, accum_out=bis[:, 4:5]),
                          r=allsc + [b_bis], w=[b_junk, b_bis])
                    else:
                        V(lambda e, c0=c0, cw=cw: e.tensor_scalar(out=junk[:, 0:cw], in0=SCORE[:, c0:c0 + cw], scalar1=bis[:, 3:4], scalar2=bis[:, 4:5],
                                                                  op0=ALU.is_ge, op1=ALU.add, accum_out=bis[:, 4:5]),
                          r=allsc + [b_bis], w=[b_junk, b_bis])
                    first = False
                thr_cnt = 255.5
                if n2 > 0:
                    V(lambda e: e.tensor_reduce(out=bis[:, 7:8], in_=bis2[:, 0:na], axis=AX.X, op=ALU.add), r=[b_bis2], w=[b_bis])
                    V(lambda e: e.scalar_tensor_tensor(out=bis[:, 4:5], in0=bis[:, 7:8], scalar=0.5, in1=bis[:, 4:5], op0=ALU.mult, op1=ALU.add),
                      r=[b_bis], w=[b_bis])
                    thr_cnt = 255.5 - 0.5 * n2
                V(lambda e, f=f, thr_cnt=thr_cnt: e.tensor_scalar(out=bis[:, 5:6], in0=bis[:, 4:5], scalar1=thr_cnt, scalar2=f, op0=ALU.is_ge, op1=ALU.mult),
                  r=[b_bis], w=[b_bis])
                V(lambda e: e.scalar_tensor_tensor(out=bis[:, 0:1], in0=bis[:, 5:6], scalar=bis[:, 2:3], in1=bis[:, 0:1], op0=ALU.mult, op1=ALU.add),
                  r=[b_bis], w=[b_bis])
                yield

        def masks(i):
            nkb = 2 * i + 2
            V(lambda e: e.tensor_scalar(out=DG[:], in0=self.identf[:], scalar1=bis[:, 0:1], scalar2=None, op0=ALU.mult), r=[b_bis, self.b_const], w=[b_thr])
            T(lambda e: e.matmul(self.bank[2][:, 0:128], lhsT=self.onesf[:], rhs=DG[:], start=True, stop=True), r=[b_thr, self.b_const], w=[self.bbank[2]])
            A(lambda e: e.activation(out=THRB[:], in_=self.bank[2][:, 0:128], func=AF.Copy), r=[self.bbank[2]], w=[b_thr])
            g = 0
            for kb0 in range(0, nkb, 4):
                n = min(4, nkb - kb0)
                bk = 1 if (g % 2 == 0) else 0
                g += 1
                for t_ in range(n):
                    kb = kb0 + t_
                    T(lambda e, bk=bk, t_=t_, kb=kb: e.transpose(out=self.bank[bk][:, t_ * 128:(t_ + 1) * 128], in_=SCORE[:, kb * 128:(kb + 1) * 128],
                                                                 identity=self.identf[:]),
                      r=[b_score[kb // 4], self.b_const], w=[self.bbank[bk]])
                V(lambda e, bk=bk, n=n, kb0=kb0: e.tensor_tensor(out=MT[:, kb0:kb0 + n, :],
                                                                 in0=self.bank[bk][:, 0:n * 128].rearrange("p (k t) -> p k t", k=n),
                                                                 in1=THRB[:].unsqueeze(1).to_broadcast([128, n, 128]), op=ALU.is_lt),
                  r=[self.bbank[bk], b_thr], w=[b_mt])

        def attention(i):
            nkb = 2 * i + 2
            ao, bao = AOUT[i % 2], b_aout[i % 2]
            Qm, b_qm, QLT, b_qlt = Qm2[i % 2], b_qm2[i % 2], QLT2[i % 2], b_qlt2[i % 2]
            for hg in range(2):
                p_o = [self.bank[4], self.bank[5]]
                bp_o = [self.bbank[4], self.bbank[5]]
                p_d, bp_d = self.bank[3], self.bbank[3]
                hs = slice(hg * 4, (hg + 1) * 4)
                state = {}

                def S_stage(kb):
                    if kb % 8 == 0:
                        s_ = self.cnt_ckvs % 2
                        self.cnt_ckvs += 1
                        n8 = min(8, nkb - kb)
                        self.dma(CKVs[s_][:, 0:n8, :], ckv_d[kb * 128:(kb + n8) * 128, :].rearrange("(k p) r -> p k r", p=128),
                                 [b_ckvd[k] for k in range(kb, kb + n8)], [b_ckvs[s_]], b_ckvs[s_])
                        state["cur%d" % (kb // 8)] = s_
                    pk = 6 + (kb % 2)
                    pst, bpst = self.bank[pk], self.bbank[pk]
                    ksl = slice(kb * 128, (kb + 1) * 128)
                    T(lambda e, pst=pst, ksl=ksl, hs=hs: e.matmul(pst[:], lhsT=CKVT[:, 0, ksl], rhs=QLT[:, 0, hs, :].rearrange("p h t -> p (h t)"),
                                                           start=True, stop=False), r=[b_kside[kb], b_qlt], w=[bpst])
                    T(lambda e, pst=pst, ksl=ksl, hs=hs: e.matmul(pst[:], lhsT=CKVT[:, 1, ksl], rhs=QLT[:, 1, hs, :].rearrange("p h t -> p (h t)"),
                                                           start=False, stop=False), r=[b_kside[kb], b_qlt], w=[bpst])
                    T(lambda e, pst=pst, ksl=ksl, hs=hs: e.matmul(pst[:], lhsT=KRT[:, ksl], rhs=Qm[:, hs, :].rearrange("p h t -> p (h t)"),
                                                           start=False, stop=False), r=[b_kside[kb], b_qm], w=[bpst])
                    T(lambda e, pst=pst, kb=kb: e.matmul(pst[:].rearrange("p (h t) -> p h t", h=4), lhsT=negi[:],
                                                         rhs=MT[:, kb, :].unsqueeze(1).to_broadcast([128, 4, 128]), start=False, stop=True),
                      r=[b_mt, self.b_const], w=[bpst])
                    t = self.cnt_pt % 3
                    self.cnt_pt += 1
                    state["t%d" % kb] = t
                    A(lambda e, pst=pst, t=t: e.activation(out=PT[t][:], in_=pst[:], func=AF.Exp), r=[bpst], w=[b_pt[t]])

                def O_stage(kb):
                    t = state["t%d" % kb]
                    cur = state["cur%d" % (kb // 8)]
                    kk = kb % 8
                    for c in range(2):
                        T(lambda e, c=c, t=t, kk=kk, cur=cur, kb=kb: e.matmul(p_o[c][:], lhsT=CKVs[cur][:, kk, c * 128:(c + 1) * 128], rhs=PT[t][:],
                                                                              start=(kb == 0), stop=(kb == nkb - 1)),
                          r=[b_ckvs[cur], b_pt[t]], w=[bp_o[c]])
                    T(lambda e, t=t, kb=kb: e.matmul(p_d[0:1, :], lhsT=self.onesb[:, 0:1], rhs=PT[t][:], start=(kb == 0), stop=(kb == nkb - 1)),
                      r=[b_pt[t], self.b_const], w=[bp_d])

                S_stage(0)
                for kb in range(nkb):
                    if kb + 1 < nkb:
                        S_stage(kb + 1)
                    O_stage(kb)
                    yield
                A(lambda e: e.activation(out=denr[:], in_=p_d[0:1, :], func=AF.Copy), r=[bp_d], w=[b_denr])
                A(lambda e: e.activation(out=OT[:, 0, :], in_=p_o[0][:], func=AF.Copy), r=[bp_o[0]], w=[b_ot])
                A(lambda e: e.activation(out=OT[:, 1, :], in_=p_o[1][:], func=AF.Copy), r=[bp_o[1]], w=[b_ot])
                p3, bp3 = self.bank[6], self.bbank[6]
                for hh in range(4):
                    T(lambda e, hh=hh: e.matmul(p3[:, hh:hh + 1], lhsT=denr[0:1, hh * 128:(hh + 1) * 128], rhs=self.onesf[0:1, 0:1], start=True, stop=True),
                      r=[b_denr, self.b_const], w=[bp3])
                A(lambda e: e.activation(out=rden[:], in_=p3[:, 0:4], func=AF.Ln), r=[bp3], w=[b_rden])
                A(lambda e: e.activation(out=rden[:], in_=rden[:], func=AF.Exp, scale=-1.0), r=[b_rden], w=[b_rden])
                p1, bp1 = self.bank[7], self.bbank[7]
                for hh in range(4):
                    h = hg * 4 + hh
                    for c in range(2):
                        T(lambda e, hh=hh, h=h, c=c: e.matmul(p1[:, hh * 64:(hh + 1) * 64], lhsT=OT[:, c, hh * 128:(hh + 1) * 128],
                                                              rhs=wuv[:, c, h * 64:(h + 1) * 64], start=(c == 0), stop=(c == 1)),
                          r=[b_ot, b_w], w=[bp1])
                for hh in range(4):
                    A(lambda e, hg=hg, hh=hh: e.activation(out=ao[:, hg * 256 + hh * 64:hg * 256 + (hh + 1) * 64], in_=p1[:, hh * 64:(hh + 1) * 64],
                                                           func=AF.Copy, scale=rden[:, hh:hh + 1]), r=[bp1, b_rden], w=[bao])
                yield
            self.dma(self.aout_d[i * 128:(i + 1) * 128, :], ao[:], [bao], [self.b_aoutd], self.b_aoutd, eng="gpsimd")

        def stage_a(i):
            yield from qside(i)
            yield
            gen = score_and_threshold(i)
            nch_ = ((2 * i + 2) * 128 + 511) // 512
            for _ in range(nch_):
                next(gen)
                yield
            if 2 * i + 2 < 2 * n_own:
                yield from kside(2 * i + 2)
                yield
                yield from kside(2 * i + 3)
                yield
            else:
                for _ in range(8):
                    yield
            yield from gen

        def n_units_a(i):
            nk = (2 * i + 2) * 128
            return 13 + (nk + 511) // 512 + N_BISECT

        def n_units_b(i):
            return 2 * (2 * i + 2) + 2

        for kb0 in (0, 1):
            for _ in kside(kb0):
                pass
        for _ in stage_a(0):
            pass
        masks(0)
        for i in range(n_own):
            gb = attention(i)
            if i + 1 < n_own:
                ga = stage_a(i + 1)
                nb = n_units_b(i)
                na_front = n_units_a(i + 1) - N_BISECT
                done_a = done_b = False
                ca = cb = 0
                while not (done_a and done_b):
                    if not done_a and ca >= na_front:
                        for _ in ga:
                            pass
                        done_a = True
                    elif not done_a and (done_b or ca * nb * 0.25 <= cb * na_front):
                        try:
                            next(ga)
                        except StopIteration:
                            done_a = True
                        ca += 1
                    else:
                        try:
                            next(gb)
                        except StopIteration:
                            done_b = True
                        cb += 1
                masks(i + 1)
            else:
                for _ in gb:
                    pass
        self.mark = mark

    def alloc_xload(self):
        sb = self.sb
        self.xf = [sb("xf%d" % k, [128, D], F32) for k in range(2)]
        self.xT = [sb("xT%d" % k, [128, 8, 128], BF16) for k in range(2)]
        self.b_xf = [Buf("xf%d" % k) for k in range(2)]
        self.b_xT = [Buf("xT%d" % k) for k in range(2)]

    def phase1b(self):
        V, A, G, T = self.V, self.A, self.G, self.T
        sb = self.sb
        n_own = self.n_own
        x_all, x_own = self.x_all, self.x_own
        TABA, TABO, b_taba, b_tabo = self.TABA, self.TABO, self.b_taba, self.b_tabo
        cpar, b_small = self.cpar, self.b_small
        wbq_d = self.dram_in("w_bq", [D, 768], F32)
        wbk_d = self.dram_in("w_bk", [D, 768], F32)
        wbv_d = self.dram_in("w_bv", [D, 768], F32)
        if self.debug:
            self.bout_d = self.dram_out("bout_d", [NOWN * 128, 256], BF16)
        else:
            self.bout_d = self.dram_tmp("bout_d", [NOWN * 128, 256], BF16)
        self.b_boutd = Buf("boutd")
        RING = 20
        wbq = sb("wbq", [128, 8, 768], BF16)
        wbk = sb("wbk", [128, 8, 768], BF16)
        wbv = sb("wbv", [128, 8, 768], BF16)
        b_w = Buf("wB")
        stg = [sb("stgB%d" % k, [128, 8, 512], F32) for k in range(2)]
        b_stg = [Buf("stgB%d" % k) for k in range(2)]
        self.alloc_xload()
        seq = []
        for i_ in range(n_own):
            seq += [x_all[(2 * i_) * 128:(2 * i_ + 1) * 128, :], x_all[(2 * i_ + 1) * 128:(2 * i_ + 2) * 128, :], x_own[i_ * 128:(i_ + 1) * 128, :]]
        self.set_x_sequence(seq)
        BKT = sb("BKT", [128, 6, RING, 128], BF16)
        BV = sb("BV", [128, RING, 12, 65], BF16)
        b_slot = [Buf("bslot%d" % k) for k in range(RING)]
        bkr = sb("bkr", [128, 12, 64], BF16)
        b_bkr = Buf("bkr")
        bqr = sb("bqr", [128, 12, 64], BF16)
        b_bqr = Buf("bqr")
        BQT2 = [sb("BQT%d" % k, [128, 2, 6, 128], BF16) for k in range(2)]
        b_bqt2 = [Buf("BQT%d" % k) for k in range(2)]
        RMs = sb("RMs", [128, 4], F32)
        b_rm = Buf("RMs")
        G(lambda e: e.iota(RMs[:, 2:3], pattern=[[0, 1]], base=0, channel_multiplier=1, allow_small_or_imprecise_dtypes=True), w=[b_rm])
        V(lambda e: e.tensor_scalar(out=RMs[:, 0:1], in0=RMs[:, 2:3], scalar1=63.5, scalar2=0.125, op0=ALU.is_lt, op1=ALU.mult), r=[b_rm], w=[b_rm])
        V(lambda e: e.tensor_scalar(out=RMs[:, 1:2], in0=RMs[:, 2:3], scalar1=63.5, scalar2=0.125, op0=ALU.is_gt, op1=ALU.mult), r=[b_rm], w=[b_rm])
        rtmp = sb("rtmpB", [128, 2, 64], F32)
        b_rtmp = Buf("rtmpB")
        MD = sb("MD", [128, 3, 128], F32)
        MDb = sb("MDb", [128, 3, 128], BF16)
        b_md = Buf("MD")
        vf = sb("vf", [128, 128], F32)
        vf2 = sb("vf2", [128, 128], F32)
        vi = sb("vi", [128, 128], I32)
        dl = sb("dl", [128, 128], F32)
        m1 = sb("m1", [128, 128], F32)
        b_dl = Buf("dl")
        MK = [sb("MK%d" % k, [128, 128], BF16) for k in range(4)]
        b_mk = [Buf("MK%d" % k) for k in range(4)]
        PT = [sb("PTb%d" % k, [128, 512], BF16) for k in range(2)]
        b_pt = [Buf("PTb%d" % k) for k in range(2)]
        PTm = [sb("PTmb%d" % k, [128, 512], BF16) for k in range(2)]
        b_ptm = [Buf("PTmb%d" % k) for k in range(2)]
        bout = sb("bout", [128, 256], BF16)
        b_bout = Buf("bout")
        rden = sb("rdenB", [128, 4], F32)
        b_rden = Buf("rdenB")

        self.load_weight_bf16(wbq, wbq_d, 768, b_w, stg, b_stg)
        self.load_weight_bf16(wbk, wbk_d, 768, b_w, stg, b_stg)
        self.load_weight_bf16(wbv, wbv_d, 768, b_w, stg, b_stg)
        G(lambda e: e.memset(BV[:, :, :, 64:65], 1.0), w=b_slot)
        V(lambda e: e.memset(MD[:, 0, :], 1.0), w=[b_md])
        for g, dil in ((1, 4), (2, 16)):
            V(lambda e, dil=dil: e.tensor_scalar(out=vf[:], in0=self.idsrc[:], scalar1=128.0, scalar2=1.0 / dil, op0=ALU.add, op1=ALU.mult),
              r=[self.b_const], w=[b_dl])
            V(lambda e: e.tensor_copy(out=vi[:], in_=vf[:]), w=[b_dl])
            V(lambda e: e.tensor_copy(out=vf2[:], in_=vi[:]), w=[b_dl])
            V(lambda e, g=g: e.tensor_tensor(out=MD[:, g, :], in0=vf[:], in1=vf2[:], op=ALU.is_equal), r=[b_dl], w=[b_md])
        V(lambda e: e.tensor_copy(out=MDb[:], in_=MD[:]), r=[b_md], w=[b_md])

        def proj768(xT, bxT, w, banks):
            for (bk, c0, cw) in ((banks[0], 0, 512), (banks[1], 512, 256)):
                for c in range(8):
                    T(lambda e, bk=bk, c=c, c0=c0, cw=cw: e.matmul(self.bank[bk][:, 0:cw], lhsT=xT[:, c, :], rhs=w[:, c, c0:c0 + cw],
                                                                   start=(c == 0), stop=(c == 7)),
                      r=[bxT, b_w], w=[self.bbank[bk]])

        def rope_heads(dst, banks, tab, blk, b_tab, b_dst):
            for (bk, hs, nh) in ((banks[0], 0, 8), (banks[1], 8, 4)):
                pv = self.bank[bk][:, 0:nh * 64].rearrange("p (h d) -> p h d", h=nh)
                cos = tab[:, blk, 0:8].unsqueeze(1).to_broadcast([128, nh, 8])
                sin = tab[:, blk, 8:16].unsqueeze(1).to_broadcast([128, nh, 8])
                tA = rtmp[:, 0, 0:nh * 8].rearrange("p (h d) -> p h d", h=nh)
                tB = rtmp[:, 1, 0:nh * 8].rearrange("p (h d) -> p h d", h=nh)
                self.rope(dst[:, hs:hs + nh, 0:8], dst[:, hs:hs + nh, 8:16], pv[:, :, 0:8], pv[:, :, 8:16], cos, sin, tA, tB,
                          [self.bbank[bk], b_tab], [b_dst], b_rtmp)
                A(lambda e, pv=pv, hs=hs, nh=nh: e.activation(out=dst[:, hs:hs + nh, 16:64], in_=pv[:, :, 16:64], func=AF.Copy),
                  r=[self.bbank[bk]], w=[b_dst])

        def kside(kb):
            slot = kb % RING
            xT, bxT = self.load_xT(x_all[kb * 128:(kb + 1) * 128, :])
            proj768(xT, bxT, wbk, (1, 3))
            rope_heads(bkr, (1, 3), TABA, kb, b_taba, b_bkr)
            bv2 = self.bview(2)
            for c in range(6):
                T(lambda e, c=c: e.transpose(out=bv2[:, c * 128:(c + 1) * 128], in_=bkr[:, 2 * c:2 * c + 2, :].rearrange("p h d -> p (h d)"),
                                             identity=self.identb[:]), r=[b_bkr, self.b_const], w=[self.bbank[2]])
            V(lambda e: e.tensor_copy(out=BKT[:, :, slot, :], in_=bv2[:, 0:768].rearrange("p (c t) -> p c t", c=6)),
              r=[self.bbank[2]], w=[b_slot[slot]])
            proj768(xT, bxT, wbv, (4, 5))
            A(lambda e: e.activation(out=BV[:, slot, 0:8, 0:64], in_=self.bank[4][:, 0:512].rearrange("p (h d) -> p h d", h=8), func=AF.Copy),
              r=[self.bbank[4]], w=[b_slot[slot]])
            V(lambda e: e.tensor_copy(out=BV[:, slot, 8:12, 0:64], in_=self.bank[5][:, 0:256].rearrange("p (h d) -> p h d", h=4)),
              r=[self.bbank[5]], w=[b_slot[slot]])

        def qside(i):
            xT, bxT = self.load_xT(x_own[i * 128:(i + 1) * 128, :])
            proj768(xT, bxT, wbq, (1, 3))
            rope_heads(bqr, (1, 3), TABO, i, b_tabo, b_bqr)
            bv2 = self.bview(2)
            for c in range(6):
                T(lambda e, c=c: e.transpose(out=bv2[:, c * 128:(c + 1) * 128], in_=bqr[:, 2 * c:2 * c + 2, :].rearrange("p h d -> p (h d)"),
                                             identity=self.identb[:]), r=[b_bqr, self.b_const], w=[self.bbank[2]])
            A(lambda e: e.activation(out=BQT2[i % 2][:, 0, :, :], in_=bv2[:, 0:768].rearrange("p (c t) -> p c t", c=6), func=AF.Copy, scale=RMs[:, 0:1]),
              r=[self.bbank[2], b_rm], w=[b_bqt2[i % 2]])
            V(lambda e: e.tensor_scalar(out=BQT2[i % 2][:, 1, :, :], in0=bv2[:, 0:768].rearrange("p (c t) -> p c t", c=6), scalar1=RMs[:, 1:2], scalar2=None,
                                        op0=ALU.mult), r=[self.bbank[2], b_rm], w=[b_bqt2[i % 2]])

        self.cnt_b = 0
        self.cnt_mk = 0
        WIN = (128.0, 512.0, 2048.0)
        WB = (1, 4, 16)

        def attention(i):
            pairs = []
            for g in range(3):
                for kb in range(2 * i - WB[g], 2 * i + 2):
                    if kb >= 0:
                        pairs.append((g, kb))
            p_o, bp_o = self.bank[6], self.bbank[6]
            npairs = len(pairs)
            tsel = {}
            BQT, b_bqt = BQT2[i % 2], b_bqt2[i % 2]

            def qk_stage(n):
                g, kb = pairs[n]
                slot = kb % RING
                t = self.cnt_b % 2
                self.cnt_b += 1
                tsel[n] = t
                pk = 7 if t == 0 else 0
                for hh in range(4):
                    h = 4 * g + hh
                    c, z = h // 2, h % 2
                    T(lambda e, pk=pk, hh=hh, c=c, z=z, slot=slot: e.matmul(self.bank[pk][:, hh * 128:(hh + 1) * 128], lhsT=BKT[:, c, slot, :],
                                                                            rhs=BQT[:, z, c, :], start=True, stop=True),
                      r=[b_slot[slot], b_bqt], w=[self.bbank[pk]])
                A(lambda e, t=t, pk=pk: e.activation(out=PT[t][:], in_=self.bank[pk][:], func=AF.Exp), r=[self.bbank[pk]], w=[b_pt[t]])
                d_rel = kb - 2 * i
                interior = (g == 1 and d_rel in (-2, -1)) or (g == 2 and -14 <= d_rel <= -1)
                if interior:
                    mask = MDb[:, g, :]
                    rmask = [b_md]
                else:
                    m = self.cnt_mk % 4
                    self.cnt_mk += 1
                    V(lambda e, d_rel=d_rel: e.tensor_scalar(out=dl[:], in0=self.idsrc[:], scalar1=cpar[:, 2:3], scalar2=-128.0 * d_rel,
                                                             op0=ALU.add, op1=ALU.add), r=[self.b_const, b_small], w=[b_dl])
                    V(lambda e, g=g: e.scalar_tensor_tensor(out=m1[:], in0=dl[:], scalar=0.0, in1=MD[:, g, :], op0=ALU.is_ge, op1=ALU.mult),
                      r=[b_md], w=[b_dl])
                    V(lambda e, g=g, m=m: e.scalar_tensor_tensor(out=MK[m][:], in0=dl[:], scalar=WIN[g], in1=m1[:], op0=ALU.is_le, op1=ALU.mult),
                      r=[b_dl], w=[b_mk[m]])
                    mask = MK[m][:]
                    rmask = [b_mk[m]]
                eng = V
                eng(lambda e, t=t, mask=mask: e.tensor_tensor(out=PTm[t][:].rearrange("p (h t) -> p h t", h=4),
                                                              in0=PT[t][:].rearrange("p (h t) -> p h t", h=4),
                                                              in1=mask.unsqueeze(1).to_broadcast([128, 4, 128]), op=ALU.mult),
                    r=[b_pt[t]] + rmask, w=[b_ptm[t]])

            def pv_stage(n):
                g, kb = pairs[n]
                slot = kb % RING
                t = tsel[n]
                for hh in range(4):
                    ci = hh
                    T(lambda e, t=t, hh=hh, slot=slot, g=g, n=n, ci=ci: e.matmul(p_o[:, hh * 65:(hh + 1) * 65], lhsT=PTm[t][:, ci * 128:(ci + 1) * 128],
                                                                          rhs=BV[:, slot, 4 * g + hh, :], start=(n == 0 and hh == 0),
                                                                          stop=(n == npairs - 1 and hh == 3), skip_group_check=True),
                      r=[b_ptm[t], b_slot[slot]], w=[bp_o])

            qk_stage(0)
            for n in range(npairs):
                if n + 1 < npairs:
                    qk_stage(n + 1)
                pv_stage(n)
                yield
            if st1b in ("attn_qk", "attn_mask", "attn_pv"):
                return
            pov = p_o[:, 0:260].rearrange("p (h d) -> p h d", h=4)
            V(lambda e: e.reciprocal(out=rden[:].unsqueeze(2), in_=pov[:, :, 64:65]), r=[bp_o], w=[b_rden])
            V(lambda e: e.tensor_tensor(out=bout[:].rearrange("p (h d) -> p h d", h=4), in0=pov[:, :, 0:64],
                                        in1=rden[:].unsqueeze(2).to_broadcast([128, 4, 64]), op=ALU.mult), r=[bp_o, b_rden], w=[b_bout])
            self.dma(self.bout_d[i * 128:(i + 1) * 128, :], bout[:], [b_bout], [self.b_boutd], self.b_boutd, eng="gpsimd")

        st1b = ""

        def stage_a(i):
            kside(2 * i)
            yield
            kside(2 * i + 1)
            yield
            qside(i)
            yield

        for _ in stage_a(0):
            pass
        for i in range(n_own):
            gb = attention(i)
            if i + 1 < n_own:
                ga = stage_a(i + 1)
                nb = sum(1 for g in range(3) for kb in range(2 * i - WB[g], 2 * i + 2) if kb >= 0) + 1
                na = 3
                done_a = done_b = False
                ca = cb = 0
                while not (done_a and done_b):
                    if not done_a and (done_b or ca * nb <= cb * na):
                        try:
                            next(ga)
                        except StopIteration:
                            done_a = True
                        ca += 1
                    else:
                        try:
                            next(gb)
                        except StopIteration:
                            done_b = True
                        cb += 1
            else:
                for _ in gb:
                    pass

    def load_bcast(self, dst, src_row_ap, b_dst):
        self.dma(dst, src_row_ap.partition_broadcast(128).rearrange("p o n -> p (o n)"), [], [b_dst], b_dst)

    def phase2(self):
        V, A, G, T = self.V, self.A, self.G, self.T
        sb = self.sb
        n_own = self.n_own
        C = self.CAP
        NSLOT = 64 * C
        ALPHA = 2.0 ** 0.25
        di = self.dram_in
        wga_d, wgb_d = di("w_ga", [D, D], F32), di("w_gb", [D, D], F32)
        bgate_d = di("b_gate", [1, 2 * D], F32)
        wba_d, wbb_d, wo_d = di("w_branch_a", [512, D], F32), di("w_branch_b", [256, D], F32), di("w_o", [D, D], F32)
        ln1g_d, ln1b_d = di("ln1_g", [1, D], F32), di("ln1_b", [1, D], F32)
        wr_d, rb_d = di("w_router", [D, 64], F32), di("router_bias", [1, 64], F32)
        ws1_d, ws3_d, ws2_d = di("ws1", [D, 256], F32), di("ws3", [D, 256], F32), di("ws2", [256, D], F32)
        self.base_d = self.dram_tmp("base_d", [NOWN * 128, D], F32)
        self.b_based = Buf("based")
        if self.debug:
            self.h_d = self.dram_out("h_d", [NOWN * 128, D], F32)
            self.b_hd = Buf("hd")

        wga, wgb = sb("wga", [128, 8, D], BF16), sb("wgb", [128, 8, D], BF16)
        wba, wbb, wo = sb("wba", [128, 4, D], BF16), sb("wbb", [128, 2, D], BF16), sb("wo", [128, 8, D], BF16)
        wr = sb("wr", [128, 8, 64], BF16)
        ws1, ws3, ws2 = sb("ws1", [128, 8, 256], BF16), sb("ws3", [128, 8, 256], BF16), sb("ws2", [128, 2, D], BF16)
        b_w = Buf("w2")
        stg = [sb("stg2%d" % k, [128, 8, 512], F32) for k in range(2)]
        b_stg = [Buf("stg2%d" % k) for k in range(2)]
        bgate = sb("bgate", [128, 2 * D], F32)
        ln1g, ln1b = sb("ln1g", [128, D], F32), sb("ln1b", [128, D], F32)
        rbias = sb("rbias", [128, 64], F32)
        b_vec = Buf("vec2")
        self.alloc_xload()
        self.set_x_sequence([self.x_own[i_ * 128:(i_ + 1) * 128, :] for i_ in range(n_own)])
        gate = [sb("gate%d" % k, [128, D], F32) for k in range(2)]
        b_gate = [Buf("gate%d" % k) for k in range(2)]
        gtmp = [sb("gtmp%d" % k, [128, 512], F32) for k in range(2)]
        b_gtmp = [Buf("gtmp%d" % k) for k in range(2)]
        ab = sb("ab", [128, 768], BF16)
        b_ab = Buf("ab")
        abT = sb("abT", [128, 6, 128], BF16)
        b_abT = Buf("abT")
        mm = sb("mm", [128, D], BF16)
        b_mm = Buf("mm")
        mT = sb("mT", [128, 8, 128], BF16)
        b_mT = Buf("mT")
        u = sb("u", [128, D], F32)
        b_u = Buf("u")
        junkf = sb("junk2", [128, D], F32)
        b_junkf = Buf("junk2")
        st = sb("st2", [128, 8], F32)
        b_st = Buf("st2")
        h2 = [sb("h%d" % k, [128, D], F32) for k in range(2)]
        b_h2 = [Buf("h%d" % k) for k in range(2)]
        hb = [sb("hb%d" % k, [128, D], BF16) for k in range(2)]
        b_hb = [Buf("hb%d" % k) for k in range(2)]
        hT2 = [sb("hT%d" % k, [128, 8, 128], BF16) for k in range(2)]
        b_hT2 = [Buf("hT%d" % k) for k in range(2)]
        EM = sb("EM", [128, NOWN, 64], BF16)
        b_em = Buf("EM")
        UT = sb("UT", [128, 128], BF16)
        eoff = sb("eoff", [128, 64], F32)
        b_c2 = Buf("c2")
        rt = sb("rt", [128, 12, 64], F32)
        b_rt = Buf("rt")
        rs = sb("rs", [128, 64], F32)
        b_rs = Buf("rs")
        s8f = sb("s8f", [128, 8], F32)
        sil = sb("sil", [128, 256], F32)
        b_sil = Buf("sil")
        GT = sb("GT", [128, 256], BF16)
        b_gt = Buf("GT")
        base = sb("base", [128, D], F32)
        b_base = Buf("base")

        def loadw(dst, src, rows, cols):
            nchunk = rows // 128
            srcv = src.rearrange("(c p) n -> p c n", p=128)
            k = 0
            for c0 in range(0, cols, 512):
                cw = min(512, cols - c0)
                sidx = k % 2
                k += 1
                self.dma(stg[sidx][:, 0:nchunk, 0:cw], srcv[:, :, c0:c0 + cw], [], [b_stg[sidx]], b_stg[sidx])
                self.cast_rr(dst[:, :, c0:c0 + cw], stg[sidx][:, 0:nchunk, 0:cw], [b_stg[sidx]], [b_w])
        loadw(wga, wga_d, D, D)
        loadw(wgb, wgb_d, D, D)
        loadw(wba, wba_d, 512, D)
        loadw(wbb, wbb_d, 256, D)
        loadw(wo, wo_d, D, D)
        loadw(wr, wr_d, D, 64)
        loadw(ws1, ws1_d, D, 256)
        loadw(ws3, ws3_d, D, 256)
        loadw(ws2, ws2_d, 256, D)
        self.load_bcast(bgate, bgate_d, b_vec)
        self.load_bcast(ln1g, ln1g_d, b_vec)
        self.load_bcast(ln1b, ln1b_d, b_vec)
        self.load_bcast(rbias, rb_d, b_vec)
        V(lambda e: e.tensor_scalar(out=UT[:], in0=self.idsrc[:], scalar1=0.0, scalar2=None, op0=ALU.is_gt), r=[self.b_const], w=[b_c2])
        G(lambda e: e.iota(eoff[:], pattern=[[C, 64]], base=0, channel_multiplier=0, allow_small_or_imprecise_dtypes=True), w=[b_c2])

        halves = ((0, 512), (512, 512))

        def layer_norm(src, b_src, dst, b_dst, gam, bet, eps):
            A(lambda e: e.activation(out=junkf[:], in_=src[:], func=AF.Copy, accum_out=st[:, 0:1]), r=[b_src], w=[b_junkf, b_st])
            A(lambda e: e.activation(out=junkf[:], in_=src[:], func=AF.Square, accum_out=st[:, 1:2]), r=[b_src], w=[b_junkf, b_st])
            V(lambda e: e.tensor_scalar(out=st[:, 2:3], in0=st[:, 0:1], scalar1=1.0 / D, scalar2=None, op0=ALU.mult), r=[b_st], w=[b_st])
            V(lambda e: e.tensor_tensor(out=st[:, 3:4], in0=st[:, 2:3], in1=st[:, 2:3], op=ALU.mult), r=[b_st], w=[b_st])
            V(lambda e: e.scalar_tensor_tensor(out=st[:, 4:5], in0=st[:, 1:2], scalar=1.0 / D, in1=st[:, 3:4], op0=ALU.mult, op1=ALU.subtract),
              r=[b_st], w=[b_st])
            V(lambda e: e.tensor_scalar(out=st[:, 4:5], in0=st[:, 4:5], scalar1=eps, scalar2=None, op0=ALU.add), r=[b_st], w=[b_st])
            A(lambda e: e.sqrt(out=st[:, 5:6], in_=st[:, 4:5]), r=[b_st], w=[b_st])
            V(lambda e: e.reciprocal(out=st[:, 6:7], in_=st[:, 5:6]), r=[b_st], w=[b_st])
            V(lambda e: e.tensor_scalar(out=dst[:], in0=src[:], scalar1=st[:, 2:3], scalar2=st[:, 6:7], op0=ALU.subtract, op1=ALU.mult),
              r=[b_src, b_st], w=[b_dst])
            V(lambda e: e.tensor_tensor(out=dst[:], in0=dst[:], in1=gam[:], op=ALU.mult), r=[b_vec], w=[b_dst])
            V(lambda e: e.tensor_tensor(out=dst[:], in0=dst[:], in1=bet[:], op=ALU.add), r=[b_vec], w=[b_dst])
        self.layer_norm = layer_norm

        def front(i):
            h, b_h, hT, b_hT = h2[i % 2], b_h2[i % 2], hT2[i % 2], b_hT2[i % 2]
            xT, bxT = self.load_xT(self.x_own[i * 128:(i + 1) * 128, :])
            xs = self.last_s
            xf, b_xf = self.xf[xs], self.b_xf[xs]
            for gi, (w, banks) in enumerate(((wga, (1, 3)), (wgb, (4, 5)))):
                for hi, (c0, cw) in enumerate(halves):
                    bk = banks[hi]
                    for c in range(8):
                        T(lambda e, bk=bk, c=c, c0=c0, w=w: e.matmul(self.bank[bk][:], lhsT=xT[:, c, :], rhs=w[:, c, c0:c0 + 512],
                                                                   start=(c == 0), stop=(c == 7)), r=[bxT, b_w], w=[self.bbank[bk]])
                    V(lambda e, bk=bk, gi=gi, c0=c0, hi=hi: e.tensor_tensor(out=gtmp[hi][:], in0=self.bank[bk][:],
                                                                          in1=bgate[:, gi * D + c0:gi * D + c0 + 512], op=ALU.add),
                      r=[self.bbank[bk], b_vec], w=[b_gtmp[hi]])
                    A(lambda e, gi=gi, c0=c0, hi=hi: e.activation(out=gate[gi][:, c0:c0 + 512], in_=gtmp[hi][:], func=AF.Sigmoid),
                      r=[b_gtmp[hi]], w=[b_gate[gi]])
                    yield
            self.dma(ab[:, 0:512], self.aout_d[i * 128:(i + 1) * 128, :], [self.b_aoutd], [b_ab], b_ab)
            self.dma(ab[:, 512:768], self.bout_d[i * 128:(i + 1) * 128, :], [self.b_boutd], [b_ab], b_ab)
            bv2 = self.bview(2)
            for c in range(6):
                T(lambda e, c=c: e.transpose(out=bv2[:, c * 128:(c + 1) * 128], in_=ab[:, c * 128:(c + 1) * 128], identity=self.identb[:]),
                  r=[b_ab, self.b_const], w=[self.bbank[2]])
            A(lambda e: e.activation(out=abT[:], in_=bv2[:, 0:768].rearrange("p (c t) -> p c t", c=6), func=AF.Copy), r=[self.bbank[2]], w=[b_abT])
            yield
            for hi, (c0, cw) in enumerate(halves):
                ba, bb_ = (1, 3)[hi], (4, 5)[hi]
                for c in range(4):
                    T(lambda e, ba=ba, c=c, c0=c0: e.matmul(self.bank[ba][:], lhsT=abT[:, c, :], rhs=wba[:, c, c0:c0 + 512], start=(c == 0), stop=(c == 3)),
                      r=[b_abT, b_w], w=[self.bbank[ba]])
                for c in range(2):
                    T(lambda e, bb_=bb_, c=c, c0=c0: e.matmul(self.bank[bb_][:], lhsT=abT[:, 4 + c, :], rhs=wbb[:, c, c0:c0 + 512], start=(c == 0), stop=(c == 1)),
                      r=[b_abT, b_w], w=[self.bbank[bb_]])
                V(lambda e, ba=ba, c0=c0: e.tensor_tensor(out=gtmp[0][:], in0=self.bank[ba][:], in1=gate[0][:, c0:c0 + 512], op=ALU.mult),
                  r=[self.bbank[ba], b_gate[0]], w=[b_gtmp[0]])
                V(lambda e, bb_=bb_, c0=c0: e.tensor_tensor(out=gtmp[1][:], in0=self.bank[bb_][:], in1=gate[1][:, c0:c0 + 512], op=ALU.mult),
                  r=[self.bbank[bb_], b_gate[1]], w=[b_gtmp[1]])
                V(lambda e, c0=c0: e.tensor_tensor(out=mm[:, c0:c0 + 512], in0=gtmp[0][:], in1=gtmp[1][:], op=ALU.add),
                  r=[b_gtmp[0], b_gtmp[1]], w=[b_mm])
                yield
            for c in range(8):
                T(lambda e, c=c: e.transpose(out=bv2[:, c * 128:(c + 1) * 128], in_=mm[:, c * 128:(c + 1) * 128], identity=self.identb[:]),
                  r=[b_mm, self.b_const], w=[self.bbank[2]])
            A(lambda e: e.activation(out=mT[:], in_=bv2[:].rearrange("p (c t) -> p c t", c=8), func=AF.Copy), r=[self.bbank[2]], w=[b_mT])
            yield
            for hi, (c0, cw) in enumerate(halves):
                bk = (1, 3)[hi]
                for c in range(8):
                    T(lambda e, bk=bk, c=c, c0=c0: e.matmul(self.bank[bk][:], lhsT=mT[:, c, :], rhs=wo[:, c, c0:c0 + 512], start=(c == 0), stop=(c == 7)),
                      r=[b_mT, b_w], w=[self.bbank[bk]])
                V(lambda e, bk=bk, c0=c0: e.scalar_tensor_tensor(out=u[:, c0:c0 + 512], in0=xf[:, c0:c0 + 512], scalar=ALPHA, in1=self.bank[bk][:],
                                                                 op0=ALU.mult, op1=ALU.add), r=[b_xf, self.bbank[bk]], w=[b_u])
                yield
            layer_norm(u, b_u, h, b_h, ln1g, ln1b, 1e-5)
            yield
            if self.debug:
                self.dma(self.h_d[i * 128:(i + 1) * 128, :], h[:], [b_h], [self.b_hd], self.b_hd)
            hbb, b_hbb = hb[i % 2], b_hb[i % 2]
            A(lambda e: e.activation(out=hbb[:], in_=h[:], func=AF.Copy), r=[b_h], w=[b_hbb])
            for c in range(8):
                T(lambda e, c=c: e.transpose(out=bv2[:, c * 128:(c + 1) * 128], in_=hbb[:, c * 128:(c + 1) * 128], identity=self.identb[:]),
                  r=[b_hbb, self.b_const], w=[self.bbank[2]])
            V(lambda e: e.tensor_copy(out=hT[:], in_=bv2[:].rearrange("p (c t) -> p c t", c=8)), r=[self.bbank[2]], w=[b_hT])

        def back(i):
            h, b_h, hT, b_hT = h2[i % 2], b_h2[i % 2], hT2[i % 2], b_hT2[i % 2]
            hbb, b_hbb = hb[i % 2], b_hb[i % 2]
            p6, bp6 = self.bank[6], self.bbank[6]
            for c in range(8):
                T(lambda e, c=c: e.matmul(p6[:, 0:64], lhsT=hT[:, c, :], rhs=wr[:, c, :], start=(c == 0), stop=(c == 7)), r=[b_hT, b_w], w=[bp6])
            sc, bia, grp2, tt, mb, emk, ts, wfull, slotf, jk = (rt[:, k, :] for k in range(10))
            gm1, gm2, gs, s8, gmask, pen, v8 = rs[:, 0:8], rs[:, 8:16], rs[:, 16:24], rs[:, 24:32], rs[:, 32:40], rs[:, 40:48], rs[:, 48:56]
            den = rs[:, 56:57]
            g3 = lambda ap: ap.rearrange("p (g e) -> p g e", g=8)
            A(lambda e: e.activation(out=sc, in_=p6[:, 0:64], func=AF.Sigmoid), r=[bp6], w=[b_rt])
            yield
            V(lambda e: e.tensor_tensor(out=bia, in0=sc, in1=rbias[:], op=ALU.add), r=[b_vec], w=[b_rt])
            V(lambda e: e.tensor_reduce(out=gm1, in_=g3(bia), axis=AX.X, op=ALU.max), r=[b_rt], w=[b_rs])
            V(lambda e: e.tensor_tensor(out=g3(tt), in0=g3(bia), in1=gm1.unsqueeze(2).to_broadcast([128, 8, 8]), op=ALU.is_ge), r=[b_rs], w=[b_rt])
            V(lambda e: e.scalar_tensor_tensor(out=grp2, in0=tt, scalar=-1.0e9, in1=bia, op0=ALU.mult, op1=ALU.add), w=[b_rt])
            V(lambda e: e.tensor_reduce(out=gm2, in_=g3(grp2), axis=AX.X, op=ALU.max), r=[b_rt], w=[b_rs])
            V(lambda e: e.tensor_tensor(out=gs, in0=gm1, in1=gm2, op=ALU.add), w=[b_rs])
            V(lambda e: e.max(out=s8, in_=gs), w=[b_rs])
            V(lambda e: e.tensor_scalar(out=gmask, in0=gs, scalar1=s8[:, 3:4], scalar2=None, op0=ALU.is_ge), w=[b_rs])
            yield
            V(lambda e: e.tensor_scalar(out=pen, in0=gmask, scalar1=-1.0, scalar2=1.0e9, op0=ALU.add, op1=ALU.mult), w=[b_rs])
            V(lambda e: e.tensor_tensor(out=g3(tt), in0=g3(bia), in1=gmask.unsqueeze(2).to_broadcast([128, 8, 8]), op=ALU.mult), r=[b_rs], w=[b_rt])
            V(lambda e: e.tensor_tensor(out=g3(mb), in0=g3(tt), in1=pen.unsqueeze(2).to_broadcast([128, 8, 8]), op=ALU.add), r=[b_rs], w=[b_rt])
            V(lambda e: e.max(out=v8, in_=mb), r=[b_rt], w=[b_rs])
            V(lambda e: e.tensor_scalar(out=emk, in0=mb, scalar1=v8[:, 7:8], scalar2=None, op0=ALU.is_ge), r=[b_rs], w=[b_rt])
            V(lambda e: e.tensor_copy(out=EM[:, i, :], in_=emk), r=[b_rt], w=[b_em])
            V(lambda e: e.tensor_tensor(out=ts, in0=sc, in1=emk, op=ALU.mult), w=[b_rt])
            V(lambda e: e.tensor_reduce(out=den, in_=ts, axis=AX.X, op=ALU.add), r=[b_rt], w=[b_rs])
            V(lambda e: e.reciprocal(out=den, in_=den), w=[b_rs])
            V(lambda e: e.tensor_scalar(out=wfull, in0=ts, scalar1=den, scalar2=2.5, op0=ALU.mult, op1=ALU.mult), r=[b_rs], w=[b_rt])
            yield
            p7, bp7 = self.bank[7], self.bbank[7]
            for j in range(i):
                T(lambda e, j=j: e.matmul(p7[:, 0:64], lhsT=self.onesb[:], rhs=EM[:, j, :], start=(j == 0), stop=False), r=[b_em, self.b_const], w=[bp7])
            T(lambda e: e.matmul(p7[:, 0:64], lhsT=UT[:], rhs=EM[:, i, :], start=(i == 0), stop=True), r=[b_em, b_c2], w=[bp7])
            V(lambda e: e.tensor_scalar(out=jk, in0=p7[:, 0:64], scalar1=C - 0.5, scalar2=1.0e6, op0=ALU.is_ge, op1=ALU.mult), r=[bp7], w=[b_rt])
            V(lambda e: e.tensor_tensor(out=slotf, in0=p7[:, 0:64], in1=eoff[:], op=ALU.add), r=[bp7, b_c2], w=[b_rt])
            V(lambda e: e.tensor_tensor(out=slotf, in0=slotf, in1=jk, op=ALU.add), w=[b_rt])
            yield
            for k in range(8):
                V(lambda e, k=k: e.scalar_tensor_tensor(out=jk, in0=mb, scalar=v8[:, k:k + 1], in1=slotf, op0=ALU.is_equal, op1=ALU.mult,
                                                        accum_out=s8f[:, k:k + 1]), r=[b_rs], w=[b_rt])
                V(lambda e, k=k: e.scalar_tensor_tensor(out=jk, in0=mb, scalar=v8[:, k:k + 1], in1=wfull, op0=ALU.is_equal, op1=ALU.mult,
                                                        accum_out=self.W8[:, i, k:k + 1]), r=[b_rs], w=[b_rt, self.b_route])
            V(lambda e: e.tensor_copy(out=self.SLOT8[:, i, :], in_=s8f[:]), r=[b_rt], w=[self.b_route])
            yield
            for k in range(8):
                self.P.op("gpsimd", lambda e, k=k: e.indirect_dma_start(
                    out=self.xe_d[:, :], out_offset=bass.IndirectOffsetOnAxis(ap=self.SLOT8[:, i, k:k + 1], axis=0),
                    in_=hbb[:], in_offset=None, bounds_check=self.bc_reg(e, NSLOT - 1), oob_is_err=False),
                    [b_hbb, self.b_route], [self.b_xed], dma_out=self.b_xed)
            yield
            for fi, w in enumerate((ws1, ws3)):
                for fc in range(2):
                    r0 = (fi * 2 + fc) * 128
                    for c in range(8):
                        T(lambda e, w=w, fc=fc, c=c, r0=r0: e.matmul(p6[:, r0:r0 + 128], lhsT=w[:, c, fc * 128:(fc + 1) * 128], rhs=hT[:, c, :],
                                                                     start=(c == 0), stop=(c == 7)), r=[b_w, b_hT], w=[bp6])
            A(lambda e: e.activation(out=sil[:], in_=p6[:, 0:256], func=AF.Silu), r=[bp6], w=[b_sil])
            V(lambda e: e.tensor_tensor(out=GT[:], in0=p6[:, 256:512], in1=sil[:], op=ALU.mult), r=[bp6, b_sil], w=[b_gt])
            yield
            for hi, (c0, cw) in enumerate(halves):
                bk = (1, 3)[hi]
                for fc in range(2):
                    T(lambda e, bk=bk, fc=fc, c0=c0: e.matmul(self.bank[bk][:], lhsT=GT[:, fc * 128:(fc + 1) * 128], rhs=ws2[:, fc, c0:c0 + 512],
                                                              start=(fc == 0), stop=(fc == 1)), r=[b_gt, b_w], w=[self.bbank[bk]])
                V(lambda e, bk=bk, c0=c0: e.scalar_tensor_tensor(out=base[:, c0:c0 + 512], in0=h[:, c0:c0 + 512], scalar=ALPHA, in1=self.bank[bk][:],
                                                                 op0=ALU.mult, op1=ALU.add), r=[b_h, self.bbank[bk]], w=[b_base])
                yield
            self.dma(self.base_d[i * 128:(i + 1) * 128, :], base[:], [b_base], [self.b_based], self.b_based, eng="gpsimd")


        for _ in front(0):
            pass
        for i in range(n_own):
            gb = back(i)
            if i + 1 < n_own:
                ga = front(i + 1)
                na, nb = 12, 9
                done_a = done_b = False
                ca = cb = 0
                while not (done_a and done_b):
                    if not done_a and (done_b or ca * nb <= cb * na):
                        try:
                            next(ga)
                        except StopIteration:
                            done_a = True
                        ca += 1
                    else:
                        try:
                            next(gb)
                        except StopIteration:
                            done_b = True
                        cb += 1
            else:
                for _ in gb:
                    pass


    def phase3(self):
        V, A, G, T = self.V, self.A, self.G, self.T
        sb = self.sb
        C = self.CAP
        NSLOT = 64 * C
        NCH = C // 256
        w1_d = self.dram_in("w1_e", [64, D, 256], F32)
        w3_d = self.dram_in("w3_e", [64, D, 256], F32)
        w2_d = self.dram_in("w2_e", [64, 256, D], F32)
        self.ye_d = self.dram_tmp("ye_d", [NSLOT, D], BF16)
        self.b_yed = Buf("yed")
        n_exp = self.n_exp
        w1s = [sb("w1s%d" % k, [128, 8, 256], F32) for k in range(2)]
        w3s = [sb("w3s%d" % k, [128, 8, 256], F32) for k in range(2)]
        w2s = [sb("w2s%d" % k, [128, 2, D], F32) for k in range(2)]
        b_ws = [[Buf("w%ds%d" % (j, k)) for j in range(3)] for k in range(2)]
        w1b = [sb("w1b%d" % k, [128, 8, 256], BF16) for k in range(2)]
        w3b = [sb("w3b%d" % k, [128, 8, 256], BF16) for k in range(2)]
        w2b = [sb("w2b%d" % k, [128, 2, D], BF16) for k in range(2)]
        b_wb = [Buf("wb%d" % k) for k in range(2)]
        xe = [sb("xe%d" % k, [128, 2, D], BF16) for k in range(2)]
        b_xe = [Buf("xe%d" % k) for k in range(2)]
        XeT = [sb("XeT%d" % k, [128, 8, 256], BF16) for k in range(2)]
        b_xet = [Buf("XeT%d" % k) for k in range(2)]
        sil = [sb("sil3%d" % k, [128, 512], F32) for k in range(2)]
        b_sil = [Buf("sil3%d" % k) for k in range(2)]
        GT = [sb("GT3%d" % k, [128, 512], BF16) for k in range(2)]
        b_gt = [Buf("GT3%d" % k) for k in range(2)]
        Y = [sb("Y%d" % k, [128, D], BF16) for k in range(4)]
        b_y = [Buf("Y%d" % k) for k in range(4)]
        steps = [(ex, ch) for ex in range(n_exp) for ch in range(NCH)]

        def load_w(ex):
            ws = ex % 2
            self.dma(w1s[ws][:], w1_d[ex].rearrange("(c p) n -> p c n", p=128), [], [b_ws[ws][0]], b_ws[ws][0])
            self.dma(w3s[ws][:], w3_d[ex].rearrange("(c p) n -> p c n", p=128), [], [b_ws[ws][1]], b_ws[ws][1])
            self.dma(w2s[ws][:], w2_d[ex].rearrange("(c p) n -> p c n", p=128), [], [b_ws[ws][2]], b_ws[ws][2])
            A(lambda e, ws=ws: e.activation(out=w1b[ws][:], in_=w1s[ws][:], func=AF.Copy), r=[b_ws[ws][0]], w=[b_wb[ws]])
            V(lambda e, ws=ws: e.tensor_copy(out=w3b[ws][:], in_=w3s[ws][:]), r=[b_ws[ws][1]], w=[b_wb[ws]])
            G(lambda e, ws=ws: e.tensor_copy(out=w2b[ws][:], in_=w2s[ws][:]), r=[b_ws[ws][2]], w=[b_wb[ws]])

        def stage1(n):
            ex, ch = steps[n]
            ws = ex % 2
            if ch == 0 and ex == 0:
                load_w(0)
            if ch == 1 and ex + 1 < n_exp:
                load_w(ex + 1)
            t = n % 2
            r0 = ex * C + ch * 256
            self.dma(xe[t][:], self.xe_d[r0:r0 + 256, :].rearrange("(k p) d -> p k d", p=128), [self.b_xed], [b_xe[t]], b_xe[t])
            for kk in range(2):
                bvt = self.bview(kk)
                for c in range(8):
                    T(lambda e, c=c, t=t, kk=kk, bvt=bvt: e.transpose(out=bvt[:, c * 128:(c + 1) * 128], in_=xe[t][:, kk, c * 128:(c + 1) * 128],
                                                                  identity=self.identb[:]), r=[b_xe[t], self.b_const], w=[self.bbank[kk]])
                if kk == 0:
                    A(lambda e, t=t, bvt=bvt: e.activation(out=XeT[t][:, :, 0:128], in_=bvt[:].rearrange("p (c t) -> p c t", c=8), func=AF.Copy),
                      r=[self.bbank[kk]], w=[b_xet[t]])
                else:
                    V(lambda e, t=t, bvt=bvt: e.tensor_copy(out=XeT[t][:, :, 128:256], in_=bvt[:].rearrange("p (c t) -> p c t", c=8)),
                      r=[self.bbank[kk]], w=[b_xet[t]])
            for fi, w in enumerate((w1b[ws], w3b[ws])):
                bk = 2 + fi
                for fc in range(2):
                    for c in range(8):
                        T(lambda e, w=w, fc=fc, c=c, t=t, bk=bk: e.matmul(self.bank[bk][:, fc * 256:(fc + 1) * 256], lhsT=w[:, c, fc * 128:(fc + 1) * 128],
                                                                      rhs=XeT[t][:, c, :], start=(c == 0), stop=(c == 7)),
                          r=[b_wb[ws], b_xet[t]], w=[self.bbank[bk]])
            A(lambda e, t=t: e.activation(out=sil[t][:], in_=self.bank[2][:], func=AF.Silu), r=[self.bbank[2]], w=[b_sil[t]])
            V(lambda e, t=t: e.tensor_tensor(out=GT[t][:], in0=self.bank[3][:], in1=sil[t][:], op=ALU.mult), r=[self.bbank[3], b_sil[t]], w=[b_gt[t]])

        def stage2(n):
            ex, ch = steps[n]
            ws = ex % 2
            t = n % 2
            for kk in range(2):
                r0 = ex * C + ch * 256 + kk * 128
                ybanks = (4, 5) if kk == 0 else (6, 7)
                yi = (2 * n + kk) % 4
                for hi in range(2):
                    bk = ybanks[hi]
                    for fc in range(2):
                        T(lambda e, bk=bk, fc=fc, hi=hi, t=t, ws=ws, kk=kk: e.matmul(self.bank[bk][:], lhsT=GT[t][:, fc * 256 + kk * 128:fc * 256 + (kk + 1) * 128],
                                                                                 rhs=w2b[ws][:, fc, hi * 512:(hi + 1) * 512], start=(fc == 0), stop=(fc == 1)),
                          r=[b_gt[t], b_wb[ws]], w=[self.bbank[bk]])
                A(lambda e, yi=yi, bk=ybanks[0]: e.activation(out=Y[yi][:, 0:512], in_=self.bank[bk][:], func=AF.Copy), r=[self.bbank[ybanks[0]]], w=[b_y[yi]])
                V(lambda e, yi=yi, bk=ybanks[1]: e.tensor_copy(out=Y[yi][:, 512:1024], in_=self.bank[bk][:]), r=[self.bbank[ybanks[1]]], w=[b_y[yi]])
                self.dma(self.ye_d[r0:r0 + 128, :], Y[yi][:], [b_y[yi]], [self.b_yed], self.b_yed, eng="gpsimd")

        ns = len(steps)
        stage1(0)
        for n in range(ns):
            if n + 1 < ns:
                stage1(n + 1)
            stage2(n)

    def phase4(self):
        V, A, G, T = self.V, self.A, self.G, self.T
        sb = self.sb
        C = self.CAP
        NSLOT = 64 * C
        ln2g_d, ln2b_d = self.dram_in("ln2_g", [1, D], F32), self.dram_in("ln2_b", [1, D], F32)
        self.out_d = self.dram_out("out_d", [NOWN * 128, D], F32)
        self.b_outd = Buf("outd")
        ln2g, ln2b = sb("ln2g", [128, D], F32), sb("ln2b", [128, D], F32)
        b_vec = Buf("vec4")
        self.load_bcast(ln2g, ln2g_d, b_vec)
        self.load_bcast(ln2b, ln2b_d, b_vec)
        acc = [sb("acc%d" % k, [128, D], F32) for k in range(2)]
        b_acc = [Buf("acc%d" % k) for k in range(2)]
        yk = [sb("yk%d" % k, [128, D], BF16) for k in range(8)]
        b_yk = [Buf("yk%d" % k) for k in range(8)]
        o = [sb("o%d" % k, [128, D], F32) for k in range(2)]
        b_o = [Buf("o%d" % k) for k in range(2)]
        junkf = sb("junk4", [128, D], F32)
        st = sb("st4", [128, 8], F32)
        b_junkf, b_st = Buf("junk4"), Buf("st4")

        def layer_norm(src, b_src, dst, b_dst, gam, bet, eps):
            A(lambda e: e.activation(out=junkf[:], in_=src[:], func=AF.Copy, accum_out=st[:, 0:1]), r=[b_src], w=[b_junkf, b_st])
            A(lambda e: e.activation(out=junkf[:], in_=src[:], func=AF.Square, accum_out=st[:, 1:2]), r=[b_src], w=[b_junkf, b_st])
            V(lambda e: e.tensor_scalar(out=st[:, 2:3], in0=st[:, 0:1], scalar1=1.0 / D, scalar2=None, op0=ALU.mult), r=[b_st], w=[b_st])
            V(lambda e: e.tensor_tensor(out=st[:, 3:4], in0=st[:, 2:3], in1=st[:, 2:3], op=ALU.mult), r=[b_st], w=[b_st])
            V(lambda e: e.scalar_tensor_tensor(out=st[:, 4:5], in0=st[:, 1:2], scalar=1.0 / D, in1=st[:, 3:4], op0=ALU.mult, op1=ALU.subtract),
              r=[b_st], w=[b_st])
            V(lambda e: e.tensor_scalar(out=st[:, 4:5], in0=st[:, 4:5], scalar1=eps, scalar2=None, op0=ALU.add), r=[b_st], w=[b_st])
            A(lambda e: e.sqrt(out=st[:, 5:6], in_=st[:, 4:5]), r=[b_st], w=[b_st])
            V(lambda e: e.reciprocal(out=st[:, 6:7], in_=st[:, 5:6]), r=[b_st], w=[b_st])
            V(lambda e: e.tensor_scalar(out=dst[:], in0=src[:], scalar1=st[:, 2:3], scalar2=st[:, 6:7], op0=ALU.subtract, op1=ALU.mult),
              r=[b_src, b_st], w=[b_dst])
            V(lambda e: e.tensor_tensor(out=dst[:], in0=dst[:], in1=gam[:], op=ALU.mult), r=[b_vec], w=[b_dst])
            V(lambda e: e.tensor_tensor(out=dst[:], in0=dst[:], in1=bet[:], op=ALU.add), r=[b_vec], w=[b_dst])

        n = 0
        self.dma(acc[0][:], self.base_d[0:128, :], [self.b_based], [b_acc[0]], b_acc[0])
        for i in range(self.n_own):
            a, b_a = acc[i % 2], b_acc[i % 2]
            if i + 1 < self.n_own:
                self.dma(acc[(i + 1) % 2][:], self.base_d[(i + 1) * 128:(i + 2) * 128, :], [self.b_based], [b_acc[(i + 1) % 2]], b_acc[(i + 1) % 2])
            for k in range(8):
                t = n % 8
                n += 1
                self.P.op("gpsimd", lambda e, t=t, i=i, k=k: e.indirect_dma_start(
                    out=yk[t][:], out_offset=None, in_=self.ye_d[:, :],
                    in_offset=bass.IndirectOffsetOnAxis(ap=self.SLOT8[:, i, k:k + 1], axis=0), bounds_check=self.bc_reg(e, NSLOT - 1), oob_is_err=False),
                    [self.b_yed, self.b_route], [b_yk[t]], dma_out=b_yk[t])
                V(lambda e, t=t, i=i, k=k, a=a: e.scalar_tensor_tensor(out=a[:], in0=yk[t][:], scalar=self.W8[:, i, k:k + 1], in1=a[:],
                                                                       op0=ALU.mult, op1=ALU.add), r=[b_yk[t], self.b_route], w=[b_a])
            oo, b_oo = o[i % 2], b_o[i % 2]
            layer_norm(a, b_a, oo, b_oo, ln2g, ln2b, 1e-5)
            self.dma(self.out_d[i * 128:(i + 1) * 128, :], oo[:], [b_oo], [self.b_outd], self.b_outd)


def build_program(n_own=NOWN, debug=False, phases="ab234", n_exp=64):
    nc = bass.Bass("TRN2", target_bir_lowering=False)
    st = ExitStack()
    with st:
        B = Builder(nc, st, n_own=n_own, debug=debug)
        B.CAP = 768
        B.n_exp = n_exp
        B.setup_common()
        B.phase1a()
        fin = [B.b_aoutd]
        if "b" in phases:
            B.phase_reset(B.mark)
            B.phase1b()
            fin.append(B.b_boutd)
        if "2" in phases:
            B.phase_reset(B.mark)
            B.phase2()
            fin += [B.b_based, B.b_xed]
            if debug:
                fin.append(B.b_hd)
        if "3" in phases:
            B.phase_reset(B.mark)
            B.phase3()
            fin.append(B.b_yed)
        if "4" in phases:
            B.phase_reset(B.mark)
            B.phase4()
            fin.append(B.b_outd)
        B.P.final_wait("sync", fin)
        B.P.emit()
        print("[kernel] semaphores used:", B.P.nsem, "ops:", {e: len(v) for e, v in B.P.ops.items()})
    return nc


def rope_consts():
    inv16 = ROPE_THETA ** (-np.arange(0, 16, 2, dtype=np.float32) / 16)
    inv8 = ROPE_THETA ** (-np.arange(0, 8, 2, dtype=np.float32) / 8)
    inv = np.concatenate([inv16, inv16, inv8, inv8]).astype(np.float32)
    off = np.concatenate([np.full(8, math.pi / 2), np.zeros(8), np.full(4, math.pi / 2), np.zeros(4)]).astype(np.float32)
    return np.tile(np.concatenate([inv, off])[None, :], (128, 1)).astype(np.float32)


def make_in_maps(inputs, cores=range(8)):
    x = inputs["x"]
    positions = inputs["positions"]
    w_in = inputs["w_in"][0]
    offs = np.cumsum([0, 512, 256, 16, 256, 32, 8, 768, 768, 768, 1024, 1024])
    col = lambda k: slice(offs[k], offs[k + 1])
    wk_dsa = np.ascontiguousarray(np.concatenate([w_in[:, col(1)], w_in[:, col(2)], w_in[:, col(4)]], axis=1))
    wq_dsa = np.ascontiguousarray(np.concatenate([w_in[:, col(0)], w_in[:, col(3)], w_in[:, col(5)]], axis=1))
    w_bq = np.ascontiguousarray(w_in[:, col(6)])
    w_bk = np.ascontiguousarray(w_in[:, col(7)])
    w_bv = np.ascontiguousarray(w_in[:, col(8)])
    w_ga = np.ascontiguousarray(w_in[:, col(9)])
    w_gb = np.ascontiguousarray(w_in[:, col(10)])
    f32c = lambda a: np.ascontiguousarray(a, dtype=np.float32)
    import ml_dtypes
    zeros_bf = np.zeros((1024, D), dtype=ml_dtypes.bfloat16)
    w1_e, w3_e, w2_e = f32c(inputs["w1_e"][0]), f32c(inputs["w3_e"][0]), f32c(inputs["w2_e"][0])
    wuk_t = np.ascontiguousarray(inputs["w_uk"][0].transpose(2, 1, 0))
    wuv = np.ascontiguousarray(inputs["w_uv"][0].reshape(256, 512))
    rc = rope_consts()
    maps = []
    for c in cores:
        b, par = c // 2, c % 2
        xb = x[b]
        x_own = np.ascontiguousarray(xb.reshape(NBLK, 128, D)[par::2].reshape(NOWN * 128, D))
        pos_t = np.ascontiguousarray(positions[b].reshape(NBLK, 128).T.astype(np.int32))
        pos_own_t = np.ascontiguousarray(positions[b].reshape(NBLK, 128)[par::2].T.astype(np.int32))
        maps.append({
            "x_all": np.ascontiguousarray(xb), "x_own": x_own, "pos_all_t": pos_t, "pos_own_t": pos_own_t,
            "par": np.full((128, 1), float(par), np.float32), "rope_c": rc,
            "wk_dsa": wk_dsa, "wq_dsa": wq_dsa, "wuk_t": wuk_t, "wuv": wuv,
            "w_bq": w_bq, "w_bk": w_bk, "w_bv": w_bv, "w_ga": w_ga, "w_gb": w_gb,
            "b_gate": f32c(inputs["b_gate"].reshape(1, 2048)), "w_branch_a": f32c(inputs["w_branch_a"][0]),
            "w_branch_b": f32c(inputs["w_branch_b"][0]), "w_o": f32c(inputs["w_o"][0]),
            "ln1_g": f32c(inputs["ln1_g"].reshape(1, D)), "ln1_b": f32c(inputs["ln1_b"].reshape(1, D)),
            "w_router": f32c(inputs["w_router"][0]), "router_bias": f32c(inputs["router_bias"].reshape(1, 64)),
            "ws1": f32c(inputs["ws1"][0]), "ws3": f32c(inputs["ws3"][0]), "ws2": f32c(inputs["ws2"][0]),
            "w1_e": w1_e, "w3_e": w3_e, "w2_e": w2_e, "zeros_d": zeros_bf,
            "ln2_g": f32c(inputs["ln2_g"].reshape(1, D)), "ln2_b": f32c(inputs["ln2_b"].reshape(1, D)),
            "g_kv": np.ascontiguousarray(inputs["g_kv"].reshape(1, 256)),
        })
    return maps


_NC_CACHE = {}


def kernel(**inputs):
    inputs = {k: np.asarray(v) for k, v in inputs.items()}
    if "nc" not in _NC_CACHE:
        _NC_CACHE["nc"] = build_program()
    nc = _NC_CACHE["nc"]
    maps = make_in_maps(inputs, cores=range(8))
    res = run_bass_kernel_spmd(nc, maps, core_ids=list(range(8)))
    out = np.empty((4, S, D), np.float32)
    for c in range(8):
        b, par = c // 2, c % 2
        o = np.asarray(res.results[c]["out_d"], dtype=np.float32).reshape(NOWN, 128, D)
        out[b].reshape(NBLK, 128, D)[par::2] = o
    return out
```

```python
import math
from contextlib import ExitStack

import numpy as np
import concourse.bass as bass
import concourse.mybir as mybir
from concourse.bass_utils import run_bass_kernel_spmd

F32 = mybir.dt.float32
BF16 = mybir.dt.bfloat16
I32 = mybir.dt.int32
U32 = mybir.dt.uint32
AF = mybir.ActivationFunctionType
ALU = mybir.AluOpType
AX = mybir.AxisListType

ENGS = ["tensor", "vector", "scalar", "gpsimd", "sync"]

D = 1024
S = 8192
NBLK = 64
NOWN = 32
ROPE_THETA = 500000.0
NEG = -1.0e30
N_BISECT = 16
TWO_PI = 2.0 * math.pi
CW1 = 6.28125
CW2 = TWO_PI - CW1


class Buf:
    __slots__ = ("name", "w", "readers", "dma_sem", "dma_cnt", "excl")

    def __init__(self, name, excl=False):
        self.name = name
        self.excl = excl
        self.w = None
        self.readers = []
        self.dma_sem = None
        self.dma_cnt = 0


class Prog:
    def __init__(self, nc, stack):
        self.nc = nc
        self.stack = stack
        self.ops = {e: [] for e in ENGS}
        self.cnt = {e: 0 for e in ENGS}
        self.sems = {e: stack.enter_context(nc.semaphore("s_" + e)) for e in ENGS}
        self.known = {e: {} for e in ENGS}
        self.nsem = len(ENGS)
        self.dma_bufs = []

    def new_sem(self, name):
        self.nsem += 1
        return self.stack.enter_context(self.nc.semaphore("%s_%d" % (name, self.nsem)))

    def _add_wait(self, waits, tok, eng):
        if tok is None:
            return
        if tok[0] == "e":
            if tok[1] == eng and eng in ("tensor", "sync"):
                return
            key = ("e", tok[1])
            val = tok[2]
        else:
            key = ("d", id(tok[1]))
            val = tok[2] * 16
            waits.setdefault("_sem", {})[key] = tok[1].dma_sem
        if waits.get(key, 0) < val:
            waits[key] = val

    def op(self, eng, fn, reads=(), writes=(), dma_out=None):
        xr = [b for b in reads if b.excl]
        if xr:
            reads = [b for b in reads if not b.excl]
            writes = list(writes) + [b for b in xr if b not in writes]
        waits = {}
        for b in reads:
            self._add_wait(waits, b.w, eng)
        for b in writes:
            self._add_wait(waits, b.w, eng)
            for r in b.readers:
                self._add_wait(waits, r, eng)
        semmap = waits.pop("_sem", {})
        wl = []
        kn = self.known[eng]
        for key, val in waits.items():
            if kn.get(key, 0) >= val:
                continue
            kn[key] = val
            if key[0] == "e":
                wl.append((self.sems[key[1]], val))
            else:
                wl.append((semmap[key], val))
        if dma_out is not None:
            if dma_out.dma_sem is None:
                dma_out.dma_sem = self.new_sem("d_" + dma_out.name)
                self.dma_bufs.append(dma_out)
            dma_out.dma_cnt += 1
            tok = ("d", dma_out, dma_out.dma_cnt)
            self.ops[eng].append((wl, fn, (dma_out.dma_sem, 16)))
        else:
            self.cnt[eng] += 1
            tok = ("e", eng, self.cnt[eng])
            self.ops[eng].append((wl, fn, (self.sems[eng], 1)))
        for b in reads:
            b.readers.append(tok)
            if len(b.readers) > 64:
                b.readers = b.readers[-48:]
        for b in writes:
            b.w = tok
            b.readers = []
        return tok

    def barrier(self):
        for eng in ENGS:
            wl = []
            for f in ENGS:
                if f != eng and self.cnt[f] > 0:
                    wl.append((self.sems[f], self.cnt[f]))
                    self.known[eng][("e", f)] = self.cnt[f]
            for b in self.dma_bufs:
                wl.append((b.dma_sem, b.dma_cnt * 16))
                self.known[eng][("d", id(b))] = b.dma_cnt * 16
            self.ops[eng].append((wl, None, None))

    def final_wait(self, eng, bufs):
        waits = {}
        for b in bufs:
            self._add_wait(waits, b.w, eng)
        semmap = waits.pop("_sem", {})
        wl = []
        for key, val in waits.items():
            if key[0] == "e":
                wl.append((self.sems[key[1]], val))
            else:
                wl.append((semmap[key], val))
        self.ops[eng].append((wl, None, None))

    def emit(self):
        nc = self.nc
        with nc.Block() as block:
            def mk(ename):
                def body(engine):
                    for wl, fn, inc in self.ops[ename]:
                        for sem, val in wl:
                            engine.wait_ge(sem, val)
                        if fn is not None:
                            ins = fn(engine)
                            ins.then_inc(inc[0], inc[1])
                return body
            block.tensor(mk("tensor"))
            block.vector(mk("vector"))
            block.scalar(mk("scalar"))
            block.gpsimd(mk("gpsimd"))
            block.sync(mk("sync"))


class Builder:
    def __init__(self, nc, st, n_own=NOWN, debug=False):
        self.nc = nc
        self.st = st
        self.P = Prog(nc, st)
        self.n_own = n_own
        self.debug = debug
        self.outs = []
        self.pool = None
        self.bump = 0

    POOL_BYTES = 212800

    def sb(self, name, shape, dt):
        if self.pool is None:
            self.pool = self.st.enter_context(self.nc.sbuf_tensor("sb_pool", [128, self.POOL_BYTES // 2], BF16))
            self.bump = 0
        esz = mybir.dt.size(dt)
        n = 1
        for d_ in shape[1:]:
            n *= d_
        nbytes = (n * esz + 63) // 64 * 64
        off = self.bump
        self.bump += nbytes
        assert self.bump <= self.POOL_BYTES, "SBUF pool overflow at %s: %d" % (name, self.bump)
        v = self.pool[0:shape[0], off // 2: off // 2 + (n * esz) // 2]
        if dt != BF16:
            v = v.bitcast(dt)
        if len(shape) == 3:
            v = v.rearrange("p (a b) -> p a b", a=shape[1])
        elif len(shape) == 4:
            v = v.rearrange("p (a b c) -> p a b c", a=shape[1], b=shape[2])
        return v

    def phase_mark(self):
        return self.bump

    def phase_reset(self, mark):
        self.P.barrier()
        self.bump = mark

    def dram_in(self, name, shape, dt):
        return self.nc.dram_tensor(name, shape, dt, kind="ExternalInput").ap()

    def dram_out(self, name, shape, dt):
        t = self.nc.dram_tensor(name, shape, dt, kind="ExternalOutput").ap()
        return t

    def dram_tmp(self, name, shape, dt):
        return self.nc.dram_tensor(name, shape, dt, kind="Internal").ap()

    def bc_reg(self, e, val):
        if getattr(self, "_bcreg", None) is None:
            self._bcreg = e.to_reg(val)
        return self._bcreg

    def cast_rr(self, out, in_, r, w):
        k = getattr(self, "_rr", 0)
        self._rr = k + 1
        if k % 3 == 0:
            self.A(lambda e: e.activation(out=out, in_=in_, func=AF.Copy), r, w)
        elif k % 3 == 1:
            self.V(lambda e: e.tensor_copy(out=out, in_=in_), r, w)
        else:
            self.G(lambda e: e.tensor_copy(out=out, in_=in_), r, w)

    def V(self, fn, r=(), w=()):
        return self.P.op("vector", fn, r, w)

    def A(self, fn, r=(), w=()):
        return self.P.op("scalar", fn, r, w)

    def G(self, fn, r=(), w=()):
        return self.P.op("gpsimd", fn, r, w)

    def T(self, fn, r=(), w=()):
        return self.P.op("tensor", fn, r, w)

    def dma(self, out, in_, r, w, dma_buf, eng="sync"):
        return self.P.op(eng, lambda e: e.dma_start(out=out, in_=in_), r, w, dma_out=dma_buf)

    def setup_common(self):
        nc = self.nc
        self.bank = []
        self.bbank = []
        for k in range(8):
            t = self.st.enter_context(nc.psum_tensor("bank%d" % k, [128, 512], F32))
            self.bank.append(t)
            self.bbank.append(Buf("bank%d" % k, excl=True))
        self.idsrc = self.sb("idsrc", [128, 128], F32)
        self.identf = self.sb("identf", [128, 128], F32)
        self.identb = self.sb("identb", [128, 128], BF16)
        self.onesf = self.sb("onesf", [128, 128], F32)
        self.onesb = self.sb("onesb", [128, 128], BF16)
        self.b_const = Buf("const")
        bc = self.b_const
        self.G(lambda e: e.iota(self.idsrc[:], pattern=[[1, 128]], base=0, channel_multiplier=-1,
                                allow_small_or_imprecise_dtypes=True), w=[bc])
        self.V(lambda e: e.tensor_scalar(out=self.identf[:], in0=self.idsrc[:], scalar1=0.0, scalar2=None,
                                         op0=ALU.is_equal), r=[bc], w=[bc])
        self.V(lambda e: e.tensor_copy(out=self.identb[:], in_=self.identf[:]), r=[bc], w=[bc])
        self.V(lambda e: e.memset(self.onesf[:], 1.0), w=[bc])
        self.V(lambda e: e.memset(self.onesb[:], 1.0), w=[bc])
        NSLOT = 64 * self.CAP
        zeros_d = self.dram_in("zeros_d", [1024, D], BF16)
        self.xe_d = self.dram_tmp("xe_d", [NSLOT, D], BF16)
        self.b_xed = Buf("xed")
        for r0 in range(0, NSLOT, 1024):
            self.P.op("gpsimd", lambda e, r0=r0: e.dma_start(out=self.xe_d[r0:r0 + 1024, :], in_=zeros_d[:, :]), [], [self.b_xed], dma_out=self.b_xed)

    def bview(self, k, dt=BF16):
        return self.bank[k][:].bitcast(dt)

    def rope_table(self, pos_i32, nblk, tab, scr, b_scr, b_tab, ropec, b_in):
        n = nblk * 24
        ang = scr[:, 0:n]
        kf = scr[:, n:2 * n]
        mm = scr[:, 2 * n:3 * n]
        ki = scr[:, 3 * n:4 * n].bitcast(I32)
        posf = scr[:, 4 * n:4 * n + nblk]
        inv = ropec[:, 0:24]
        off = ropec[:, 24:48]
        ang3 = ang.rearrange("p (b j) -> p b j", j=24)
        V = self.V
        V(lambda e: e.tensor_copy(out=posf, in_=pos_i32), r=[b_in], w=[b_scr])
        V(lambda e: e.tensor_tensor(out=ang3, in0=posf.unsqueeze(2).to_broadcast([128, nblk, 24]),
                                    in1=inv.unsqueeze(1).to_broadcast([128, nblk, 24]), op=ALU.mult), r=[b_in], w=[b_scr])
        V(lambda e: e.tensor_tensor(out=ang3, in0=ang3, in1=off.unsqueeze(1).to_broadcast([128, nblk, 24]),
                                    op=ALU.add), r=[b_in], w=[b_scr])
        V(lambda e: e.tensor_scalar(out=ki, in0=ang, scalar1=1.0 / TWO_PI, scalar2=None, op0=ALU.mult), w=[b_scr])
        V(lambda e: e.tensor_copy(out=kf, in_=ki), w=[b_scr])
        V(lambda e: e.scalar_tensor_tensor(out=ang, in0=kf, scalar=-CW1, in1=ang, op0=ALU.mult, op1=ALU.add), w=[b_scr])
        V(lambda e: e.scalar_tensor_tensor(out=ang, in0=kf, scalar=-CW2, in1=ang, op0=ALU.mult, op1=ALU.add), w=[b_scr])
        V(lambda e: e.tensor_scalar(out=mm, in0=ang, scalar1=math.pi, scalar2=-TWO_PI, op0=ALU.is_gt, op1=ALU.mult), w=[b_scr])
        V(lambda e: e.tensor_tensor(out=ang, in0=ang, in1=mm, op=ALU.add), w=[b_scr])
        V(lambda e: e.tensor_scalar(out=mm, in0=ang, scalar1=-math.pi, scalar2=TWO_PI, op0=ALU.is_lt, op1=ALU.mult), w=[b_scr])
        V(lambda e: e.tensor_tensor(out=ang, in0=ang, in1=mm, op=ALU.add), w=[b_scr])
        V(lambda e: e.tensor_scalar(out=ang, in0=ang, scalar1=3.14159, scalar2=-3.14159, op0=ALU.min, op1=ALU.max), w=[b_scr])
        self.A(lambda e: e.activation(out=tab[:].rearrange("p b j -> p (b j)"), in_=ang, func=AF.Sin), r=[b_scr], w=[b_tab])

    def rope(self, o1, o2, x1, x2, cos, sin, tA, tB, r, w, b_tmp):
        V = self.V
        V(lambda e: e.tensor_tensor(out=tA, in0=x1, in1=cos, op=ALU.mult), r=r, w=[b_tmp])
        V(lambda e: e.tensor_tensor(out=tB, in0=x2, in1=sin, op=ALU.mult), r=r, w=[b_tmp])
        V(lambda e: e.tensor_tensor(out=o1, in0=tA, in1=tB, op=ALU.subtract), r=[b_tmp], w=w)
        V(lambda e: e.tensor_tensor(out=tA, in0=x2, in1=cos, op=ALU.mult), r=r, w=[b_tmp])
        V(lambda e: e.tensor_tensor(out=tB, in0=x1, in1=sin, op=ALU.mult), r=r, w=[b_tmp])
        V(lambda e: e.tensor_tensor(out=o2, in0=tA, in1=tB, op=ALU.add), r=[b_tmp], w=w)

    def load_weight_bf16(self, dst, src_ap, ncols, b_dst, stage, b_stage, chunk=512):
        srcv = src_ap.rearrange("(c p) n -> p c n", p=128)
        k = 0
        for c0 in range(0, ncols, chunk):
            cw = min(chunk, ncols - c0)
            sidx = k % len(stage)
            k += 1
            stg = stage[sidx]
            self.dma(stg[:, :, 0:cw], srcv[:, :, c0:c0 + cw], [], [b_stage[sidx]], b_stage[sidx])
            self.cast_rr(dst[:, :, c0:c0 + cw], stg[:, :, 0:cw], [b_stage[sidx]], [b_dst])

    def set_x_sequence(self, seq):
        self.xseq = list(seq)
        self.xl_k = 0
        self.x_issued = 0

    def _issue_x(self, k):
        s = k % 2
        self.dma(self.xf[s][:], self.xseq[k], [], [self.b_xf[s]], self.b_xf[s])

    def load_xT(self, src_rows=None):
        k = self.xl_k
        self.xl_k += 1
        s = k % 2
        if self.x_issued <= k:
            self._issue_x(k)
            self.x_issued = k + 1
        if k + 1 < len(self.xseq) and self.x_issued <= k + 1:
            self._issue_x(k + 1)
            self.x_issued = k + 2
        xf, bxf = self.xf[s], self.b_xf[s]
        xT, bxT = self.xT[s], self.b_xT[s]
        for half, bk in ((0, 0), (1, 2)):
            for c4 in range(4):
                c = half * 4 + c4
                self.T(lambda e, c=c, c4=c4, bk=bk: e.transpose(out=self.bank[bk][:, c4 * 128:(c4 + 1) * 128], in_=xf[:, c * 128:(c + 1) * 128],
                                                              identity=self.identf[:]), r=[bxf, self.b_const], w=[self.bbank[bk]])
        self.A(lambda e: e.activation(out=xT[:, 0:4, :], in_=self.bank[0][:].rearrange("p (c t) -> p c t", c=4), func=AF.Copy),
               r=[self.bbank[0]], w=[bxT])
        self.V(lambda e: e.tensor_copy(out=xT[:, 4:8, :], in_=self.bank[2][:].rearrange("p (c t) -> p c t", c=4)),
               r=[self.bbank[2]], w=[bxT])
        self.last_s = s
        return xT, bxT

    def phase1a(self):
        nc, P = self.nc, self.P
        V, A, G, T = self.V, self.A, self.G, self.T
        n_own = self.n_own
        x_all = self.dram_in("x_all", [S, D], F32)
        x_own = self.dram_in("x_own", [NOWN * 128, D], F32)
        pos_all = self.dram_in("pos_all_t", [128, NBLK], I32)
        pos_own = self.dram_in("pos_own_t", [128, NOWN], I32)
        par_d = self.dram_in("par", [128, 1], F32)
        ropec_d = self.dram_in("rope_c", [128, 48], F32)
        wk_d = self.dram_in("wk_dsa", [D, 304], F32)
        wq_d = self.dram_in("wq_dsa", [D, 776], F32)
        wuk_d = self.dram_in("wuk_t", [48, 8, 256], F32)
        wuv_d = self.dram_in("wuv", [256, 512], F32)
        gkv_d = self.dram_in("g_kv", [1, 256], F32)
        ckv_d = self.dram_out("ckv_d", [S, 256], BF16) if self.debug else self.dram_tmp("ckv_d", [S, 256], BF16)
        if self.debug:
            self.aout_d = self.dram_out("aout_d", [NOWN * 128, 512], BF16)
        else:
            self.aout_d = self.dram_tmp("aout_d", [NOWN * 128, 512], BF16)
        _ckvd8 = [Buf("ckvd%d" % k) for k in range(8)]
        b_ckvd = [_ckvd8[k % 8] for k in range(NBLK)]
        self.b_aoutd = Buf("aoutd")

        sb = self.sb
        par = sb("par", [128, 1], F32)
        cpar = sb("cpar", [128, 4], F32)
        ropec = sb("ropec", [128, 48], F32)
        posa = sb("posa", [128, NBLK], I32)
        poso = sb("poso", [128, NOWN], I32)
        b_small = Buf("smallin")
        TABA = sb("TABA", [128, NBLK, 24], F32)
        TABO = sb("TABO", [128, NOWN, 24], F32)
        b_taba, b_tabo = Buf("taba"), Buf("tabo")
        bm8 = sb("bm8", [128, 8], BF16)
        tmpv = sb("tmpv", [128, 16], F32)
        self.SLOT8 = sb("SLOT8", [128, NOWN, 8], I32)
        self.W8 = sb("W8", [128, NOWN, 8], F32)
        self.b_route = Buf("route")
        self.par, self.cpar, self.TABA, self.TABO = par, cpar, TABA, TABO
        self.b_small, self.b_taba, self.b_tabo = b_small, b_taba, b_tabo
        self.x_all, self.x_own = x_all, x_own
        mark = self.phase_mark()
        CKVT = sb("CKVT", [128, 2, S], BF16)
        KRT = sb("KRT", [128, S], BF16)
        IKT = sb("IKT", [128, S], BF16)
        b_kside = [Buf("kside%d" % k) for k in range(NBLK)]
        SCORE = sb("SCORE", [128, S], F32)
        b_score = [Buf("score%d" % k) for k in range(16)]
        b_scoreall = b_score
        MT = sb("MT", [128, NBLK, 128], BF16)
        b_mt = Buf("MT")
        junk = sb("junk", [128, 1024], BF16)
        b_junk = Buf("junk")
        self.alloc_xload()
        xa = lambda kb: x_all[kb * 128:(kb + 1) * 128, :]
        xo = lambda i_: x_own[i_ * 128:(i_ + 1) * 128, :]
        nkb_tot = 2 * n_own
        seq = [xa(0), xa(1)]
        for i_ in range(n_own):
            seq.append(xo(i_))
            if 2 * i_ + 2 < nkb_tot:
                seq += [xa(2 * i_ + 2), xa(2 * i_ + 3)]
        self.set_x_sequence(seq)
        wk = sb("wk", [128, 8, 304], BF16)
        wq = sb("wq", [128, 8, 776], BF16)
        wuk = sb("wuk", [48, 8, 256], BF16)
        wuv = sb("wuv", [128, 2, 512], BF16)
        gkv = sb("gkv", [128, 256], F32)
        b_w = Buf("weights")

        self.dma(par[:], par_d[:, :], [], [b_small], b_small)
        self.dma(ropec[:], ropec_d[:, :], [], [b_small], b_small)
        self.dma(posa[:], pos_all[:, :], [], [b_small], b_small)
        self.dma(poso[:], pos_own[:, :], [], [b_small], b_small)
        self.dma(gkv[:], gkv_d.partition_broadcast(128).rearrange("p o r -> p (o r)"), [], [b_small], b_small)
        b_setup = Buf("setup")
        stg = [SCORE[:, k * 4096:(k + 1) * 4096].rearrange("p (c n) -> p c n", c=8) for k in range(2)]
        b_stg = [b_setup, b_setup]
        self.load_weight_bf16(wk, wk_d, 304, b_w, stg, b_stg)
        self.load_weight_bf16(wq, wq_d, 776, b_w, stg, b_stg)
        s0 = SCORE[0:48, 0:2048].rearrange("p (h r) -> p h r", h=8)
        self.dma(s0, wuk_d[:, :, :], [], [b_setup], b_setup)
        G(lambda e: e.tensor_copy(out=wuk[:], in_=s0), r=[b_setup], w=[b_w])
        s1 = SCORE[:, 4096:5120].rearrange("p (c n) -> p c n", c=2)
        self.dma(s1, wuv_d.rearrange("(c p) n -> p c n", p=128), [], [b_setup], b_setup)
        G(lambda e: e.tensor_copy(out=wuv[:], in_=s1), r=[b_setup], w=[b_w])
        V(lambda e: e.tensor_scalar(out=cpar[:, 0:1], in0=par[:], scalar1=-128.0, scalar2=None, op0=ALU.mult), r=[b_small], w=[b_small])
        V(lambda e: e.tensor_scalar(out=cpar[:, 1:2], in0=par[:], scalar1=-128.0, scalar2=128.0, op0=ALU.mult, op1=ALU.add), r=[b_small], w=[b_small])
        V(lambda e: e.tensor_scalar(out=cpar[:, 2:3], in0=par[:], scalar1=128.0, scalar2=None, op0=ALU.mult), r=[b_small], w=[b_small])
        G(lambda e: e.iota(tmpv[:, 0:8], pattern=[[-16, 8]], base=0, channel_multiplier=1, allow_small_or_imprecise_dtypes=True), w=[b_small])
        V(lambda e: e.tensor_scalar(out=tmpv[:, 8:16], in0=tmpv[:, 0:8], scalar1=0.0, scalar2=0.125, op0=ALU.is_ge, op1=ALU.mult), r=[b_small], w=[b_small])
        V(lambda e: e.tensor_scalar(out=tmpv[:, 0:8], in0=tmpv[:, 0:8], scalar1=15.0, scalar2=None, op0=ALU.is_le), r=[b_small], w=[b_small])
        V(lambda e: e.tensor_tensor(out=bm8[:], in0=tmpv[:, 0:8], in1=tmpv[:, 8:16], op=ALU.mult), r=[b_small], w=[b_small])
        scr = SCORE[:, 0:8192]
        self.rope_table(posa[:], NBLK, TABA, scr, b_setup, b_taba, ropec, b_small)
        self.rope_table(poso[:], NOWN, TABO, scr, b_setup, b_tabo, ropec, b_small)
        tok = V(lambda e: e.memset(SCORE[:, 0:2], 0.0), w=[b_setup])
        for k in range(16):
            b_score[k].w = tok

        ss = sb("ss", [128, 4], F32)
        b_ss = Buf("ss")
        ckvn = [sb("ckvn%d" % k, [128, 256], BF16) for k in range(2)]
        b_ckvn = [Buf("ckvn%d" % k) for k in range(2)]
        rtmp = sb("rtmp", [128, 2, 64], F32)
        b_rtmp = Buf("rtmp")
        krr = sb("krr", [128, 16], F32)
        krrep = sb("krrep", [128, 8, 16], BF16)
        ikr = sb("ikr", [128, 32], BF16)
        b_kr = Buf("kr")

        def kside(kb):
            xT, bxT = self.load_xT(x_all[kb * 128:(kb + 1) * 128, :])
            pk = self.bank[1]
            bpk = self.bbank[1]
            yield
            for c in range(8):
                T(lambda e, c=c: e.matmul(pk[:, 0:304], lhsT=xT[:, c, :], rhs=wk[:, c, :], start=(c == 0), stop=(c == 7)),
                  r=[bxT, b_w], w=[bpk])
            s = kb % 2
            A(lambda e: e.activation(out=junkA[:, 0:256], in_=pk[:, 0:256], func=AF.Square, accum_out=ss[:, 0:1]), r=[bpk], w=[b_junkA, b_ss])
            V(lambda e: e.tensor_scalar(out=ss[:, 1:2], in0=ss[:, 0:1], scalar1=1.0 / 256.0, scalar2=1e-6, op0=ALU.mult, op1=ALU.add), r=[b_ss], w=[b_ss])
            A(lambda e: e.sqrt(out=ss[:, 2:3], in_=ss[:, 1:2]), r=[b_ss], w=[b_ss])
            V(lambda e: e.reciprocal(out=ss[:, 3:4], in_=ss[:, 2:3]), r=[b_ss], w=[b_ss])
            V(lambda e: e.scalar_tensor_tensor(out=ckvn[s][:], in0=pk[:, 0:256], scalar=ss[:, 3:4], in1=gkv[:], op0=ALU.mult, op1=ALU.mult),
              r=[bpk, b_ss, b_small], w=[b_ckvn[s]])
            self.dma(ckv_d[kb * 128:(kb + 1) * 128, :], ckvn[s][:], [b_ckvn[s]], [b_ckvd[kb]], b_ckvd[kb], eng="gpsimd")
            cosA = TABA[:, kb, 0:8]
            sinA = TABA[:, kb, 8:16]
            cosI = TABA[:, kb, 16:20]
            sinI = TABA[:, kb, 20:24]
            self.rope(krr[:, 0:8], krr[:, 8:16], pk[:, 256:264], pk[:, 264:272], cosA, sinA, rtmp[:, 0, 0:8], rtmp[:, 1, 0:8],
                      [bpk, b_taba], [b_kr], b_rtmp)
            V(lambda e: e.tensor_copy(out=krrep[:], in_=krr[:].unsqueeze(1).to_broadcast([128, 8, 16])), r=[b_kr], w=[b_kr])
            self.rope(ikr[:, 0:4], ikr[:, 4:8], pk[:, 272:276], pk[:, 276:280], cosI, sinI, rtmp[:, 0, 0:4], rtmp[:, 1, 0:4],
                      [bpk, b_taba], [b_kr], b_rtmp)
            V(lambda e: e.tensor_copy(out=ikr[:, 8:32], in_=pk[:, 280:304]), r=[bpk], w=[b_kr])
            bv = self.bview(2)
            bb2 = self.bbank[2]
            yield
            for c in range(2):
                T(lambda e, c=c: e.transpose(out=bv[:, c * 128:(c + 1) * 128], in_=ckvn[s][:, c * 128:(c + 1) * 128], identity=self.identb[:]),
                  r=[b_ckvn[s], self.b_const], w=[bb2])
            yield
            T(lambda e: e.transpose(out=bv[:, 256:384], in_=krrep[:].rearrange("p h d -> p (h d)"), identity=self.identb[:]),
              r=[b_kr, self.b_const], w=[bb2])
            T(lambda e: e.transpose(out=bv[0:32, 384:512], in_=ikr[:], identity=self.identb[:]), r=[b_kr, self.b_const], w=[bb2])
            ksl = slice(kb * 128, (kb + 1) * 128)
            A(lambda e: e.activation(out=CKVT[:, :, ksl], in_=bv[:, 0:256].rearrange("p (c t) -> p c t", c=2), func=AF.Copy), r=[bb2], w=[b_kside[kb]])
            V(lambda e: e.tensor_copy(out=KRT[:, ksl], in_=bv[:, 256:384]), r=[bb2], w=[b_kside[kb]])
            V(lambda e: e.tensor_copy(out=IKT[0:32, ksl], in_=bv[0:32, 384:512]), r=[bb2], w=[b_kside[kb]])

        aqn = sb("aqn", [128, 8, 48], BF16)
        aqrp = sb("aqrp", [128, 8, 16], BF16)
        b_aq = Buf("aq")
        aqnT = sb("aqnT", [48, 8, 128], BF16)
        b_aqnT = Buf("aqnT")
        Qm2 = [sb("Qm%d" % k, [128, 8, 128], BF16) for k in range(2)]
        b_qm2 = [Buf("Qm%d" % k) for k in range(2)]
        QLT2 = [sb("QLT%d" % k, [128, 2, 8, 128], BF16) for k in range(2)]
        b_qlt2 = [Buf("QLT%d" % k) for k in range(2)]
        iqr = sb("iqr", [128, 8, 32], BF16)
        b_iq = Buf("iq")
        IQT = sb("IQT", [128, 8, 128], BF16)
        b_iqt = Buf("IQT")
        b_zpad = Buf("zpad")
        G(lambda e: e.memset(IQT[:], 0.0), w=[b_iqt, b_zpad])
        for q4 in range(4):
            G(lambda e, q4=q4: e.memset(IKT[:, q4 * 2048:(q4 + 1) * 2048], 0.0), w=[b_zpad] + [b_kside[k] for k in range(q4 * 16, (q4 + 1) * 16)])
        tbf = [sb("tbf%d" % k, [128, 512], BF16) for k in range(3)]
        b_tbf = [Buf("tbf%d" % k) for k in range(3)]
        wq16 = sb("wq16", [128, 8], F32)
        DGW = sb("DGW", [128, 8, 128], BF16)
        b_dgw = Buf("DGW")
        negi = sb("negi", [128, 128], BF16)
        V(lambda e: e.tensor_scalar(out=negi[:], in0=self.identf[:], scalar1=-30000.0, scalar2=None, op0=ALU.mult), r=[self.b_const], w=[self.b_const])
        junkA = sb("junkA", [128, 1024], BF16)
        b_junkA = Buf("junkA")
        bis2 = sb("bis2", [128, 8], F32)
        b_bis2 = Buf("bis2")
        bis = sb("bis", [128, 8], F32)
        b_bis = Buf("bis")
        DG = sb("DG", [128, 128], F32)
        THRB = sb("THRB", [128, 128], F32)
        b_thr = Buf("thr")
        mk1, b_mk1 = DG, b_thr
        PT = [sb("PT%d" % k, [128, 512], BF16) for k in range(3)]
        b_pt = [Buf("PT%d" % k) for k in range(3)]
        CKVs = [sb("CKVs%d" % k, [128, 8, 256], BF16) for k in range(2)]
        b_ckvs = [Buf("CKVs%d" % k) for k in range(2)]
        OT = sb("OT", [128, 2, 512], BF16)
        b_ot = Buf("OT")
        denr = sb("denr", [1, 512], F32)
        b_denr = Buf("denr")
        rden = sb("rden", [128, 4], F32)
        b_rden = Buf("rden")
        AOUT = [sb("AOUT0", [128, 512], BF16)] * 2
        b_aout = [Buf("AOUT0")] * 2
        self.cnt_ckvs = 0
        self.cnt_pt = 0
        self.cnt_tb = 0

        def qside(i):
            xT, bxT = self.load_xT(x_own[i * 128:(i + 1) * 128, :])
            Qm, b_qm, QLT, b_qlt = Qm2[i % 2], b_qm2[i % 2], QLT2[i % 2], b_qlt2[i % 2]
            p1, bp1 = self.bank[1], self.bbank[1]
            p3, bp3 = self.bank[0], self.bbank[0]
            yield
            for c in range(8):
                T(lambda e, c=c: e.matmul(p1[:, 0:512], lhsT=xT[:, c, :], rhs=wq[:, c, 0:512], start=(c == 0), stop=(c == 7)),
                  r=[bxT, b_w], w=[bp1])
            for c in range(8):
                T(lambda e, c=c: e.matmul(p3[:, 0:264], lhsT=xT[:, c, :], rhs=wq[:, c, 512:776], start=(c == 0), stop=(c == 7)),
                  r=[bxT, b_w], w=[bp3])
            qs = ""
            p1v = p1[:, 0:512].rearrange("p (h d) -> p h d", h=8)
            cosA = TABO[:, i, 0:8].unsqueeze(1).to_broadcast([128, 8, 8])
            sinA = TABO[:, i, 8:16].unsqueeze(1).to_broadcast([128, 8, 8])
            cosI = TABO[:, i, 16:20].unsqueeze(1).to_broadcast([128, 8, 4])
            sinI = TABO[:, i, 20:24].unsqueeze(1).to_broadcast([128, 8, 4])
            rt0 = rtmp[:, 0, 0:64].rearrange("p (h d) -> p h d", h=8)
            rt1 = rtmp[:, 1, 0:64].rearrange("p (h d) -> p h d", h=8)
            if qs != "q0b_norope" and qs != "q0b_none":
                self.rope(aqrp[:, :, 0:8], aqrp[:, :, 8:16], p1v[:, :, 0:8], p1v[:, :, 8:16], cosA, sinA, rt0, rt1, [bp1, b_tabo], [b_aq], b_rtmp)
            if qs != "q0b_nocopy" and qs != "q0b_none":
                A(lambda e: e.activation(out=aqn[:], in_=p1v[:, :, 16:64], func=AF.Copy), r=[bp1], w=[b_aq])
            if qs.startswith("q0b_"):
                return
            if qs == "q0b":
                return
            p3v = p3[:, 0:256].rearrange("p (h d) -> p h d", h=8)
            rt0i = rtmp[:, 0, 0:32].rearrange("p (h d) -> p h d", h=8)
            rt1i = rtmp[:, 1, 0:32].rearrange("p (h d) -> p h d", h=8)
            A(lambda e: e.activation(out=iqr[:, :, 8:32], in_=p3v[:, :, 8:32], func=AF.Copy), r=[bp3], w=[b_iq])
            self.rope(iqr[:, :, 0:4], iqr[:, :, 4:8], p3v[:, :, 0:4], p3v[:, :, 4:8], cosI, sinI, rt0i, rt1i, [bp3, b_tabo], [b_iq], b_rtmp)
            if qs == "q0c":
                return
            V(lambda e: e.tensor_scalar(out=wq16[:], in0=p3[:, 256:264], scalar1=1.0 / 16.0, scalar2=None, op0=ALU.mult), r=[bp3], w=[b_dgw])
            V(lambda e: e.tensor_tensor(out=DGW[:], in0=self.identf[:].unsqueeze(1).to_broadcast([128, 8, 128]),
                                        in1=wq16[:].unsqueeze(2).to_broadcast([128, 8, 128]), op=ALU.mult), r=[self.b_const], w=[b_dgw])
            if qs == "q1":
                return
            bv = self.bview(2)
            bb2 = self.bbank[2]
            yield
            for h in range(8):
                T(lambda e, h=h: e.transpose(out=bv[0:48, h * 128:(h + 1) * 128], in_=aqn[:, h, :], identity=self.identb[:]),
                  r=[b_aq, self.b_const], w=[bb2])
            A(lambda e: e.activation(out=aqnT[:], in_=bv[0:48, :].rearrange("p (h t) -> p h t", h=8), func=AF.Copy), r=[bb2], w=[b_aqnT])
            if qs == "q2":
                return
            yield
            T(lambda e: e.transpose(out=bv[:, 0:128], in_=aqrp[:].rearrange("p h d -> p (h d)"), identity=self.identb[:]),
              r=[b_aq, self.b_const, b_aqnT], w=[bb2])
            V(lambda e: e.tensor_tensor(out=Qm[:], in0=bv[:, 0:128].unsqueeze(1).to_broadcast([128, 8, 128]),
                                        in1=bm8[:].unsqueeze(2).to_broadcast([128, 8, 128]), op=ALU.mult),
              r=[bb2, b_small], w=[b_qm])
            if qs == "q3":
                return
            for h in range(8):
                T(lambda e, h=h: e.transpose(out=bv[0:32, h * 128:(h + 1) * 128], in_=iqr[:, h, :], identity=self.identb[:]),
                  r=[b_iq, self.b_const, b_qm], w=[bb2])
            A(lambda e: e.activation(out=IQT[0:32, :, :], in_=bv[0:32, :].rearrange("p (h t) -> p h t", h=8), func=AF.Copy), r=[bb2], w=[b_iqt])
            if qs == "q4":
                return
            yield
            for c in range(2):
                for hg in range(2):
                    bk = hg
                    for hh in range(4):
                        h = hg * 4 + hh
                        T(lambda e, c=c, h=h, hh=hh, bk=bk: e.matmul(self.bank[bk][:, hh * 128:(hh + 1) * 128],
                                                                     lhsT=wuk[:, h, c * 128:(c + 1) * 128], rhs=aqnT[:, h, :],
                                                                     start=True, stop=True),
                          r=[b_w, b_aqnT], w=[self.bbank[bk]])
                    A(lambda e, c=c, hg=hg, bk=bk: e.activation(out=QLT[:, c, hg * 4:(hg + 1) * 4, :],
                                                                in_=self.bank[bk][:].rearrange("p (h t) -> p h t", h=4),
                                                                func=AF.Copy, scale=0.125),
                      r=[self.bbank[bk]], w=[b_qlt])

        def score_and_threshold(i):
            nkb = 2 * i + 2
            nk = nkb * 128
            nch = (nk + 511) // 512
            for j in range(nch):
                wj = min(512, nk - 512 * j)
                sc = SCORE[:, 512 * j:512 * j + wj]
                abk = 2
                pacc, bpacc = self.bank[abk], self.bbank[abk]
                kbufs = [b_kside[k] for k in range(4 * j, 4 * j + wj // 128)]
                tt = {}

                def s_stage(h):
                    bk = 1 if (h % 2 == 0) else 0
                    pb, bpb = self.bank[bk], self.bbank[bk]
                    T(lambda e, pb=pb, h=h, wj=wj, j=j: e.matmul(pb[:, 0:wj], lhsT=IQT[:, h, :], rhs=IKT[:, 512 * j:512 * j + wj], start=True, stop=True),
                      r=[b_iqt] + kbufs, w=[bpb])
                    t = self.cnt_tb % 3
                    self.cnt_tb += 1
                    tt[h] = t
                    if h % 2 == 0:
                        A(lambda e, pb=pb, t=t, wj=wj: e.activation(out=tbf[t][:, 0:wj], in_=pb[:, 0:wj], func=AF.Relu), r=[bpb], w=[b_tbf[t]])
                    else:
                        V(lambda e, pb=pb, t=t, wj=wj: e.tensor_scalar(out=tbf[t][:, 0:wj], in0=pb[:, 0:wj], scalar1=0.0, scalar2=None, op0=ALU.max),
                          r=[bpb], w=[b_tbf[t]])

                def a_stage(h):
                    t = tt[h]
                    T(lambda e, h=h, t=t, wj=wj, pacc=pacc: e.matmul(pacc[:, 0:wj], lhsT=DGW[:, h, :], rhs=tbf[t][:, 0:wj], start=(h == 0), stop=(h == 7)),
                      r=[b_tbf[t], b_dgw], w=[bpacc])

                s_stage(0)
                for h in range(8):
                    if h + 1 < 8:
                        s_stage(h + 1)
                    a_stage(h)
                V(lambda e, sc=sc, pacc=pacc, wj=wj: e.tensor_copy(out=sc, in_=pacc[:, 0:wj]), r=[bpacc], w=[b_score[j]])
                yield
            allsc = [b_score[j] for j in range(nch)]
            V(lambda e: e.tensor_reduce(out=bis[:, 0:1], in_=SCORE[:, 0:nk], axis=AX.X, op=ALU.min), r=allsc, w=[b_bis])
            for t_, kb in enumerate((2 * i, 2 * i + 1)):
                blk = SCORE[:, kb * 128:(kb + 1) * 128]
                j = kb // 4
                V(lambda e, t_=t_: e.tensor_scalar(out=mk1[:], in0=self.idsrc[:], scalar1=cpar[:, t_:t_ + 1], scalar2=0.0, op0=ALU.add, op1=ALU.is_gt),
                  r=[self.b_const, b_small], w=[b_mk1])
                V(lambda e, blk=blk: e.scalar_tensor_tensor(out=blk, in0=mk1[:], scalar=NEG, in1=blk, op0=ALU.mult, op1=ALU.add),
                  r=[b_mk1], w=[b_score[j]])
            V(lambda e: e.tensor_reduce(out=bis[:, 1:2], in_=SCORE[:, 0:nk], axis=AX.X, op=ALU.max), r=allsc, w=[b_bis])
            V(lambda e: e.tensor_tensor(out=bis[:, 2:3], in0=bis[:, 1:2], in1=bis[:, 0:1], op=ALU.subtract), r=[b_bis], w=[b_bis])
            split = nk
            n2 = nk - split
            na = (n2 + 1023) // 1024
            for it in range(1, N_BISECT + 1):
                f = 2.0 ** (-it)
                V(lambda e, f=f: e.tensor_scalar(out=bis[:, 3:4], in0=bis[:, 2:3], scalar1=f, scalar2=bis[:, 0:1], op0=ALU.mult, op1=ALU.add),
                  r=[b_bis], w=[b_bis])
                if n2 > 0:
                    V(lambda e: e.tensor_scalar(out=bis[:, 6:7], in0=bis[:, 3:4], scalar1=-1.0, scalar2=None, op0=ALU.mult), r=[b_bis], w=[b_bis])
                    for ci in range(na):
                        c0 = split + ci * 1024
                        cw = min(1024, nk - c0)
                        A(lambda e, c0=c0, cw=cw, ci=ci: e.activation(out=junkA[:, 0:cw], in_=SCORE[:, c0:c0 + cw], func=AF.Sign, bias=bis[:, 6:7],
                                                                      accum_out=bis2[:, ci:ci + 1]), r=allsc + [b_bis], w=[b_junkA, b_bis2])
                first = True
                for c0 in range(0, split, 1024):
                    cw = min(1024, split - c0)
                    if first:
                        V(lambda e, c0=c0, cw=cw: e.tensor_scalar(out=junk[:, 0:cw], in0=SCORE[:, c0:c0 + cw], scalar1=bis[:, 3:4], scalar2=None,
                                                                  op0=ALU.is_ge, op1=ALU.add, accum_out=bis[:, 4:5]),
                          r=allsc + [b_bis], w=[b_junk, b_bis])
                    else:
                        V(lambda e, c0=c0, cw=cw: e.tensor_scalar(out=junk[:, 0:cw], in0=SCORE[:, c0:c0 + cw], scalar1=bis[:, 3:4], scalar2=bis[:, 4:5],
                                                                  op0=ALU.is_ge, op1=ALU.add, accum_out=bis[:, 4:5]),
                          r=allsc + [b_bis], w=[b_junk, b_bis])
                    first = False
                thr_cnt = 255.5
                if n2 > 0:
                    V(lambda e: e.tensor_reduce(out=bis[:, 7:8], in_=bis2[:, 0:na], axis=AX.X, op=ALU.add), r=[b_bis2], w=[b_bis])
                    V(lambda e: e.scalar_tensor_tensor(out=bis[:, 4:5], in0=bis[:, 7:8], scalar=0.5, in1=bis[:, 4:5], op0=ALU.mult, op1=ALU.add),
                      r=[b_bis], w=[b_bis])
                    thr_cnt = 255.5 - 0.5 * n2
                V(lambda e, f=f, thr_cnt=thr_cnt: e.tensor_scalar(out=bis[:, 5:6], in0=bis[:, 4:5], scalar1=thr_cnt, scalar2=f, op0=ALU.is_ge, op1=ALU.mult),
                  r=[b_bis], w=[b_bis])
                V(lambda e: e.scalar_tensor_tensor(out=bis[:, 0:1], in0=bis[:, 5:6], scalar=bis[:, 2:3], in1=bis[:, 0:1], op0=ALU.mult, op1=ALU.add),
                  r=[b_bis], w=[b_bis])
                yield

        def masks(i):
            nkb = 2 * i + 2
            V(lambda e: e.tensor_scalar(out=DG[:], in0=self.identf[:], scalar1=bis[:, 0:1], scalar2=None, op0=ALU.mult), r=[b_bis, self.b_const], w=[b_thr])
            T(lambda e: e.matmul(self.bank[2][:, 0:128], lhsT=self.onesf[:], rhs=DG[:], start=True, stop=True), r=[b_thr, self.b_const], w=[self.bbank[2]])
            A(lambda e: e.activation(out=THRB[:], in_=self.bank[2][:, 0:128], func=AF.Copy), r=[self.bbank[2]], w=[b_thr])
            g = 0
            for kb0 in range(0, nkb, 4):
                n = min(4, nkb - kb0)
                bk = 1 if (g % 2 == 0) else 0
                g += 1
                for t_ in range(n):
                    kb = kb0 + t_
                    T(lambda e, bk=bk, t_=t_, kb=kb: e.transpose(out=self.bank[bk][:, t_ * 128:(t_ + 1) * 128], in_=SCORE[:, kb * 128:(kb + 1) * 128],
                                                                 identity=self.identf[:]),
                      r=[b_score[kb // 4], self.b_const], w=[self.bbank[bk]])
                V(lambda e, bk=bk, n=n, kb0=kb0: e.tensor_tensor(out=MT[:, kb0:kb0 + n, :],
                                                                 in0=self.bank[bk][:, 0:n * 128].rearrange("p (k t) -> p k t", k=n),
                                                                 in1=THRB[:].unsqueeze(1).to_broadcast([128, n, 128]), op=ALU.is_lt),
                  r=[self.bbank[bk], b_thr], w=[b_mt])

        def attention(i):
            nkb = 2 * i + 2
            ao, bao = AOUT[i % 2], b_aout[i % 2]
            Qm, b_qm, QLT, b_qlt = Qm2[i % 2], b_qm2[i % 2], QLT2[i % 2], b_qlt2[i % 2]
            for hg in range(2):
                p_o = [self.bank[4], self.bank[5]]
                bp_o = [self.bbank[4], self.bbank[5]]
                p_d, bp_d = self.bank[3], self.bbank[3]
                hs = slice(hg * 4, (hg + 1) * 4)
                state = {}

                def S_stage(kb):
                    if kb % 8 == 0:
                        s_ = self.cnt_ckvs % 2
                        self.cnt_ckvs += 1
                        n8 = min(8, nkb - kb)
                        self.dma(CKVs[s_][:, 0:n8, :], ckv_d[kb * 128:(kb + n8) * 128, :].rearrange("(k p) r -> p k r", p=128),
                                 [b_ckvd[k] for k in range(kb, kb + n8)], [b_ckvs[s_]], b_ckvs[s_])
                        state["cur%d" % (kb // 8)] = s_
                    pk = 6 + (kb % 2)
                    pst, bpst = self.bank[pk], self.bbank[pk]
                    ksl = slice(kb * 128, (kb + 1) * 128)
                    T(lambda e, pst=pst, ksl=ksl, hs=hs: e.matmul(pst[:], lhsT=CKVT[:, 0, ksl], rhs=QLT[:, 0, hs, :].rearrange("p h t -> p (h t)"),
                                                           start=True, stop=False), r=[b_kside[kb], b_qlt], w=[bpst])
                    T(lambda e, pst=pst, ksl=ksl, hs=hs: e.matmul(pst[:], lhsT=CKVT[:, 1, ksl], rhs=QLT[:, 1, hs, :].rearrange("p h t -> p (h t)"),
                                                           start=False, stop=False), r=[b_kside[kb], b_qlt], w=[bpst])
                    T(lambda e, pst=pst, ksl=ksl, hs=hs: e.matmul(pst[:], lhsT=KRT[:, ksl], rhs=Qm[:, hs, :].rearrange("p h t -> p (h t)"),
                                                           start=False, stop=False), r=[b_kside[kb], b_qm], w=[bpst])
                    T(lambda e, pst=pst, kb=kb: e.matmul(pst[:].rearrange("p (h t) -> p h t", h=4), lhsT=negi[:],
                                                         rhs=MT[:, kb, :].unsqueeze(1).to_broadcast([128, 4, 128]), start=False, stop=True),
                      r=[b_mt, self.b_const], w=[bpst])
                    t = self.cnt_pt % 3
                    self.cnt_pt += 1
                    state["t%d" % kb] = t
                    A(lambda e, pst=pst, t=t: e.activation(out=PT[t][:], in_=pst[:], func=AF.Exp), r=[bpst], w=[b_pt[t]])

                def O_stage(kb):
                    t = state["t%d" % kb]
                    cur = state["cur%d" % (kb // 8)]
                    kk = kb % 8
                    for c in range(2):
                        T(lambda e, c=c, t=t, kk=kk, cur=cur, kb=kb: e.matmul(p_o[c][:], lhsT=CKVs[cur][:, kk, c * 128:(c + 1) * 128], rhs=PT[t][:],
                                                                              start=(kb == 0), stop=(kb == nkb - 1)),
                          r=[b_ckvs[cur], b_pt[t]], w=[bp_o[c]])
                    T(lambda e, t=t, kb=kb: e.matmul(p_d[0:1, :], lhsT=self.onesb[:, 0:1], rhs=PT[t][:], start=(kb == 0), stop=(kb == nkb - 1)),
                      r=[b_pt[t], self.b_const], w=[bp_d])

                S_stage(0)
                for kb in range(nkb):
                    if kb + 1 < nkb:
                        S_stage(kb + 1)
                    O_stage(kb)
                    yield
                A(lambda e: e.activation(out=denr[:], in_=p_d[0:1, :], func=AF.Copy), r=[bp_d], w=[b_denr])
                A(lambda e: e.activation(out=OT[:, 0, :], in_=p_o[0][:], func=AF.Copy), r=[bp_o[0]], w=[b_ot])
                A(lambda e: e.activation(out=OT[:, 1, :], in_=p_o[1][:], func=AF.Copy), r=[bp_o[1]], w=[b_ot])
                p3, bp3 = self.bank[6], self.bbank[6]
                for hh in range(4):
                    T(lambda e, hh=hh: e.matmul(p3[:, hh:hh + 1], lhsT=denr[0:1, hh * 128:(hh + 1) * 128], rhs=self.onesf[0:1, 0:1], start=True, stop=True),
                      r=[b_denr, self.b_const], w=[bp3])
                A(lambda e: e.activation(out=rden[:], in_=p3[:, 0:4], func=AF.Ln), r=[bp3], w=[b_rden])
                A(lambda e: e.activation(out=rden[:], in_=rden[:], func=AF.Exp, scale=-1.0), r=[b_rden], w=[b_rden])
                p1, bp1 = self.bank[7], self.bbank[7]
                for hh in range(4):
                    h = hg * 4 + hh
                    for c in range(2):
                        T(lambda e, hh=hh, h=h, c=c: e.matmul(p1[:, hh * 64:(hh + 1) * 64], lhsT=OT[:, c, hh * 128:(hh + 1) * 128],
                                                              rhs=wuv[:, c, h * 64:(h + 1) * 64], start=(c == 0), stop=(c == 1)),
                          r=[b_ot, b_w], w=[bp1])
                for hh in range(4):
                    A(lambda e, hg=hg, hh=hh: e.activation(out=ao[:, hg * 256 + hh * 64:hg * 256 + (hh + 1) * 64], in_=p1[:, hh * 64:(hh + 1) * 64],
                                                           func=AF.Copy, scale=rden[:, hh:hh + 1]), r=[bp1, b_rden], w=[bao])
                yield
            self.dma(self.aout_d[i * 128:(i + 1) * 128, :], ao[:], [bao], [self.b_aoutd], self.b_aoutd, eng="gpsimd")

        def stage_a(i):
            yield from qside(i)
            yield
            gen = score_and_threshold(i)
            nch_ = ((2 * i + 2) * 128 + 511) // 512
            for _ in range(nch_):
                next(gen)
                yield
            if 2 * i + 2 < 2 * n_own:
                yield from kside(2 * i + 2)
                yield
                yield from kside(2 * i + 3)
                yield
            else:
                for _ in range(8):
                    yield
            yield from gen

        def n_units_a(i):
            nk = (2 * i + 2) * 128
            return 13 + (nk + 511) // 512 + N_BISECT

        def n_units_b(i):
            return 2 * (2 * i + 2) + 2

        for kb0 in (0, 1):
            for _ in kside(kb0):
                pass
        for _ in stage_a(0):
            pass
        masks(0)
        for i in range(n_own):
            gb = attention(i)
            if i + 1 < n_own:
                ga = stage_a(i + 1)
                nb = n_units_b(i)
                na_front = n_units_a(i + 1) - N_BISECT
                done_a = done_b = False
                ca = cb = 0
                while not (done_a and done_b):
                    if not done_a and ca >= na_front:
                        for _ in ga:
                            pass
                        done_a = True
                    elif not done_a and (done_b or ca * nb * 0.12 <= cb * na_front):
                        try:
                            next(ga)
                        except StopIteration:
                            done_a = True
                        ca += 1
                    else:
                        try:
                            next(gb)
                        except StopIteration:
                            done_b = True
                        cb += 1
                masks(i + 1)
            else:
                for _ in gb:
                    pass
        self.mark = mark

    def alloc_xload(self):
        sb = self.sb
        self.xf = [sb("xf%d" % k, [128, D], F32) for k in range(2)]
        self.xT = [sb("xT%d" % k, [128, 8, 128], BF16) for k in range(2)]
        self.b_xf = [Buf("xf%d" % k) for k in range(2)]
        self.b_xT = [Buf("xT%d" % k) for k in range(2)]

    def phase1b(self):
        V, A, G, T = self.V, self.A, self.G, self.T
        sb = self.sb
        n_own = self.n_own
        x_all, x_own = self.x_all, self.x_own
        TABA, TABO, b_taba, b_tabo = self.TABA, self.TABO, self.b_taba, self.b_tabo
        cpar, b_small = self.cpar, self.b_small
        wbq_d = self.dram_in("w_bq", [D, 768], F32)
        wbk_d = self.dram_in("w_bk", [D, 768], F32)
        wbv_d = self.dram_in("w_bv", [D, 768], F32)
        if self.debug:
            self.bout_d = self.dram_out("bout_d", [NOWN * 128, 256], BF16)
        else:
            self.bout_d = self.dram_tmp("bout_d", [NOWN * 128, 256], BF16)
        self.b_boutd = Buf("boutd")
        RING = 20
        wbq = sb("wbq", [128, 8, 768], BF16)
        wbk = sb("wbk", [128, 8, 768], BF16)
        wbv = sb("wbv", [128, 8, 768], BF16)
        b_w = Buf("wB")
        stg = [sb("stgB%d" % k, [128, 8, 512], F32) for k in range(2)]
        b_stg = [Buf("stgB%d" % k) for k in range(2)]
        self.alloc_xload()
        seq = []
        for i_ in range(n_own):
            seq += [x_all[(2 * i_) * 128:(2 * i_ + 1) * 128, :], x_all[(2 * i_ + 1) * 128:(2 * i_ + 2) * 128, :], x_own[i_ * 128:(i_ + 1) * 128, :]]
        self.set_x_sequence(seq)
        BKT = sb("BKT", [128, 6, RING, 128], BF16)
        BV = sb("BV", [128, RING, 12, 65], BF16)
        b_slot = [Buf("bslot%d" % k) for k in range(RING)]
        bkr = sb("bkr", [128, 12, 64], BF16)
        b_bkr = Buf("bkr")
        bqr = sb("bqr", [128, 12, 64], BF16)
        b_bqr = Buf("bqr")
        BQT2 = [sb("BQT%d" % k, [128, 2, 6, 128], BF16) for k in range(2)]
        b_bqt2 = [Buf("BQT%d" % k) for k in range(2)]
        RMs = sb("RMs", [128, 4], F32)
        b_rm = Buf("RMs")
        G(lambda e: e.iota(RMs[:, 2:3], pattern=[[0, 1]], base=0, channel_multiplier=1, allow_small_or_imprecise_dtypes=True), w=[b_rm])
        V(lambda e: e.tensor_scalar(out=RMs[:, 0:1], in0=RMs[:, 2:3], scalar1=63.5, scalar2=0.125, op0=ALU.is_lt, op1=ALU.mult), r=[b_rm], w=[b_rm])
        V(lambda e: e.tensor_scalar(out=RMs[:, 1:2], in0=RMs[:, 2:3], scalar1=63.5, scalar2=0.125, op0=ALU.is_gt, op1=ALU.mult), r=[b_rm], w=[b_rm])
        rtmp = sb("rtmpB", [128, 2, 64], F32)
        b_rtmp = Buf("rtmpB")
        MD = sb("MD", [128, 3, 128], F32)
        MDb = sb("MDb", [128, 3, 128], BF16)
        b_md = Buf("MD")
        vf = sb("vf", [128, 128], F32)
        vf2 = sb("vf2", [128, 128], F32)
        vi = sb("vi", [128, 128], I32)
        dl = sb("dl", [128, 128], F32)
        m1 = sb("m1", [128, 128], F32)
        b_dl = Buf("dl")
        MK = [sb("MK%d" % k, [128, 128], BF16) for k in range(4)]
        b_mk = [Buf("MK%d" % k) for k in range(4)]
        PT = [sb("PTb%d" % k, [128, 512], BF16) for k in range(2)]
        b_pt = [Buf("PTb%d" % k) for k in range(2)]
        PTm = [sb("PTmb%d" % k, [128, 512], BF16) for k in range(2)]
        b_ptm = [Buf("PTmb%d" % k) for k in range(2)]
        bout = sb("bout", [128, 256], BF16)
        b_bout = Buf("bout")
        rden = sb("rdenB", [128, 4], F32)
        b_rden = Buf("rdenB")

        self.load_weight_bf16(wbq, wbq_d, 768, b_w, stg, b_stg)
        self.load_weight_bf16(wbk, wbk_d, 768, b_w, stg, b_stg)
        self.load_weight_bf16(wbv, wbv_d, 768, b_w, stg, b_stg)
        G(lambda e: e.memset(BV[:, :, :, 64:65], 1.0), w=b_slot)
        V(lambda e: e.memset(MD[:, 0, :], 1.0), w=[b_md])
        for g, dil in ((1, 4), (2, 16)):
            V(lambda e, dil=dil: e.tensor_scalar(out=vf[:], in0=self.idsrc[:], scalar1=128.0, scalar2=1.0 / dil, op0=ALU.add, op1=ALU.mult),
              r=[self.b_const], w=[b_dl])
            V(lambda e: e.tensor_copy(out=vi[:], in_=vf[:]), w=[b_dl])
            V(lambda e: e.tensor_copy(out=vf2[:], in_=vi[:]), w=[b_dl])
            V(lambda e, g=g: e.tensor_tensor(out=MD[:, g, :], in0=vf[:], in1=vf2[:], op=ALU.is_equal), r=[b_dl], w=[b_md])
        V(lambda e: e.tensor_copy(out=MDb[:], in_=MD[:]), r=[b_md], w=[b_md])

        def proj768(xT, bxT, w, banks):
            for (bk, c0, cw) in ((banks[0], 0, 512), (banks[1], 512, 256)):
                for c in range(8):
                    T(lambda e, bk=bk, c=c, c0=c0, cw=cw: e.matmul(self.bank[bk][:, 0:cw], lhsT=xT[:, c, :], rhs=w[:, c, c0:c0 + cw],
                                                                   start=(c == 0), stop=(c == 7)),
                      r=[bxT, b_w], w=[self.bbank[bk]])

        def rope_heads(dst, banks, tab, blk, b_tab, b_dst):
            for (bk, hs, nh) in ((banks[0], 0, 8), (banks[1], 8, 4)):
                pv = self.bank[bk][:, 0:nh * 64].rearrange("p (h d) -> p h d", h=nh)
                cos = tab[:, blk, 0:8].unsqueeze(1).to_broadcast([128, nh, 8])
                sin = tab[:, blk, 8:16].unsqueeze(1).to_broadcast([128, nh, 8])
                tA = rtmp[:, 0, 0:nh * 8].rearrange("p (h d) -> p h d", h=nh)
                tB = rtmp[:, 1, 0:nh * 8].rearrange("p (h d) -> p h d", h=nh)
                self.rope(dst[:, hs:hs + nh, 0:8], dst[:, hs:hs + nh, 8:16], pv[:, :, 0:8], pv[:, :, 8:16], cos, sin, tA, tB,
                          [self.bbank[bk], b_tab], [b_dst], b_rtmp)
                A(lambda e, pv=pv, hs=hs, nh=nh: e.activation(out=dst[:, hs:hs + nh, 16:64], in_=pv[:, :, 16:64], func=AF.Copy),
                  r=[self.bbank[bk]], w=[b_dst])

        def kside(kb):
            slot = kb % RING
            xT, bxT = self.load_xT(x_all[kb * 128:(kb + 1) * 128, :])
            proj768(xT, bxT, wbk, (1, 3))
            rope_heads(bkr, (1, 3), TABA, kb, b_taba, b_bkr)
            bv2 = self.bview(2)
            for c in range(6):
                T(lambda e, c=c: e.transpose(out=bv2[:, c * 128:(c + 1) * 128], in_=bkr[:, 2 * c:2 * c + 2, :].rearrange("p h d -> p (h d)"),
                                             identity=self.identb[:]), r=[b_bkr, self.b_const], w=[self.bbank[2]])
            V(lambda e: e.tensor_copy(out=BKT[:, :, slot, :], in_=bv2[:, 0:768].rearrange("p (c t) -> p c t", c=6)),
              r=[self.bbank[2]], w=[b_slot[slot]])
            proj768(xT, bxT, wbv, (4, 5))
            A(lambda e: e.activation(out=BV[:, slot, 0:8, 0:64], in_=self.bank[4][:, 0:512].rearrange("p (h d) -> p h d", h=8), func=AF.Copy),
              r=[self.bbank[4]], w=[b_slot[slot]])
            V(lambda e: e.tensor_copy(out=BV[:, slot, 8:12, 0:64], in_=self.bank[5][:, 0:256].rearrange("p (h d) -> p h d", h=4)),
              r=[self.bbank[5]], w=[b_slot[slot]])

        def qside(i):
            xT, bxT = self.load_xT(x_own[i * 128:(i + 1) * 128, :])
            proj768(xT, bxT, wbq, (1, 3))
            rope_heads(bqr, (1, 3), TABO, i, b_tabo, b_bqr)
            bv2 = self.bview(2)
            for c in range(6):
                T(lambda e, c=c: e.transpose(out=bv2[:, c * 128:(c + 1) * 128], in_=bqr[:, 2 * c:2 * c + 2, :].rearrange("p h d -> p (h d)"),
                                             identity=self.identb[:]), r=[b_bqr, self.b_const], w=[self.bbank[2]])
            A(lambda e: e.activation(out=BQT2[i % 2][:, 0, :, :], in_=bv2[:, 0:768].rearrange("p (c t) -> p c t", c=6), func=AF.Copy, scale=RMs[:, 0:1]),
              r=[self.bbank[2], b_rm], w=[b_bqt2[i % 2]])
            V(lambda e: e.tensor_scalar(out=BQT2[i % 2][:, 1, :, :], in0=bv2[:, 0:768].rearrange("p (c t) -> p c t", c=6), scalar1=RMs[:, 1:2], scalar2=None,
                                        op0=ALU.mult), r=[self.bbank[2], b_rm], w=[b_bqt2[i % 2]])

        self.cnt_b = 0
        self.cnt_mk = 0
        WIN = (128.0, 512.0, 2048.0)
        WB = (1, 4, 16)

        def attention(i):
            pairs = []
            for g in range(3):
                for kb in range(2 * i - WB[g], 2 * i + 2):
                    if kb >= 0:
                        pairs.append((g, kb))
            p_o, bp_o = self.bank[6], self.bbank[6]
            npairs = len(pairs)
            tsel = {}
            BQT, b_bqt = BQT2[i % 2], b_bqt2[i % 2]

            def qk_stage(n):
                g, kb = pairs[n]
                slot = kb % RING
                t = self.cnt_b % 2
                self.cnt_b += 1
                tsel[n] = t
                pk = 7 if t == 0 else 0
                for hh in range(4):
                    h = 4 * g + hh
                    c, z = h // 2, h % 2
                    T(lambda e, pk=pk, hh=hh, c=c, z=z, slot=slot: e.matmul(self.bank[pk][:, hh * 128:(hh + 1) * 128], lhsT=BKT[:, c, slot, :],
                                                                            rhs=BQT[:, z, c, :], start=True, stop=True),
                      r=[b_slot[slot], b_bqt], w=[self.bbank[pk]])
                A(lambda e, t=t, pk=pk: e.activation(out=PT[t][:], in_=self.bank[pk][:], func=AF.Exp), r=[self.bbank[pk]], w=[b_pt[t]])
                d_rel = kb - 2 * i
                interior = (g == 1 and d_rel in (-2, -1)) or (g == 2 and -14 <= d_rel <= -1)
                if interior:
                    mask = MDb[:, g, :]
                    rmask = [b_md]
                else:
                    m = self.cnt_mk % 4
                    self.cnt_mk += 1
                    V(lambda e, d_rel=d_rel: e.tensor_scalar(out=dl[:], in0=self.idsrc[:], scalar1=cpar[:, 2:3], scalar2=-128.0 * d_rel,
                                                             op0=ALU.add, op1=ALU.add), r=[self.b_const, b_small], w=[b_dl])
                    V(lambda e, g=g: e.scalar_tensor_tensor(out=m1[:], in0=dl[:], scalar=0.0, in1=MD[:, g, :], op0=ALU.is_ge, op1=ALU.mult),
                      r=[b_md], w=[b_dl])
                    V(lambda e, g=g, m=m: e.scalar_tensor_tensor(out=MK[m][:], in0=dl[:], scalar=WIN[g], in1=m1[:], op0=ALU.is_le, op1=ALU.mult),
                      r=[b_dl], w=[b_mk[m]])
                    mask = MK[m][:]
                    rmask = [b_mk[m]]
                eng = V
                eng(lambda e, t=t, mask=mask: e.tensor_tensor(out=PTm[t][:].rearrange("p (h t) -> p h t", h=4),
                                                              in0=PT[t][:].rearrange("p (h t) -> p h t", h=4),
                                                              in1=mask.unsqueeze(1).to_broadcast([128, 4, 128]), op=ALU.mult),
                    r=[b_pt[t]] + rmask, w=[b_ptm[t]])

            def pv_stage(n):
                g, kb = pairs[n]
                slot = kb % RING
                t = tsel[n]
                for hh in range(4):
                    ci = hh
                    T(lambda e, t=t, hh=hh, slot=slot, g=g, n=n, ci=ci: e.matmul(p_o[:, hh * 65:(hh + 1) * 65], lhsT=PTm[t][:, ci * 128:(ci + 1) * 128],
                                                                          rhs=BV[:, slot, 4 * g + hh, :], start=(n == 0 and hh == 0),
                                                                          stop=(n == npairs - 1 and hh == 3), skip_group_check=True),
                      r=[b_ptm[t], b_slot[slot]], w=[bp_o])

            qk_stage(0)
            for n in range(npairs):
                if n + 1 < npairs:
                    qk_stage(n + 1)
                pv_stage(n)
                yield
            if st1b in ("attn_qk", "attn_mask", "attn_pv"):
                return
            pov = p_o[:, 0:260].rearrange("p (h d) -> p h d", h=4)
            V(lambda e: e.reciprocal(out=rden[:].unsqueeze(2), in_=pov[:, :, 64:65]), r=[bp_o], w=[b_rden])
            V(lambda e: e.tensor_tensor(out=bout[:].rearrange("p (h d) -> p h d", h=4), in0=pov[:, :, 0:64],
                                        in1=rden[:].unsqueeze(2).to_broadcast([128, 4, 64]), op=ALU.mult), r=[bp_o, b_rden], w=[b_bout])
            self.dma(self.bout_d[i * 128:(i + 1) * 128, :], bout[:], [b_bout], [self.b_boutd], self.b_boutd, eng="gpsimd")

        st1b = ""

        def stage_a(i):
            kside(2 * i)
            yield
            kside(2 * i + 1)
            yield
            qside(i)
            yield

        for _ in stage_a(0):
            pass
        for i in range(n_own):
            gb = attention(i)
            if i + 1 < n_own:
                ga = stage_a(i + 1)
                nb = sum(1 for g in range(3) for kb in range(2 * i - WB[g], 2 * i + 2) if kb >= 0) + 1
                na = 3
                done_a = done_b = False
                ca = cb = 0
                while not (done_a and done_b):
                    if not done_a and (done_b or ca * nb <= cb * na):
                        try:
                            next(ga)
                        except StopIteration:
                            done_a = True
                        ca += 1
                    else:
                        try:
                            next(gb)
                        except StopIteration:
                            done_b = True
                        cb += 1
            else:
                for _ in gb:
                    pass

    def load_bcast(self, dst, src_row_ap, b_dst):
        self.dma(dst, src_row_ap.partition_broadcast(128).rearrange("p o n -> p (o n)"), [], [b_dst], b_dst)

    def phase2(self):
        V, A, G, T = self.V, self.A, self.G, self.T
        sb = self.sb
        n_own = self.n_own
        C = self.CAP
        NSLOT = 64 * C
        ALPHA = 2.0 ** 0.25
        di = self.dram_in
        wga_d, wgb_d = di("w_ga", [D, D], F32), di("w_gb", [D, D], F32)
        bgate_d = di("b_gate", [1, 2 * D], F32)
        wba_d, wbb_d, wo_d = di("w_branch_a", [512, D], F32), di("w_branch_b", [256, D], F32), di("w_o", [D, D], F32)
        ln1g_d, ln1b_d = di("ln1_g", [1, D], F32), di("ln1_b", [1, D], F32)
        wr_d, rb_d = di("w_router", [D, 64], F32), di("router_bias", [1, 64], F32)
        ws1_d, ws3_d, ws2_d = di("ws1", [D, 256], F32), di("ws3", [D, 256], F32), di("ws2", [256, D], F32)
        self.base_d = self.dram_tmp("base_d", [NOWN * 128, D], F32)
        self.b_based = Buf("based")
        if self.debug:
            self.h_d = self.dram_out("h_d", [NOWN * 128, D], F32)
            self.b_hd = Buf("hd")

        wga, wgb = sb("wga", [128, 8, D], BF16), sb("wgb", [128, 8, D], BF16)
        wba, wbb, wo = sb("wba", [128, 4, D], BF16), sb("wbb", [128, 2, D], BF16), sb("wo", [128, 8, D], BF16)
        wr = sb("wr", [128, 8, 64], BF16)
        ws1, ws3, ws2 = sb("ws1", [128, 8, 256], BF16), sb("ws3", [128, 8, 256], BF16), sb("ws2", [128, 2, D], BF16)
        b_w = Buf("w2")
        stg = [sb("stg2%d" % k, [128, 8, 512], F32) for k in range(2)]
        b_stg = [Buf("stg2%d" % k) for k in range(2)]
        bgate = sb("bgate", [128, 2 * D], F32)
        ln1g, ln1b = sb("ln1g", [128, D], F32), sb("ln1b", [128, D], F32)
        rbias = sb("rbias", [128, 64], F32)
        b_vec = Buf("vec2")
        self.alloc_xload()
        self.set_x_sequence([self.x_own[i_ * 128:(i_ + 1) * 128, :] for i_ in range(n_own)])
        gate = [sb("gate%d" % k, [128, D], F32) for k in range(2)]
        b_gate = [Buf("gate%d" % k) for k in range(2)]
        gtmp = [sb("gtmp%d" % k, [128, 512], F32) for k in range(2)]
        b_gtmp = [Buf("gtmp%d" % k) for k in range(2)]
        ab = sb("ab", [128, 768], BF16)
        b_ab = Buf("ab")
        abT = sb("abT", [128, 6, 128], BF16)
        b_abT = Buf("abT")
        mm = sb("mm", [128, D], BF16)
        b_mm = Buf("mm")
        mT = sb("mT", [128, 8, 128], BF16)
        b_mT = Buf("mT")
        u = sb("u", [128, D], F32)
        b_u = Buf("u")
        junkf = sb("junk2", [128, D], F32)
        b_junkf = Buf("junk2")
        st = sb("st2", [128, 8], F32)
        b_st = Buf("st2")
        h2 = [sb("h%d" % k, [128, D], F32) for k in range(2)]
        b_h2 = [Buf("h%d" % k) for k in range(2)]
        hb = [sb("hb%d" % k, [128, D], BF16) for k in range(2)]
        b_hb = [Buf("hb%d" % k) for k in range(2)]
        hT2 = [sb("hT%d" % k, [128, 8, 128], BF16) for k in range(2)]
        b_hT2 = [Buf("hT%d" % k) for k in range(2)]
        EM = sb("EM", [128, NOWN, 64], BF16)
        b_em = Buf("EM")
        UT = sb("UT", [128, 128], BF16)
        eoff = sb("eoff", [128, 64], F32)
        b_c2 = Buf("c2")
        rt = sb("rt", [128, 12, 64], F32)
        b_rt = Buf("rt")
        rs = sb("rs", [128, 64], F32)
        b_rs = Buf("rs")
        s8f = sb("s8f", [128, 8], F32)
        sil = sb("sil", [128, 256], F32)
        b_sil = Buf("sil")
        GT = sb("GT", [128, 256], BF16)
        b_gt = Buf("GT")
        base = sb("base", [128, D], F32)
        b_base = Buf("base")

        def loadw(dst, src, rows, cols):
            nchunk = rows // 128
            srcv = src.rearrange("(c p) n -> p c n", p=128)
            k = 0
            for c0 in range(0, cols, 512):
                cw = min(512, cols - c0)
                sidx = k % 2
                k += 1
                self.dma(stg[sidx][:, 0:nchunk, 0:cw], srcv[:, :, c0:c0 + cw], [], [b_stg[sidx]], b_stg[sidx])
                self.cast_rr(dst[:, :, c0:c0 + cw], stg[sidx][:, 0:nchunk, 0:cw], [b_stg[sidx]], [b_w])
        loadw(wga, wga_d, D, D)
        loadw(wgb, wgb_d, D, D)
        loadw(wba, wba_d, 512, D)
        loadw(wbb, wbb_d, 256, D)
        loadw(wo, wo_d, D, D)
        loadw(wr, wr_d, D, 64)
        loadw(ws1, ws1_d, D, 256)
        loadw(ws3, ws3_d, D, 256)
        loadw(ws2, ws2_d, 256, D)
        self.load_bcast(bgate, bgate_d, b_vec)
        self.load_bcast(ln1g, ln1g_d, b_vec)
        self.load_bcast(ln1b, ln1b_d, b_vec)
        self.load_bcast(rbias, rb_d, b_vec)
        V(lambda e: e.tensor_scalar(out=UT[:], in0=self.idsrc[:], scalar1=0.0, scalar2=None, op0=ALU.is_gt), r=[self.b_const], w=[b_c2])
        G(lambda e: e.iota(eoff[:], pattern=[[C, 64]], base=0, channel_multiplier=0, allow_small_or_imprecise_dtypes=True), w=[b_c2])

        halves = ((0, 512), (512, 512))

        def layer_norm(src, b_src, dst, b_dst, gam, bet, eps):
            A(lambda e: e.activation(out=junkf[:], in_=src[:], func=AF.Copy, accum_out=st[:, 0:1]), r=[b_src], w=[b_junkf, b_st])
            A(lambda e: e.activation(out=junkf[:], in_=src[:], func=AF.Square, accum_out=st[:, 1:2]), r=[b_src], w=[b_junkf, b_st])
            V(lambda e: e.tensor_scalar(out=st[:, 2:3], in0=st[:, 0:1], scalar1=1.0 / D, scalar2=None, op0=ALU.mult), r=[b_st], w=[b_st])
            V(lambda e: e.tensor_tensor(out=st[:, 3:4], in0=st[:, 2:3], in1=st[:, 2:3], op=ALU.mult), r=[b_st], w=[b_st])
            V(lambda e: e.scalar_tensor_tensor(out=st[:, 4:5], in0=st[:, 1:2], scalar=1.0 / D, in1=st[:, 3:4], op0=ALU.mult, op1=ALU.subtract),
              r=[b_st], w=[b_st])
            V(lambda e: e.tensor_scalar(out=st[:, 4:5], in0=st[:, 4:5], scalar1=eps, scalar2=None, op0=ALU.add), r=[b_st], w=[b_st])
            A(lambda e: e.sqrt(out=st[:, 5:6], in_=st[:, 4:5]), r=[b_st], w=[b_st])
            V(lambda e: e.reciprocal(out=st[:, 6:7], in_=st[:, 5:6]), r=[b_st], w=[b_st])
            V(lambda e: e.tensor_scalar(out=dst[:], in0=src[:], scalar1=st[:, 2:3], scalar2=st[:, 6:7], op0=ALU.subtract, op1=ALU.mult),
              r=[b_src, b_st], w=[b_dst])
            V(lambda e: e.tensor_tensor(out=dst[:], in0=dst[:], in1=gam[:], op=ALU.mult), r=[b_vec], w=[b_dst])
            V(lambda e: e.tensor_tensor(out=dst[:], in0=dst[:], in1=bet[:], op=ALU.add), r=[b_vec], w=[b_dst])
        self.layer_norm = layer_norm

        def front(i):
            h, b_h, hT, b_hT = h2[i % 2], b_h2[i % 2], hT2[i % 2], b_hT2[i % 2]
            xT, bxT = self.load_xT(self.x_own[i * 128:(i + 1) * 128, :])
            xs = self.last_s
            xf, b_xf = self.xf[xs], self.b_xf[xs]
            for gi, (w, banks) in enumerate(((wga, (1, 3)), (wgb, (4, 5)))):
                for hi, (c0, cw) in enumerate(halves):
                    bk = banks[hi]
                    for c in range(8):
                        T(lambda e, bk=bk, c=c, c0=c0, w=w: e.matmul(self.bank[bk][:], lhsT=xT[:, c, :], rhs=w[:, c, c0:c0 + 512],
                                                                   start=(c == 0), stop=(c == 7)), r=[bxT, b_w], w=[self.bbank[bk]])
                    V(lambda e, bk=bk, gi=gi, c0=c0, hi=hi: e.tensor_tensor(out=gtmp[hi][:], in0=self.bank[bk][:],
                                                                          in1=bgate[:, gi * D + c0:gi * D + c0 + 512], op=ALU.add),
                      r=[self.bbank[bk], b_vec], w=[b_gtmp[hi]])
                    A(lambda e, gi=gi, c0=c0, hi=hi: e.activation(out=gate[gi][:, c0:c0 + 512], in_=gtmp[hi][:], func=AF.Sigmoid),
                      r=[b_gtmp[hi]], w=[b_gate[gi]])
                    yield
            self.dma(ab[:, 0:512], self.aout_d[i * 128:(i + 1) * 128, :], [self.b_aoutd], [b_ab], b_ab)
            self.dma(ab[:, 512:768], self.bout_d[i * 128:(i + 1) * 128, :], [self.b_boutd], [b_ab], b_ab)
            bv2 = self.bview(2)
            for c in range(6):
                T(lambda e, c=c: e.transpose(out=bv2[:, c * 128:(c + 1) * 128], in_=ab[:, c * 128:(c + 1) * 128], identity=self.identb[:]),
                  r=[b_ab, self.b_const], w=[self.bbank[2]])
            A(lambda e: e.activation(out=abT[:], in_=bv2[:, 0:768].rearrange("p (c t) -> p c t", c=6), func=AF.Copy), r=[self.bbank[2]], w=[b_abT])
            yield
            for hi, (c0, cw) in enumerate(halves):
                ba, bb_ = (1, 3)[hi], (4, 5)[hi]
                for c in range(4):
                    T(lambda e, ba=ba, c=c, c0=c0: e.matmul(self.bank[ba][:], lhsT=abT[:, c, :], rhs=wba[:, c, c0:c0 + 512], start=(c == 0), stop=(c == 3)),
                      r=[b_abT, b_w], w=[self.bbank[ba]])
                for c in range(2):
                    T(lambda e, bb_=bb_, c=c, c0=c0: e.matmul(self.bank[bb_][:], lhsT=abT[:, 4 + c, :], rhs=wbb[:, c, c0:c0 + 512], start=(c == 0), stop=(c == 1)),
                      r=[b_abT, b_w], w=[self.bbank[bb_]])
                V(lambda e, ba=ba, c0=c0: e.tensor_tensor(out=gtmp[0][:], in0=self.bank[ba][:], in1=gate[0][:, c0:c0 + 512], op=ALU.mult),
                  r=[self.bbank[ba], b_gate[0]], w=[b_gtmp[0]])
                V(lambda e, bb_=bb_, c0=c0: e.tensor_tensor(out=gtmp[1][:], in0=self.bank[bb_][:], in1=gate[1][:, c0:c0 + 512], op=ALU.mult),
                  r=[self.bbank[bb_], b_gate[1]], w=[b_gtmp[1]])
                V(lambda e, c0=c0: e.tensor_tensor(out=mm[:, c0:c0 + 512], in0=gtmp[0][:], in1=gtmp[1][:], op=ALU.add),
                  r=[b_gtmp[0], b_gtmp[1]], w=[b_mm])
                yield
            for c in range(8):
                T(lambda e, c=c: e.transpose(out=bv2[:, c * 128:(c + 1) * 128], in_=mm[:, c * 128:(c + 1) * 128], identity=self.identb[:]),
                  r=[b_mm, self.b_const], w=[self.bbank[2]])
            A(lambda e: e.activation(out=mT[:], in_=bv2[:].rearrange("p (c t) -> p c t", c=8), func=AF.Copy), r=[self.bbank[2]], w=[b_mT])
            yield
            for hi, (c0, cw) in enumerate(halves):
                bk = (1, 3)[hi]
                for c in range(8):
                    T(lambda e, bk=bk, c=c, c0=c0: e.matmul(self.bank[bk][:], lhsT=mT[:, c, :], rhs=wo[:, c, c0:c0 + 512], start=(c == 0), stop=(c == 7)),
                      r=[b_mT, b_w], w=[self.bbank[bk]])
                V(lambda e, bk=bk, c0=c0: e.scalar_tensor_tensor(out=u[:, c0:c0 + 512], in0=xf[:, c0:c0 + 512], scalar=ALPHA, in1=self.bank[bk][:],
                                                                 op0=ALU.mult, op1=ALU.add), r=[b_xf, self.bbank[bk]], w=[b_u])
                yield
            layer_norm(u, b_u, h, b_h, ln1g, ln1b, 1e-5)
            yield
            if self.debug:
                self.dma(self.h_d[i * 128:(i + 1) * 128, :], h[:], [b_h], [self.b_hd], self.b_hd)
            hbb, b_hbb = hb[i % 2], b_hb[i % 2]
            A(lambda e: e.activation(out=hbb[:], in_=h[:], func=AF.Copy), r=[b_h], w=[b_hbb])
            for c in range(8):
                T(lambda e, c=c: e.transpose(out=bv2[:, c * 128:(c + 1) * 128], in_=hbb[:, c * 128:(c + 1) * 128], identity=self.identb[:]),
                  r=[b_hbb, self.b_const], w=[self.bbank[2]])
            V(lambda e: e.tensor_copy(out=hT[:], in_=bv2[:].rearrange("p (c t) -> p c t", c=8)), r=[self.bbank[2]], w=[b_hT])

        def back(i):
            h, b_h, hT, b_hT = h2[i % 2], b_h2[i % 2], hT2[i % 2], b_hT2[i % 2]
            hbb, b_hbb = hb[i % 2], b_hb[i % 2]
            p6, bp6 = self.bank[6], self.bbank[6]
            for c in range(8):
                T(lambda e, c=c: e.matmul(p6[:, 0:64], lhsT=hT[:, c, :], rhs=wr[:, c, :], start=(c == 0), stop=(c == 7)), r=[b_hT, b_w], w=[bp6])
            sc, bia, grp2, tt, mb, emk, ts, wfull, slotf, jk = (rt[:, k, :] for k in range(10))
            gm1, gm2, gs, s8, gmask, pen, v8 = rs[:, 0:8], rs[:, 8:16], rs[:, 16:24], rs[:, 24:32], rs[:, 32:40], rs[:, 40:48], rs[:, 48:56]
            den = rs[:, 56:57]
            g3 = lambda ap: ap.rearrange("p (g e) -> p g e", g=8)
            A(lambda e: e.activation(out=sc, in_=p6[:, 0:64], func=AF.Sigmoid), r=[bp6], w=[b_rt])
            yield
            V(lambda e: e.tensor_tensor(out=bia, in0=sc, in1=rbias[:], op=ALU.add), r=[b_vec], w=[b_rt])
            V(lambda e: e.tensor_reduce(out=gm1, in_=g3(bia), axis=AX.X, op=ALU.max), r=[b_rt], w=[b_rs])
            V(lambda e: e.tensor_tensor(out=g3(tt), in0=g3(bia), in1=gm1.unsqueeze(2).to_broadcast([128, 8, 8]), op=ALU.is_ge), r=[b_rs], w=[b_rt])
            V(lambda e: e.scalar_tensor_tensor(out=grp2, in0=tt, scalar=-1.0e9, in1=bia, op0=ALU.mult, op1=ALU.add), w=[b_rt])
            V(lambda e: e.tensor_reduce(out=gm2, in_=g3(grp2), axis=AX.X, op=ALU.max), r=[b_rt], w=[b_rs])
            V(lambda e: e.tensor_tensor(out=gs, in0=gm1, in1=gm2, op=ALU.add), w=[b_rs])
            V(lambda e: e.max(out=s8, in_=gs), w=[b_rs])
            V(lambda e: e.tensor_scalar(out=gmask, in0=gs, scalar1=s8[:, 3:4], scalar2=None, op0=ALU.is_ge), w=[b_rs])
            yield
            V(lambda e: e.tensor_scalar(out=pen, in0=gmask, scalar1=-1.0, scalar2=1.0e9, op0=ALU.add, op1=ALU.mult), w=[b_rs])
            V(lambda e: e.tensor_tensor(out=g3(tt), in0=g3(bia), in1=gmask.unsqueeze(2).to_broadcast([128, 8, 8]), op=ALU.mult), r=[b_rs], w=[b_rt])
            V(lambda e: e.tensor_tensor(out=g3(mb), in0=g3(tt), in1=pen.unsqueeze(2).to_broadcast([128, 8, 8]), op=ALU.add), r=[b_rs], w=[b_rt])
            V(lambda e: e.max(out=v8, in_=mb), r=[b_rt], w=[b_rs])
            V(lambda e: e.tensor_scalar(out=emk, in0=mb, scalar1=v8[:, 7:8], scalar2=None, op0=ALU.is_ge), r=[b_rs], w=[b_rt])
            V(lambda e: e.tensor_copy(out=EM[:, i, :], in_=emk), r=[b_rt], w=[b_em])
            V(lambda e: e.tensor_tensor(out=ts, in0=sc, in1=emk, op=ALU.mult), w=[b_rt])
            V(lambda e: e.tensor_reduce(out=den, in_=ts, axis=AX.X, op=ALU.add), r=[b_rt], w=[b_rs])
            V(lambda e: e.reciprocal(out=den, in_=den), w=[b_rs])
            V(lambda e: e.tensor_scalar(out=wfull, in0=ts, scalar1=den, scalar2=2.5, op0=ALU.mult, op1=ALU.mult), r=[b_rs], w=[b_rt])
            yield
            p7, bp7 = self.bank[7], self.bbank[7]
            for j in range(i):
                T(lambda e, j=j: e.matmul(p7[:, 0:64], lhsT=self.onesb[:], rhs=EM[:, j, :], start=(j == 0), stop=False), r=[b_em, self.b_const], w=[bp7])
            T(lambda e: e.matmul(p7[:, 0:64], lhsT=UT[:], rhs=EM[:, i, :], start=(i == 0), stop=True), r=[b_em, b_c2], w=[bp7])
            V(lambda e: e.tensor_scalar(out=jk, in0=p7[:, 0:64], scalar1=C - 0.5, scalar2=1.0e6, op0=ALU.is_ge, op1=ALU.mult), r=[bp7], w=[b_rt])
            V(lambda e: e.tensor_tensor(out=slotf, in0=p7[:, 0:64], in1=eoff[:], op=ALU.add), r=[bp7, b_c2], w=[b_rt])
            V(lambda e: e.tensor_tensor(out=slotf, in0=slotf, in1=jk, op=ALU.add), w=[b_rt])
            yield
            for k in range(8):
                V(lambda e, k=k: e.scalar_tensor_tensor(out=jk, in0=mb, scalar=v8[:, k:k + 1], in1=slotf, op0=ALU.is_equal, op1=ALU.mult,
                                                        accum_out=s8f[:, k:k + 1]), r=[b_rs], w=[b_rt])
                V(lambda e, k=k: e.scalar_tensor_tensor(out=jk, in0=mb, scalar=v8[:, k:k + 1], in1=wfull, op0=ALU.is_equal, op1=ALU.mult,
                                                        accum_out=self.W8[:, i, k:k + 1]), r=[b_rs], w=[b_rt, self.b_route])
            V(lambda e: e.tensor_copy(out=self.SLOT8[:, i, :], in_=s8f[:]), r=[b_rt], w=[self.b_route])
            yield
            for k in range(8):
                self.P.op("gpsimd", lambda e, k=k: e.indirect_dma_start(
                    out=self.xe_d[:, :], out_offset=bass.IndirectOffsetOnAxis(ap=self.SLOT8[:, i, k:k + 1], axis=0),
                    in_=hbb[:], in_offset=None, bounds_check=self.bc_reg(e, NSLOT - 1), oob_is_err=False),
                    [b_hbb, self.b_route], [self.b_xed], dma_out=self.b_xed)
            yield
            for fi, w in enumerate((ws1, ws3)):
                for fc in range(2):
                    r0 = (fi * 2 + fc) * 128
                    for c in range(8):
                        T(lambda e, w=w, fc=fc, c=c, r0=r0: e.matmul(p6[:, r0:r0 + 128], lhsT=w[:, c, fc * 128:(fc + 1) * 128], rhs=hT[:, c, :],
                                                                     start=(c == 0), stop=(c == 7)), r=[b_w, b_hT], w=[bp6])
            A(lambda e: e.activation(out=sil[:], in_=p6[:, 0:256], func=AF.Silu), r=[bp6], w=[b_sil])
            V(lambda e: e.tensor_tensor(out=GT[:], in0=p6[:, 256:512], in1=sil[:], op=ALU.mult), r=[bp6, b_sil], w=[b_gt])
            yield
            for hi, (c0, cw) in enumerate(halves):
                bk = (1, 3)[hi]
                for fc in range(2):
                    T(lambda e, bk=bk, fc=fc, c0=c0: e.matmul(self.bank[bk][:], lhsT=GT[:, fc * 128:(fc + 1) * 128], rhs=ws2[:, fc, c0:c0 + 512],
                                                              start=(fc == 0), stop=(fc == 1)), r=[b_gt, b_w], w=[self.bbank[bk]])
                V(lambda e, bk=bk, c0=c0: e.scalar_tensor_tensor(out=base[:, c0:c0 + 512], in0=h[:, c0:c0 + 512], scalar=ALPHA, in1=self.bank[bk][:],
                                                                 op0=ALU.mult, op1=ALU.add), r=[b_h, self.bbank[bk]], w=[b_base])
                yield
            self.dma(self.base_d[i * 128:(i + 1) * 128, :], base[:], [b_base], [self.b_based], self.b_based, eng="gpsimd")


        for _ in front(0):
            pass
        for i in range(n_own):
            gb = back(i)
            if i + 1 < n_own:
                ga = front(i + 1)
                na, nb = 12, 9
                done_a = done_b = False
                ca = cb = 0
                while not (done_a and done_b):
                    if not done_a and (done_b or ca * nb <= cb * na):
                        try:
                            next(ga)
                        except StopIteration:
                            done_a = True
                        ca += 1
                    else:
                        try:
                            next(gb)
                        except StopIteration:
                            done_b = True
                        cb += 1
            else:
                for _ in gb:
                    pass


    def phase3(self):
        V, A, G, T = self.V, self.A, self.G, self.T
        sb = self.sb
        C = self.CAP
        NSLOT = 64 * C
        NCH = C // 256
        w1_d = self.dram_in("w1_e", [64, D, 256], F32)
        w3_d = self.dram_in("w3_e", [64, D, 256], F32)
        w2_d = self.dram_in("w2_e", [64, 256, D], F32)
        self.ye_d = self.dram_tmp("ye_d", [NSLOT, D], BF16)
        self.b_yed = Buf("yed")
        n_exp = self.n_exp
        w1s = [sb("w1s%d" % k, [128, 8, 256], F32) for k in range(2)]
        w3s = [sb("w3s%d" % k, [128, 8, 256], F32) for k in range(2)]
        w2s = [sb("w2s%d" % k, [128, 2, D], F32) for k in range(2)]
        b_ws = [[Buf("w%ds%d" % (j, k)) for j in range(3)] for k in range(2)]
        w1b = [sb("w1b%d" % k, [128, 8, 256], BF16) for k in range(2)]
        w3b = [sb("w3b%d" % k, [128, 8, 256], BF16) for k in range(2)]
        w2b = [sb("w2b%d" % k, [128, 2, D], BF16) for k in range(2)]
        b_wb = [Buf("wb%d" % k) for k in range(2)]
        xe = [sb("xe%d" % k, [128, 2, D], BF16) for k in range(2)]
        b_xe = [Buf("xe%d" % k) for k in range(2)]
        XeT = [sb("XeT%d" % k, [128, 8, 256], BF16) for k in range(2)]
        b_xet = [Buf("XeT%d" % k) for k in range(2)]
        sil = [sb("sil3%d" % k, [128, 512], F32) for k in range(2)]
        b_sil = [Buf("sil3%d" % k) for k in range(2)]
        GT = [sb("GT3%d" % k, [128, 512], BF16) for k in range(2)]
        b_gt = [Buf("GT3%d" % k) for k in range(2)]
        Y = [sb("Y%d" % k, [128, D], BF16) for k in range(4)]
        b_y = [Buf("Y%d" % k) for k in range(4)]
        steps = [(ex, ch) for ex in range(n_exp) for ch in range(NCH)]

        def load_w(ex):
            ws = ex % 2
            self.dma(w1s[ws][:], w1_d[ex].rearrange("(c p) n -> p c n", p=128), [], [b_ws[ws][0]], b_ws[ws][0])
            self.dma(w3s[ws][:], w3_d[ex].rearrange("(c p) n -> p c n", p=128), [], [b_ws[ws][1]], b_ws[ws][1])
            self.dma(w2s[ws][:], w2_d[ex].rearrange("(c p) n -> p c n", p=128), [], [b_ws[ws][2]], b_ws[ws][2])
            A(lambda e, ws=ws: e.activation(out=w1b[ws][:], in_=w1s[ws][:], func=AF.Copy), r=[b_ws[ws][0]], w=[b_wb[ws]])
            V(lambda e, ws=ws: e.tensor_copy(out=w3b[ws][:], in_=w3s[ws][:]), r=[b_ws[ws][1]], w=[b_wb[ws]])
            G(lambda e, ws=ws: e.tensor_copy(out=w2b[ws][:], in_=w2s[ws][:]), r=[b_ws[ws][2]], w=[b_wb[ws]])

        def stage1(n):
            ex, ch = steps[n]
            ws = ex % 2
            if ch == 0 and ex == 0:
                load_w(0)
            if ch == 1 and ex + 1 < n_exp:
                load_w(ex + 1)
            t = n % 2
            r0 = ex * C + ch * 256
            self.dma(xe[t][:], self.xe_d[r0:r0 + 256, :].rearrange("(k p) d -> p k d", p=128), [self.b_xed], [b_xe[t]], b_xe[t])
            for kk in range(2):
                bvt = self.bview(kk)
                for c in range(8):
                    T(lambda e, c=c, t=t, kk=kk, bvt=bvt: e.transpose(out=bvt[:, c * 128:(c + 1) * 128], in_=xe[t][:, kk, c * 128:(c + 1) * 128],
                                                                  identity=self.identb[:]), r=[b_xe[t], self.b_const], w=[self.bbank[kk]])
                if kk == 0:
                    A(lambda e, t=t, bvt=bvt: e.activation(out=XeT[t][:, :, 0:128], in_=bvt[:].rearrange("p (c t) -> p c t", c=8), func=AF.Copy),
                      r=[self.bbank[kk]], w=[b_xet[t]])
                else:
                    V(lambda e, t=t, bvt=bvt: e.tensor_copy(out=XeT[t][:, :, 128:256], in_=bvt[:].rearrange("p (c t) -> p c t", c=8)),
                      r=[self.bbank[kk]], w=[b_xet[t]])
            for fi, w in enumerate((w1b[ws], w3b[ws])):
                bk = 2 + fi
                for fc in range(2):
                    for c in range(8):
                        T(lambda e, w=w, fc=fc, c=c, t=t, bk=bk: e.matmul(self.bank[bk][:, fc * 256:(fc + 1) * 256], lhsT=w[:, c, fc * 128:(fc + 1) * 128],
                                                                      rhs=XeT[t][:, c, :], start=(c == 0), stop=(c == 7)),
                          r=[b_wb[ws], b_xet[t]], w=[self.bbank[bk]])
            A(lambda e, t=t: e.activation(out=sil[t][:], in_=self.bank[2][:], func=AF.Silu), r=[self.bbank[2]], w=[b_sil[t]])
            V(lambda e, t=t: e.tensor_tensor(out=GT[t][:], in0=self.bank[3][:], in1=sil[t][:], op=ALU.mult), r=[self.bbank[3], b_sil[t]], w=[b_gt[t]])

        def stage2(n):
            ex, ch = steps[n]
            ws = ex % 2
            t = n % 2
            for kk in range(2):
                r0 = ex * C + ch * 256 + kk * 128
                ybanks = (4, 5) if kk == 0 else (6, 7)
                yi = (2 * n + kk) % 4
                for hi in range(2):
                    bk = ybanks[hi]
                    for fc in range(2):
                        T(lambda e, bk=bk, fc=fc, hi=hi, t=t, ws=ws, kk=kk: e.matmul(self.bank[bk][:], lhsT=GT[t][:, fc * 256 + kk * 128:fc * 256 + (kk + 1) * 128],
                                                                                 rhs=w2b[ws][:, fc, hi * 512:(hi + 1) * 512], start=(fc == 0), stop=(fc == 1)),
                          r=[b_gt[t], b_wb[ws]], w=[self.bbank[bk]])
                A(lambda e, yi=yi, bk=ybanks[0]: e.activation(out=Y[yi][:, 0:512], in_=self.bank[bk][:], func=AF.Copy), r=[self.bbank[ybanks[0]]], w=[b_y[yi]])
                V(lambda e, yi=yi, bk=ybanks[1]: e.tensor_copy(out=Y[yi][:, 512:1024], in_=self.bank[bk][:]), r=[self.bbank[ybanks[1]]], w=[b_y[yi]])
                self.dma(self.ye_d[r0:r0 + 128, :], Y[yi][:], [b_y[yi]], [self.b_yed], self.b_yed, eng="gpsimd")

        ns = len(steps)
        stage1(0)
        for n in range(ns):
            if n + 1 < ns:
                stage1(n + 1)
            stage2(n)

    def phase4(self):
        V, A, G, T = self.V, self.A, self.G, self.T
        sb = self.sb
        C = self.CAP
        NSLOT = 64 * C
        ln2g_d, ln2b_d = self.dram_in("ln2_g", [1, D], F32), self.dram_in("ln2_b", [1, D], F32)
        self.out_d = self.dram_out("out_d", [NOWN * 128, D], F32)
        self.b_outd = Buf("outd")
        ln2g, ln2b = sb("ln2g", [128, D], F32), sb("ln2b", [128, D], F32)
        b_vec = Buf("vec4")
        self.load_bcast(ln2g, ln2g_d, b_vec)
        self.load_bcast(ln2b, ln2b_d, b_vec)
        acc = [sb("acc%d" % k, [128, D], F32) for k in range(2)]
        b_acc = [Buf("acc%d" % k) for k in range(2)]
        yk = [sb("yk%d" % k, [128, D], BF16) for k in range(8)]
        b_yk = [Buf("yk%d" % k) for k in range(8)]
        o = [sb("o%d" % k, [128, D], F32) for k in range(2)]
        b_o = [Buf("o%d" % k) for k in range(2)]
        junkf = sb("junk4", [128, D], F32)
        st = sb("st4", [128, 8], F32)
        b_junkf, b_st = Buf("junk4"), Buf("st4")

        def layer_norm(src, b_src, dst, b_dst, gam, bet, eps):
            A(lambda e: e.activation(out=junkf[:], in_=src[:], func=AF.Copy, accum_out=st[:, 0:1]), r=[b_src], w=[b_junkf, b_st])
            A(lambda e: e.activation(out=junkf[:], in_=src[:], func=AF.Square, accum_out=st[:, 1:2]), r=[b_src], w=[b_junkf, b_st])
            V(lambda e: e.tensor_scalar(out=st[:, 2:3], in0=st[:, 0:1], scalar1=1.0 / D, scalar2=None, op0=ALU.mult), r=[b_st], w=[b_st])
            V(lambda e: e.tensor_tensor(out=st[:, 3:4], in0=st[:, 2:3], in1=st[:, 2:3], op=ALU.mult), r=[b_st], w=[b_st])
            V(lambda e: e.scalar_tensor_tensor(out=st[:, 4:5], in0=st[:, 1:2], scalar=1.0 / D, in1=st[:, 3:4], op0=ALU.mult, op1=ALU.subtract),
              r=[b_st], w=[b_st])
            V(lambda e: e.tensor_scalar(out=st[:, 4:5], in0=st[:, 4:5], scalar1=eps, scalar2=None, op0=ALU.add), r=[b_st], w=[b_st])
            A(lambda e: e.sqrt(out=st[:, 5:6], in_=st[:, 4:5]), r=[b_st], w=[b_st])
            V(lambda e: e.reciprocal(out=st[:, 6:7], in_=st[:, 5:6]), r=[b_st], w=[b_st])
            V(lambda e: e.tensor_scalar(out=dst[:], in0=src[:], scalar1=st[:, 2:3], scalar2=st[:, 6:7], op0=ALU.subtract, op1=ALU.mult),
              r=[b_src, b_st], w=[b_dst])
            V(lambda e: e.tensor_tensor(out=dst[:], in0=dst[:], in1=gam[:], op=ALU.mult), r=[b_vec], w=[b_dst])
            V(lambda e: e.tensor_tensor(out=dst[:], in0=dst[:], in1=bet[:], op=ALU.add), r=[b_vec], w=[b_dst])

        n = 0
        self.dma(acc[0][:], self.base_d[0:128, :], [self.b_based], [b_acc[0]], b_acc[0])
        for i in range(self.n_own):
            a, b_a = acc[i % 2], b_acc[i % 2]
            if i + 1 < self.n_own:
                self.dma(acc[(i + 1) % 2][:], self.base_d[(i + 1) * 128:(i + 2) * 128, :], [self.b_based], [b_acc[(i + 1) % 2]], b_acc[(i + 1) % 2])
            for k in range(8):
                t = n % 8
                n += 1
                self.P.op("gpsimd", lambda e, t=t, i=i, k=k: e.indirect_dma_start(
                    out=yk[t][:], out_offset=None, in_=self.ye_d[:, :],
                    in_offset=bass.IndirectOffsetOnAxis(ap=self.SLOT8[:, i, k:k + 1], axis=0), bounds_check=self.bc_reg(e, NSLOT - 1), oob_is_err=False),
                    [self.b_yed, self.b_route], [b_yk[t]], dma_out=b_yk[t])
                V(lambda e, t=t, i=i, k=k, a=a: e.scalar_tensor_tensor(out=a[:], in0=yk[t][:], scalar=self.W8[:, i, k:k + 1], in1=a[:],
                                                                       op0=ALU.mult, op1=ALU.add), r=[b_yk[t], self.b_route], w=[b_a])
            oo, b_oo = o[i % 2], b_o[i % 2]
            layer_norm(a, b_a, oo, b_oo, ln2g, ln2b, 1e-5)
            self.dma(self.out_d[i * 128:(i + 1) * 128, :], oo[:], [b_oo], [self.b_outd], self.b_outd)


def build_program(n_own=NOWN, debug=False, phases="ab234", n_exp=64):
    nc = bass.Bass("TRN2", target_bir_lowering=False)
    st = ExitStack()
    with st:
        B = Builder(nc, st, n_own=n_own, debug=debug)
        B.CAP = 768
        B.n_exp = n_exp
        B.setup_common()
        B.phase1a()
        fin = [B.b_aoutd]
        if "b" in phases:
            B.phase_reset(B.mark)
            B.phase1b()
            fin.append(B.b_boutd)
        if "2" in phases:
            B.phase_reset(B.mark)
            B.phase2()
            fin += [B.b_based, B.b_xed]
            if debug:
                fin.append(B.b_hd)
        if "3" in phases:
            B.phase_reset(B.mark)
            B.phase3()
            fin.append(B.b_yed)
        if "4" in phases:
            B.phase_reset(B.mark)
            B.phase4()
            fin.append(B.b_outd)
        B.P.final_wait("sync", fin)
        B.P.emit()
        print("[kernel] semaphores used:", B.P.nsem, "ops:", {e: len(v) for e, v in B.P.ops.items()})
    return nc


def rope_consts():
    inv16 = ROPE_THETA ** (-np.arange(0, 16, 2, dtype=np.float32) / 16)
    inv8 = ROPE_THETA ** (-np.arange(0, 8, 2, dtype=np.float32) / 8)
    inv = np.concatenate([inv16, inv16, inv8, inv8]).astype(np.float32)
    off = np.concatenate([np.full(8, math.pi / 2), np.zeros(8), np.full(4, math.pi / 2), np.zeros(4)]).astype(np.float32)
    return np.tile(np.concatenate([inv, off])[None, :], (128, 1)).astype(np.float32)


def make_in_maps(inputs, cores=range(8)):
    x = inputs["x"]
    positions = inputs["positions"]
    w_in = inputs["w_in"][0]
    offs = np.cumsum([0, 512, 256, 16, 256, 32, 8, 768, 768, 768, 1024, 1024])
    col = lambda k: slice(offs[k], offs[k + 1])
    wk_dsa = np.ascontiguousarray(np.concatenate([w_in[:, col(1)], w_in[:, col(2)], w_in[:, col(4)]], axis=1))
    wq_dsa = np.ascontiguousarray(np.concatenate([w_in[:, col(0)], w_in[:, col(3)], w_in[:, col(5)]], axis=1))
    w_bq = np.ascontiguousarray(w_in[:, col(6)])
    w_bk = np.ascontiguousarray(w_in[:, col(7)])
    w_bv = np.ascontiguousarray(w_in[:, col(8)])
    w_ga = np.ascontiguousarray(w_in[:, col(9)])
    w_gb = np.ascontiguousarray(w_in[:, col(10)])
    f32c = lambda a: np.ascontiguousarray(a, dtype=np.float32)
    import ml_dtypes
    zeros_bf = np.zeros((1024, D), dtype=ml_dtypes.bfloat16)
    w1_e, w3_e, w2_e = f32c(inputs["w1_e"][0]), f32c(inputs["w3_e"][0]), f32c(inputs["w2_e"][0])
    wuk_t = np.ascontiguousarray(inputs["w_uk"][0].transpose(2, 1, 0))
    wuv = np.ascontiguousarray(inputs["w_uv"][0].reshape(256, 512))
    rc = rope_consts()
    maps = []
    for c in cores:
        b, par = c // 2, c % 2
        xb = x[b]
        x_own = np.ascontiguousarray(xb.reshape(NBLK, 128, D)[par::2].reshape(NOWN * 128, D))
        pos_t = np.ascontiguousarray(positions[b].reshape(NBLK, 128).T.astype(np.int32))
        pos_own_t = np.ascontiguousarray(positions[b].reshape(NBLK, 128)[par::2].T.astype(np.int32))
        maps.append({
            "x_all": np.ascontiguousarray(xb), "x_own": x_own, "pos_all_t": pos_t, "pos_own_t": pos_own_t,
            "par": np.full((128, 1), float(par), np.float32), "rope_c": rc,
            "wk_dsa": wk_dsa, "wq_dsa": wq_dsa, "wuk_t": wuk_t, "wuv": wuv,
            "w_bq": w_bq, "w_bk": w_bk, "w_bv": w_bv, "w_ga": w_ga, "w_gb": w_gb,
            "b_gate": f32c(inputs["b_gate"].reshape(1, 2048)), "w_branch_a": f32c(inputs["w_branch_a"][0]),
            "w_branch_b": f32c(inputs["w_branch_b"][0]), "w_o": f32c(inputs["w_o"][0]),
            "ln1_g": f32c(inputs["ln1_g"].reshape(1, D)), "ln1_b": f32c(inputs["ln1_b"].reshape(1, D)),
            "w_router": f32c(inputs["w_router"][0]), "router_bias": f32c(inputs["router_bias"].reshape(1, 64)),
            "ws1": f32c(inputs["ws1"][0]), "ws3": f32c(inputs["ws3"][0]), "ws2": f32c(inputs["ws2"][0]),
            "w1_e": w1_e, "w3_e": w3_e, "w2_e": w2_e, "zeros_d": zeros_bf,
            "ln2_g": f32c(inputs["ln2_g"].reshape(1, D)), "ln2_b": f32c(inputs["ln2_b"].reshape(1, D)),
            "g_kv": np.ascontiguousarray(inputs["g_kv"].reshape(1, 256)),
        })
    return maps


_NC_CACHE = {}


def kernel(**inputs):
    inputs = {k: np.asarray(v) for k, v in inputs.items()}
    if "nc" not in _NC_CACHE:
        _NC_CACHE["nc"] = build_program()
    nc = _NC_CACHE["nc"]
    maps = make_in_maps(inputs, cores=range(8))
    res = run_bass_kernel_spmd(nc, maps, core_ids=list(range(8)))
    out = np.empty((4, S, D), np.float32)
    for c in range(8):
        b, par = c // 2, c % 2
        o = np.asarray(res.results[c]["out_d"], dtype=np.float32).reshape(NOWN, 128, D)
        out[b].reshape(NBLK, 128, D)[par::2] = o
    return out
```
